# Optimizing a Trainium2 kernel written in Bass

```python
import math
import jax
import jax.numpy as jnp
from jax import lax
import numpy as np

D_MODEL = 2048
BATCH = 1
SEQ = 8192
DEPTH = 1

CHUNK = 64
Q_BLOCK = 128
DIFF_HEADS = 8
DIFF_HEAD_DIM = 64
DIFF_V_DIM = 2 * DIFF_HEAD_DIM
DIFF_QK_WIDTH = DIFF_HEADS * 2 * DIFF_HEAD_DIM
DIFF_WIDTH = DIFF_HEADS * DIFF_V_DIM
GDN_HEADS = 8
GDN_HEAD_DIM = 128
GDN_WIDTH = GDN_HEADS * GDN_HEAD_DIM
CONV_WIDTH = 4
REL_BUCKETS = 32
REL_MAX_DISTANCE = 128
N_EXPERTS = 64
TOP_K = 8
N_GROUPS = 8
TOPK_GROUPS = 4
EXPERT_FF = 512
SHARED_FF = 512
ROUTED_SCALE = 2.5
EXPERT_BLOCK = 128
DEEPNORM_ALPHA = (2 * DEPTH) ** 0.25
DEEPNORM_BETA = (8 * DEPTH) ** -0.25
LN_EPS = 1e-5
NORM_EPS = 1e-6
IN_SPLIT = (DIFF_QK_WIDTH, DIFF_QK_WIDTH, DIFF_WIDTH,
            GDN_WIDTH, GDN_WIDTH, GDN_WIDTH, GDN_WIDTH, GDN_HEADS, GDN_HEADS,
            D_MODEL, D_MODEL)
IN_WIDTH = sum(IN_SPLIT)

kernel_name = "chunk_causal_hybrid_diffattn_gdn_moe"


def layer_norm(x, g, b):
    xf = x.astype(jnp.float32)
    mu = jnp.mean(xf, axis=-1, keepdims=True)
    var = jnp.mean(jnp.square(xf - mu), axis=-1, keepdims=True)
    y = (xf - mu) * lax.rsqrt(var + LN_EPS) * g.astype(jnp.float32) + b.astype(jnp.float32)
    return y.astype(x.dtype)


def rms_norm(x, w):
    xf = x.astype(jnp.float32)
    y = xf * lax.rsqrt(jnp.mean(jnp.square(xf), axis=-1, keepdims=True) + NORM_EPS) * w.astype(jnp.float32)
    return y.astype(x.dtype)


def l2_normalize(x):
    return x * lax.rsqrt(jnp.sum(jnp.square(x), axis=-1, keepdims=True) + NORM_EPS)


def t5_bucket(rel):
    nb = REL_BUCKETS // 2
    base = jnp.where(rel > 0, nb, 0)
    n = jnp.abs(rel)
    max_exact = nb // 2
    nf = jnp.maximum(n, 1).astype(jnp.float32)
    large = max_exact + (jnp.log(nf / max_exact) / math.log(REL_MAX_DISTANCE / max_exact)
                         * (nb - max_exact)).astype(jnp.int32)
    large = jnp.minimum(large, nb - 1)
    return base + jnp.where(n < max_exact, n, large)


def causal_conv(x, w):
    T = x.shape[1]
    xp = jnp.pad(x, ((0, 0), (CONV_WIDTH - 1, 0), (0, 0)))
    out = xp[:, 0:T] * w[0]
    for j in range(1, CONV_WIDTH):
        out = out + xp[:, j:j + T] * w[j]
    return out


def diff_attention(q, k, v, lam_vecs, subln_w, rel_table, lambda_init):
    B, T, _ = q.shape
    H, dh, dv = DIFF_HEADS, DIFF_HEAD_DIM, DIFF_V_DIM
    q = q.reshape(B, T, H, 2, dh).transpose(3, 0, 2, 1, 4)
    k = k.reshape(B, T, H, 2, dh).transpose(3, 0, 2, 1, 4)
    v = v.reshape(B, T, H, dv).transpose(0, 2, 1, 3)
    lf = lam_vecs.astype(jnp.float32)
    lam = jnp.exp(jnp.sum(lf[0] * lf[1])) - jnp.exp(jnp.sum(lf[2] * lf[3])) + lambda_init
    n_blocks = T // Q_BLOCK
    qb = q.reshape(2, B, H, n_blocks, Q_BLOCK, dh).transpose(3, 0, 1, 2, 4, 5)
    kpos = jnp.arange(T)
    scale = dh ** -0.5

    def block(args):
        qblk, bi = args
        qpos = bi * Q_BLOCK + jnp.arange(Q_BLOCK)
        visible = (kpos[None, :] // CHUNK) <= (qpos[:, None] // CHUNK)
        bias = rel_table[t5_bucket(kpos[None, :] - qpos[:, None])]
        bias = jnp.transpose(bias, (2, 0, 1)).astype(jnp.float32)
        s = jnp.einsum('mbhqd,mbhkd->mbhqk', qblk, k).astype(jnp.float32) * scale + bias
        p = jax.nn.softmax(jnp.where(visible, s, -jnp.inf), axis=-1)
        a = p[0] - lam * p[1]
        return jnp.einsum('bhqk,bhkd->bhqd', a.astype(v.dtype), v)

    o = lax.map(block, (qb, jnp.arange(n_blocks)))
    o = o.transpose(1, 0, 3, 2, 4).reshape(B, T, H, dv)
    o = rms_norm(o, subln_w) * (1.0 - lambda_init)
    return o.reshape(B, T, DIFF_WIDTH)


def gated_delta_chunked(q, k, v, g, beta):
    B, H, T, dk = q.shape
    dv = v.shape[-1]
    C = CHUNK
    N = T // C
    q = q * (dk ** -0.5)
    q = q.reshape(B, H, N, C, dk)
    k = k.reshape(B, H, N, C, dk)
    v = v.reshape(B, H, N, C, dv)
    beta = beta.reshape(B, H, N, C)
    gc = jnp.cumsum(g.reshape(B, H, N, C), axis=-1)
    tri = jnp.tril(jnp.ones((C, C), dtype=bool))
    strict = jnp.tril(jnp.ones((C, C), dtype=bool), -1)
    decay = jnp.exp(jnp.where(tri, gc[..., :, None] - gc[..., None, :], -jnp.inf))
    kb = k * beta[..., None]
    L = jnp.where(strict, jnp.einsum('bhncd,bhnsd->bhncs', kb, k) * decay, 0.0)
    eye = jnp.broadcast_to(jnp.eye(C, dtype=L.dtype), L.shape)
    Tm = lax.linalg.triangular_solve(L + eye, eye, left_side=True, lower=True, unit_diagonal=True)
    u = jnp.einsum('bhncs,bhnsd->bhncd', Tm, v * beta[..., None])
    w = jnp.einsum('bhncs,bhnsd->bhncd', Tm, kb * jnp.exp(gc)[..., None])
    a_qk = jnp.where(tri, jnp.einsum('bhncd,bhnsd->bhncs', q, k) * decay, 0.0)
    q_dec = q * jnp.exp(gc)[..., None]
    k_dec = k * jnp.exp(gc[..., -1:] - gc)[..., None]
    g_last = jnp.exp(gc[..., -1])

    def step(S, inp):
        u_n, w_n, qd_n, aqk_n, kd_n, gl_n = inp
        v_new = u_n - jnp.einsum('bhck,bhkv->bhcv', w_n, S)
        o_n = jnp.einsum('bhck,bhkv->bhcv', qd_n, S) + jnp.einsum('bhcs,bhsv->bhcv', aqk_n, v_new)
        S = S * gl_n[..., None, None] + jnp.einsum('bhck,bhcv->bhkv', kd_n, v_new)
        return S, o_n

    xs = tuple(jnp.moveaxis(t, 2, 0) for t in (u, w, q_dec, a_qk, k_dec, g_last))
    S0 = jnp.zeros((B, H, dk, dv), jnp.float32)
    _, o = lax.scan(step, S0, xs)
    return jnp.moveaxis(o, 0, 2).reshape(B, H, T, dv)


def hybrid_mixer(x, w_in, conv_w, a_log, dt_bias, gdn_norm_w, diff_lambda, diff_subln_w,
                 rel_table, w_branch_a, w_branch_b, w_out, lambda_init):
    B, T, _ = x.shape
    proj = x @ w_in
    points = np.cumsum(IN_SPLIT)[:-1].tolist()
    dq, dk, dvv, gq, gk, gv, gz, ga, gb, gate_a, gate_b = jnp.split(proj, points, axis=-1)
    ya = diff_attention(dq, dk, dvv, diff_lambda, diff_subln_w, rel_table, lambda_init)
    qkv = jax.nn.silu(causal_conv(jnp.concatenate([gq, gk, gv], axis=-1), conv_w))
    gq, gk, gv = jnp.split(qkv, 3, axis=-1)
    heads = lambda t: t.reshape(B, T, GDN_HEADS, GDN_HEAD_DIM).transpose(0, 2, 1, 3).astype(jnp.float32)
    q = l2_normalize(heads(gq))
    k = l2_normalize(heads(gk))
    v = heads(gv)
    beta = jax.nn.sigmoid(gb.astype(jnp.float32)).transpose(0, 2, 1)
    g = (-jnp.exp(a_log.astype(jnp.float32))
         * jax.nn.softplus(ga.astype(jnp.float32) + dt_bias.astype(jnp.float32))).transpose(0, 2, 1)
    o = gated_delta_chunked(q, k, v, g, beta).transpose(0, 2, 1, 3)
    z = gz.reshape(B, T, GDN_HEADS, GDN_HEAD_DIM).astype(jnp.float32)
    o = rms_norm(o, gdn_norm_w) * jax.nn.silu(z)
    yb = o.reshape(B, T, GDN_WIDTH).astype(x.dtype)
    merged = jax.nn.sigmoid(gate_a) * (ya @ w_branch_a) + jax.nn.sigmoid(gate_b) * (yb @ w_branch_b)
    return merged @ w_out


def moe_ffn(x, w_router, router_bias, w_gate, w_up, w_down, ws_gate, ws_up, ws_down):
    B, T, D = x.shape
    n_tok = B * T
    xf = x.reshape(n_tok, D)
    scores = jax.nn.sigmoid(xf.astype(jnp.float32) @ w_router.astype(jnp.float32))
    choice = scores + router_bias.astype(jnp.float32)
    grp = choice.reshape(n_tok, N_GROUPS, N_EXPERTS // N_GROUPS)
    grp_score = lax.top_k(grp, 2)[0].sum(-1)
    _, gidx = lax.top_k(grp_score, TOPK_GROUPS)
    gmask = jax.nn.one_hot(gidx, N_GROUPS, dtype=jnp.float32).sum(1)
    emask = jnp.repeat(gmask, N_EXPERTS // N_GROUPS, axis=-1) > 0
    _, eidx = lax.top_k(jnp.where(emask, choice, -jnp.inf), TOP_K)
    wsel = jnp.take_along_axis(scores, eidx, axis=-1)
    wsel = wsel / jnp.sum(wsel, axis=-1, keepdims=True) * ROUTED_SCALE
    n_assign = n_tok * TOP_K
    n_blocks = -(-n_assign // EXPERT_BLOCK) + N_EXPERTS
    n_rows = n_blocks * EXPERT_BLOCK
    flat_e = eidx.reshape(-1)
    flat_w = wsel.reshape(-1)
    order = jnp.argsort(flat_e)
    sorted_e = flat_e[order]
    counts = jnp.bincount(flat_e, length=N_EXPERTS)
    starts = jnp.cumsum(counts) - counts
    padded = (counts + EXPERT_BLOCK - 1) // EXPERT_BLOCK * EXPERT_BLOCK
    pad_ends = jnp.cumsum(padded)
    pad_starts = pad_ends - padded
    dest = pad_starts[sorted_e] + jnp.arange(n_assign) - starts[sorted_e]
    row_token = jnp.full((n_rows,), n_tok, jnp.int32).at[dest].set((order // TOP_K).astype(jnp.int32))
    row_gate = jnp.zeros((n_rows,), jnp.float32).at[dest].set(flat_w[order])
    block_expert = jnp.minimum(jnp.searchsorted(pad_ends, jnp.arange(n_blocks) * EXPERT_BLOCK, side='right'),
                               N_EXPERTS - 1)
    x_pad = jnp.concatenate([xf, jnp.zeros((1, D), xf.dtype)], axis=0)

    def block(args):
        tok, gate, e = args
        xb = x_pad[tok]
        h = jax.nn.silu(xb @ w_gate[e]) * (xb @ w_up[e])
        return (h @ w_down[e]) * gate[:, None].astype(xb.dtype)

    y = lax.map(block, (row_token.reshape(n_blocks, EXPERT_BLOCK),
                        row_gate.reshape(n_blocks, EXPERT_BLOCK), block_expert))
    routed = jax.ops.segment_sum(y.reshape(n_rows, D), row_token, num_segments=n_tok + 1)[:n_tok]
    shared = (jax.nn.silu(xf @ ws_gate) * (xf @ ws_up)) @ ws_down
    return (routed + shared).reshape(B, T, D)


def setup_inputs(seed: int = 0) -> dict:
    key = jax.random.key(seed)
    ks = jax.random.split(key, 26)
    L, D = DEPTH, D_MODEL
    f32 = jnp.float32
    nrm = lambda k, shape, s: jax.random.normal(k, shape, f32) * s
    x = nrm(ks[0], (BATCH, SEQ, D), 1.0)
    col_scale = jnp.concatenate([
        jnp.ones((2 * DIFF_QK_WIDTH,), f32), jnp.full((DIFF_WIDTH,), DEEPNORM_BETA, f32),
        jnp.ones((2 * GDN_WIDTH,), f32), jnp.full((GDN_WIDTH,), DEEPNORM_BETA, f32),
        jnp.ones((GDN_WIDTH + 2 * GDN_HEADS + 2 * D,), f32)])
    w_in = nrm(ks[1], (L, D, IN_WIDTH), D ** -0.5) * col_scale
    conv_w = nrm(ks[2], (L, CONV_WIDTH, 3 * GDN_WIDTH), CONV_WIDTH ** -0.5)
    gdn_a_log = jnp.log(jax.random.uniform(ks[3], (L, GDN_HEADS), f32, 1.0, 16.0))
    dt = jnp.exp(jax.random.uniform(ks[4], (L, GDN_HEADS), f32, math.log(1e-3), math.log(1e-1)))
    gdn_dt_bias = dt + jnp.log(-jnp.expm1(-dt))
    gdn_norm_w = 1.0 + nrm(ks[5], (L, GDN_HEAD_DIM), 0.02)
    diff_lambda = nrm(ks[6], (L, 4, DIFF_HEAD_DIM), 0.1)
    diff_subln_w = 1.0 + nrm(ks[7], (L, DIFF_V_DIM), 0.02)
    rel_bias_table = nrm(ks[8], (REL_BUCKETS, DIFF_HEADS), 0.5)
    w_branch_a = nrm(ks[9], (L, DIFF_WIDTH, D), DIFF_WIDTH ** -0.5)
    w_branch_b = nrm(ks[10], (L, GDN_WIDTH, D), GDN_WIDTH ** -0.5)
    w_out = nrm(ks[11], (L, D, D), D ** -0.5 * DEEPNORM_BETA)
    ln1_g = 1.0 + nrm(ks[12], (L, D), 0.02)
    ln1_b = nrm(ks[13], (L, D), 0.02)
    w_router = nrm(ks[14], (L, D, N_EXPERTS), D ** -0.5)
    router_bias = nrm(ks[15], (L, N_EXPERTS), 0.01)
    w_gate = nrm(ks[16], (L, N_EXPERTS, D, EXPERT_FF), D ** -0.5)
    w_up = nrm(ks[17], (L, N_EXPERTS, D, EXPERT_FF), D ** -0.5)
    w_down = nrm(ks[18], (L, N_EXPERTS, EXPERT_FF, D), EXPERT_FF ** -0.5 * DEEPNORM_BETA)
    ws_gate = nrm(ks[19], (L, D, SHARED_FF), D ** -0.5)
    ws_up = nrm(ks[20], (L, D, SHARED_FF), D ** -0.5)
    ws_down = nrm(ks[21], (L, SHARED_FF, D), SHARED_FF ** -0.5 * DEEPNORM_BETA)
    ln2_g = 1.0 + nrm(ks[22], (L, D), 0.02)
    ln2_b = nrm(ks[23], (L, D), 0.02)
    return {"x": x, "w_in": w_in, "conv_w": conv_w, "gdn_a_log": gdn_a_log, "gdn_dt_bias": gdn_dt_bias,
            "gdn_norm_w": gdn_norm_w, "diff_lambda": diff_lambda, "diff_subln_w": diff_subln_w,
            "rel_bias_table": rel_bias_table, "w_branch_a": w_branch_a, "w_branch_b": w_branch_b,
            "w_out": w_out, "ln1_g": ln1_g, "ln1_b": ln1_b, "w_router": w_router,
            "router_bias": router_bias, "w_gate": w_gate, "w_up": w_up, "w_down": w_down,
            "ws_gate": ws_gate, "ws_up": ws_up, "ws_down": ws_down, "ln2_g": ln2_g, "ln2_b": ln2_b}


def reference(x, w_in, conv_w, gdn_a_log, gdn_dt_bias, gdn_norm_w, diff_lambda, diff_subln_w,
              rel_bias_table, w_branch_a, w_branch_b, w_out, ln1_g, ln1_b, w_router, router_bias,
              w_gate, w_up, w_down, ws_gate, ws_up, ws_down, ln2_g, ln2_b):
    for l in range(DEPTH):
        lambda_init = 0.8 - 0.6 * math.exp(-0.3 * l)
        h = hybrid_mixer(x, w_in[l], conv_w[l], gdn_a_log[l], gdn_dt_bias[l], gdn_norm_w[l],
                         diff_lambda[l], diff_subln_w[l], rel_bias_table, w_branch_a[l],
                         w_branch_b[l], w_out[l], lambda_init)
        x = layer_norm(DEEPNORM_ALPHA * x + h, ln1_g[l], ln1_b[l])
        h = moe_ffn(x, w_router[l], router_bias[l], w_gate[l], w_up[l], w_down[l],
                    ws_gate[l], ws_up[l], ws_down[l])
        x = layer_norm(DEEPNORM_ALPHA * x + h, ln2_g[l], ln2_b[l])
    return x
```

```python
import numpy as np
import ml_dtypes
import concourse.bass as bass
import concourse.mybir as mybir
from concourse.bass_utils import run_bass_kernel_spmd

F32 = mybir.dt.float32
BF16 = mybir.dt.bfloat16
I32 = mybir.dt.int32
AF = mybir.ActivationFunctionType
ALU = mybir.AluOpType
AX = mybir.AxisListType

T = 8192
D = 2048
NCORE = 8
ENGS = ["pe", "act", "dve", "pool", "sp"]


class Buf:
    __slots__ = ("name", "writes", "reads", "sem", "cnt", "excl")

    def __init__(self, name, excl=False):
        self.name = name
        self.excl = excl
        self.writes = []
        self.reads = []
        self.sem = None
        self.cnt = 0


class Sched:
    def __init__(self, nc):
        self.nc = nc
        self.prog = {e: [] for e in ENGS}
        self.sem = {e: nc.alloc_semaphore(name=f"s_{e}") for e in ENGS if e != "sp"}
        self.cnt = {e: 0 for e in ENGS}
        self.seen = {e: {} for e in ENGS}
        self.n_sems = 4

    def _waits(self, eng, reads, writes, same_gen=False):
        toks = []
        for b in reads:
            toks.extend(b.writes)
        for b in writes:
            toks.extend(b.reads)
            if not same_gen:
                toks.extend(b.writes)
        need = {}
        for (sem, val, src) in toks:
            if src == "pe" and eng == "pe":
                continue
            k = id(sem)
            if self.seen[eng].get(k, 0) >= val:
                continue
            if k not in need or need[k][1] < val:
                need[k] = (sem, val)
        out = []
        for k, (sem, val) in need.items():
            self.seen[eng][k] = val
            out.append((sem, val))
        return out

    def _commit(self, tok, reads, writes, same_gen=False):
        for b in reads:
            b.reads.append(tok)
            if len(b.reads) > 64:
                b.reads = b.reads[-64:] if False else b.reads
        for b in writes:
            if same_gen:
                b.writes.append(tok)
            else:
                b.writes = [tok]
                b.reads = []

    def op(self, eng, fn, reads=(), writes=()):
        self.nops = getattr(self, "nops", 0) + 1
        if self.nops > getattr(self, "max_ops", 10 ** 9):
            return None
        ex = [b for b in reads if b.excl]
        if ex:
            reads = [b for b in reads if not b.excl]
            writes = list(writes) + ex
        waits = self._waits(eng, reads, writes)
        self.cnt[eng] += 1
        tok = (self.sem[eng], self.cnt[eng], eng)
        self.prog[eng].append((waits, fn, (self.sem[eng], 1)))
        self._commit(tok, reads, writes)
        return tok

    def dma(self, q, fn, reads=(), writes=(), sem_buf=None, same_gen=False, inc=16):
        self.nops = getattr(self, "nops", 0) + 1
        if self.nops > getattr(self, "max_ops", 10 ** 9):
            return None
        waits = self._waits(q, reads, writes, same_gen=same_gen)
        if sem_buf.sem is None:
            sem_buf.sem = self.nc.alloc_semaphore(name=f"d_{sem_buf.name}")
            self.n_sems += 1
        sem_buf.cnt += inc
        tok = (sem_buf.sem, sem_buf.cnt, "dma")
        self.prog[q].append((waits, fn, (sem_buf.sem, inc)))
        self._commit(tok, reads, writes, same_gen=same_gen)
        return tok

    def wait_final(self, eng, bufs):
        need = {}
        for b in bufs:
            for (sem, val, src) in list(b.writes) + list(b.reads):
                k = id(sem)
                if k not in need or need[k][1] < val:
                    need[k] = (sem, val)
        self.prog[eng].append((list(need.values()), None, None))

    def emit(self):
        nc = self.nc
        handles = {"pe": "tensor", "act": "scalar", "dve": "vector", "pool": "gpsimd", "sp": "sync"}
        with nc.Block() as block:
            for e in ENGS:
                prog = self.prog[e]
                if not prog:
                    continue

                def body(eng, prog=prog):
                    for waits, fn, inc in prog:
                        for (sem, val) in waits:
                            eng.wait_ge(sem, val)
                        if fn is not None:
                            fn(eng).then_inc(inc[0], inc[1])

                getattr(block, handles[e])(body)


class Ctx:
    def __init__(self, nc, S):
        self.nc = nc
        self.S = S
        self.n = 0

    def sb(self, shape, dt, name=None):
        self.n += 1
        name = name or f"t{self.n}"
        t = self.nc.alloc_sbuf_tensor("sb_" + name, list(shape), dt)
        return t, Buf(name)


NT_A = 16
GDN_SQ = 6


def build_phase_a(nc, S, dr, n_tiles=NT_A, do_gdn=True, do_attn=True):
    C = Ctx(nc, S)
    op = S.op

    xT = dr["xT"].rearrange("(c p) t -> p c t", p=128)
    W, bW = C.sb([128, 16, 898], BF16, "W")
    wa_v = dr["wA"].rearrange("(c p) n -> p c n", p=128)
    for c4 in range(4):
        b = Buf(f"Wl{c4}")
        S.dma("pool", lambda e, c4=c4: e.dma_start(out=W[:, c4 * 4:(c4 + 1) * 4, :], in_=wa_v[:, c4 * 4:(c4 + 1) * 4, :]),
              writes=[bW], sem_buf=bW, same_gen=True)
    misc, bmisc = C.sb([128, 8], F32, "misc")
    S.dma("sp", lambda e: e.dma_start(out=misc[:], in_=dr["miscA"]), writes=[bmisc], sem_buf=bmisc)
    cw, bcw = C.sb([128, 3, 4], F32, "cw")
    S.dma("sp", lambda e: e.dma_start(out=cw[:], in_=dr["convw"]), writes=[bcw], sem_buf=bcw)
    nrm, bnrm = C.sb([128, 2, 128], F32, "nrm")
    S.dma("sp", lambda e: e.dma_start(out=nrm[:], in_=dr["nrmw"]), writes=[bnrm], sem_buf=bnrm)
    lamt, blamt = C.sb([128, 2, 2, 64], F32, "lamt")
    S.dma("sp", lambda e: e.dma_start(out=lamt[:], in_=dr["lam"]), writes=[blamt], sem_buf=blamt)
    BT, bBT = C.sb([128, 2, 128], F32, "BT")
    S.dma("sp", lambda e: e.dma_start(out=BT[:], in_=dr["biasT"]), writes=[bBT], sem_buf=bBT)

    onesf, bones = C.sb([128, 128], F32, "onesf")
    op("pool", lambda e: e.memset(onesf[:], 1.0), writes=[bones])
    identf, bident = C.sb([128, 128], F32, "identf")
    op("pool", lambda e: e.affine_select(out=identf[:], in_=onesf[:], pattern=[[-1, 128]], compare_op=ALU.is_equal,
                                         fill=0.0, base=0, channel_multiplier=1), reads=[bones], writes=[bident])
    triu, btriu = C.sb([128, 128], F32, "triu")
    op("pool", lambda e: e.affine_select(out=triu[:], in_=onesf[:], pattern=[[1, 128]], compare_op=ALU.is_ge,
                                         fill=0.0, base=0, channel_multiplier=-1), reads=[bones], writes=[btriu])
    identb, bidentb = C.sb([128, 128], BF16, "identb")
    op("dve", lambda e: e.tensor_copy(out=identb[:], in_=identf[:]), reads=[bident], writes=[bidentb])

    epsc, bepsc = C.sb([128, 1], F32, "epsc")
    op("pool", lambda e: e.memset(epsc[:], 1e-6), writes=[bepsc])
    negA, bnegA = C.sb([128, 1], F32, "negA")
    op("act", lambda e: e.activation(out=negA[:], in_=misc[:, 0:1], func=AF.Exp), reads=[bmisc], writes=[bnegA])
    op("dve", lambda e: e.tensor_scalar(out=negA[:], in0=negA[:], scalar1=-1.0, scalar2=None, op0=ALU.mult),
       reads=[bnegA], writes=[bnegA])
    lprod, blprod = C.sb([128, 2, 64], F32, "lprod")
    op("dve", lambda e: e.tensor_tensor(out=lprod[:], in0=lamt[:, :, 0, :], in1=lamt[:, :, 1, :], op=ALU.mult),
       reads=[blamt], writes=[blprod])
    lsum, blsum = C.sb([128, 2], F32, "lsum")
    op("dve", lambda e: e.tensor_reduce(out=lsum[:], in_=lprod[:], axis=AX.X, op=ALU.add), reads=[blprod], writes=[blsum])
    op("act", lambda e: e.activation(out=lsum[:], in_=lsum[:], func=AF.Exp), reads=[blsum], writes=[blsum])
    lam, blam = C.sb([128, 1], F32, "lam")
    op("dve", lambda e: e.scalar_tensor_tensor(out=lam[:], in0=lsum[:, 0:1], scalar=0.2, in1=lsum[:, 1:2],
                                               op0=ALU.add, op1=ALU.subtract), reads=[blsum], writes=[blam])
    op("dve", lambda e: e.tensor_scalar(out=BT[:], in0=BT[:], scalar1=misc[:, 2:3], scalar2=None, op0=ALU.subtract),
       reads=[bBT, bmisc], writes=[bBT])
    op("dve", lambda e: e.memset(BT[64:128, 0, 0:64], -30000.0), writes=[bBT])
    op("dve", lambda e: e.tensor_scalar(out=nrm[:, 1, :], in0=nrm[:, 1, :], scalar1=0.8, scalar2=None, op0=ALU.mult),
       reads=[bnrm], writes=[bnrm])

    KT, _ = C.sb([128, T], BF16, "KT")
    bKT = [Buf(f"KT{j}") for j in range(NT_A)]
    VA, _ = C.sb([128, 64, 130], BF16, "VA")
    bVA = [Buf(f"VA{j}") for j in range(64)]
    op("pool", lambda e: e.memset(VA[:, :, 128:130], 1.0), writes=bVA)
    QT = [C.sb([128, 512], BF16, f"QT{i}") for i in range(2)]
    xt = [C.sb([128, 16, 512], BF16, f"xt{i}") for i in range(2)]
    cin, bcin = C.sb([128, 3, 515], F32, "cin")
    op("pool", lambda e: e.memset(cin[:, :, 0:3], 0.0), writes=[bcin])
    cout, bcout = C.sb([128, 3, 512], F32, "cout")
    sqt, bsqt = C.sb([128, 512], F32, "sqt")
    qkn = [C.sb([128, 512], F32, f"qkn{i}") for i in range(2)]
    tm, btm = C.sb([128, 4, 130], F32, "tm")
    sc = {nm: C.sb([128, 4], F32, "sc_" + nm) for nm in
          ["x", "ax", "e", "l", "g", "beta", "gc", "gtot", "eg", "nbeg", "ekd", "glast", "nbeta", "tmp", "ss4", "rr4"]}
    Sst = [C.sb([128, 128], F32, f"Sst{i}") for i in range(2)]
    op("pool", lambda e: e.memset(Sst[0][0][:], 0.0), writes=[Sst[0][1]])
    ystage = [C.sb([128, 512], BF16, f"ystg{i}") for i in range(2)]

    pb = [nc.alloc_psum_tensor(f"pb{i}", [128, 512], F32) for i in range(8)]
    pq = [[Buf(f"pb{i}q{k}", excl=True) for k in range(4)] for i in range(8)]

    def preg(bank, c0, n):
        return pb[bank][:, c0:c0 + n], [pq[bank][0]]

    def mk(name, shape=(128, 128), dt=F32):
        return C.sb(list(shape), dt, name)
    G4 = {nm: mk("g4_" + nm, (128, 4, 128)) for nm in ["dg", "t1", "E1", "E2", "Erow", "M", "MT", "X", "XT", "AqkT", "kdec",
                                                       "ATm", "qd", "QeffT", "zs", "yb", "junk"]}
    G4["R"] = mk("g4_R", (128, 4, 256))
    nonesf, bnones = C.sb([128, 128], F32, "nonesf")
    op("pool", lambda e: e.memset(nonesf[:], -1.0), writes=[bnones])
    nrm4, bnrm4 = C.sb([128, 4, 128], F32, "nrm4")
    for s4 in range(4):
        op("pool", lambda e, s4=s4: e.tensor_copy(out=nrm4[:, s4, :], in_=nrm[:, 0, :]), reads=[bnrm], writes=[bnrm4])
    rr_t = {nm: C.sb([128, 1], F32, "rr_" + nm) for nm in ["ss", "rr", "r1", "r2", "ss2", "rr2"]}
    PT = [C.sb([128, 512], BF16, f"PT{i}") for i in range(4)]
    at_t2, bat_t2 = C.sb([128, 128], F32, "at_t2")
    at_a, bat_a = C.sb([128, 128], F32, "at_a")
    at_y, bat_y = C.sb([128, 128], F32, "at_y")
    at_junk, bat_junk = C.sb([128, 128], F32, "at_junk")

    b_out = Buf("outA")
    pt_ctr = [0]

    for j in range(n_tiles):
        t0 = j * 512
        xtt, bxt = xt[j % 2]
        for half in range(2):
            S.dma("pool", lambda e, xtt=xtt, half=half, t0=t0: e.dma_start(
                out=xtt[:, half * 8:(half + 1) * 8, :], in_=xT[:, half * 8:(half + 1) * 8, t0:t0 + 512]),
                writes=[bxt], sem_buf=bxt, same_gen=(half == 1))
        QTc, bQTc = QT[j % 2]
        for g in range(5):
            bank = g % 2
            ps, pbufs = preg(bank, 0, 512)
            for c in range(16):
                op("pe", lambda e, ps=ps, g=g, c=c, xtt=xtt: e.matmul(ps, lhsT=W[:, c, g * 128:(g + 1) * 128],
                                                                      rhs=xtt[:, c, :], start=(c == 0), stop=(c == 15)),
                   reads=[bW, bxt], writes=pbufs)
            if g == 0:
                op("act", lambda e, ps=ps, QTc=QTc: e.activation(out=QTc[:], in_=ps, func=AF.Copy, scale=0.125),
                   reads=pbufs, writes=[bQTc])
            elif g == 1:
                op("act", lambda e, ps=ps, t0=t0: e.activation(out=KT[:, t0:t0 + 512], in_=ps, func=AF.Copy),
                   reads=pbufs, writes=[bKT[j]])
            else:
                op("act", lambda e, ps=ps, g=g: e.activation(out=cin[:, g - 2, 3:515], in_=ps, func=AF.Copy),
                   reads=pbufs, writes=[bcin])
        for s in range(4):
            ps, pbufs = preg(2, 0, 258)
            for c in range(16):
                op("pe", lambda e, ps=ps, c=c, s=s, xtt=xtt: e.matmul(ps, lhsT=xtt[:, c, s * 128:(s + 1) * 128],
                                                                      rhs=W[:, c, 640:898], start=(c == 0), stop=(c == 15)),
                   reads=[bW, bxt], writes=pbufs)
            op("act", lambda e, s=s, j=j: e.activation(out=VA[:, 4 * j + s, 0:128], in_=pb[2][:, 0:128], func=AF.Copy),
               reads=pbufs, writes=[bVA[4 * j + s]])
            op("dve", lambda e, s=s: e.tensor_copy(out=tm[:, s, :], in_=pb[2][:, 128:258]), reads=pbufs, writes=[btm])

        if do_gdn:
            for i in range(3):
                op("dve", lambda e, i=i: e.tensor_scalar(out=cout[:, i, :], in0=cin[:, i, 0:512], scalar1=cw[:, i, 0:1],
                                                         scalar2=None, op0=ALU.mult), reads=[bcin, bcw], writes=[bcout])
                for jj in range(1, 4):
                    op("dve", lambda e, i=i, jj=jj: e.scalar_tensor_tensor(
                        out=cout[:, i, :], in0=cin[:, i, jj:jj + 512], scalar=cw[:, i, jj:jj + 1], in1=cout[:, i, :],
                        op0=ALU.mult, op1=ALU.add), reads=[bcin, bcw, bcout], writes=[bcout])
            op("dve", lambda e: e.tensor_copy(out=cin[:, :, 0:3], in_=cin[:, :, 512:515]), reads=[bcin], writes=[bcin])
            op("act", lambda e: e.activation(out=cout[:], in_=cout[:], func=AF.Silu), reads=[bcout], writes=[bcout])
            for i in range(2):
                qk, bqk = qkn[i]
                op("act", lambda e, i=i: e.activation(out=sqt[:], in_=cout[:, i, :], func=AF.Square),
                   reads=[bcout], writes=[bsqt])
                ps, pbufs = preg(i, 0, 512)
                op("pe", lambda e, ps=ps: e.matmul(ps, lhsT=onesf[:], rhs=sqt[:], start=True, stop=True),
                   reads=[bones, bsqt], writes=pbufs)
                op("act", lambda e, ps=ps: e.activation(out=sqt[:], in_=ps, func=AF.Ln, bias=epsc[:, 0:1]), reads=pbufs + [bepsc], writes=[bsqt])
                op("act", lambda e: e.activation(out=sqt[:], in_=sqt[:], func=AF.Exp, scale=-0.5), reads=[bsqt], writes=[bsqt])
                scl = (128.0 ** -0.5) if i == 0 else 1.0
                op("dve", lambda e, i=i, qk=qk, scl=scl: e.scalar_tensor_tensor(
                    out=qk[:], in0=cout[:, i, :], scalar=scl, in1=sqt[:], op0=ALU.mult, op1=ALU.mult),
                    reads=[bcout, bsqt], writes=[bqk])
            def sct(nm):
                return sc[nm][0], sc[nm][1]
            x_, bx_ = sct("x"); ax_, bax_ = sct("ax"); e_, be_ = sct("e"); l_, bl_ = sct("l"); g_, bg_ = sct("g")
            beta_, bbeta_ = sct("beta"); gc_, bgc_ = sct("gc"); gtot_, bgtot_ = sct("gtot"); eg_, beg_ = sct("eg")
            nbeg_, bnbeg_ = sct("nbeg"); ekd_, bekd_ = sct("ekd"); glast_, bglast_ = sct("glast")
            nbeta_, bnbeta_ = sct("nbeta"); tmp_, btmp_ = sct("tmp")
            op("dve", lambda e: e.tensor_scalar(out=x_[:], in0=tm[:, :, 128], scalar1=misc[:, 1:2], scalar2=None,
                                                op0=ALU.add), reads=[btm, bmisc], writes=[bx_])
            op("dve", lambda e: e.scalar_tensor_tensor(out=ax_[:], in0=x_[:], scalar=-1.0, in1=x_[:], op0=ALU.mult,
                                                       op1=ALU.max), reads=[bx_], writes=[bax_])
            op("act", lambda e: e.activation(out=e_[:], in_=ax_[:], func=AF.Exp, scale=-1.0), reads=[bax_], writes=[be_])
            op("act", lambda e: e.activation(out=l_[:], in_=e_[:], func=AF.Ln, bias=onesf[:, 0:1]), reads=[be_, bones], writes=[bl_])
            op("dve", lambda e: e.scalar_tensor_tensor(out=g_[:], in0=x_[:], scalar=0.0, in1=l_[:], op0=ALU.max,
                                                       op1=ALU.add), reads=[bx_, bl_], writes=[bg_])
            op("dve", lambda e: e.tensor_scalar(out=g_[:], in0=g_[:], scalar1=negA[:, 0:1], scalar2=None, op0=ALU.mult),
               reads=[bg_, bnegA], writes=[bg_])
            op("act", lambda e: e.activation(out=beta_[:], in_=tm[:, :, 129], func=AF.Sigmoid), reads=[btm], writes=[bbeta_])
            psg, pgb = preg(6, 384, 8)
            op("pe", lambda e: e.matmul(psg[:, 0:4], lhsT=triu[:], rhs=g_[:], start=True, stop=True),
               reads=[btriu, bg_], writes=pgb)
            op("pe", lambda e: e.matmul(psg[:, 4:8], lhsT=onesf[:], rhs=g_[:], start=True, stop=True),
               reads=[bones, bg_], writes=pgb)
            op("dve", lambda e: e.tensor_copy(out=gc_[:], in_=psg[:, 0:4]), reads=pgb, writes=[bgc_])
            op("dve", lambda e: e.tensor_copy(out=gtot_[:], in_=psg[:, 4:8]), reads=pgb, writes=[bgtot_])
            op("act", lambda e: e.activation(out=eg_[:], in_=gc_[:], func=AF.Exp), reads=[bgc_], writes=[beg_])
            op("act", lambda e: e.activation(out=glast_[:], in_=gtot_[:], func=AF.Exp), reads=[bgtot_], writes=[bglast_])
            op("dve", lambda e: e.tensor_tensor(out=tmp_[:], in0=gtot_[:], in1=gc_[:], op=ALU.subtract),
               reads=[bgtot_, bgc_], writes=[btmp_])
            op("act", lambda e: e.activation(out=ekd_[:], in_=tmp_[:], func=AF.Exp), reads=[btmp_], writes=[bekd_])
            op("dve", lambda e: e.tensor_scalar(out=nbeta_[:], in0=beta_[:], scalar1=-1.0, scalar2=None, op0=ALU.mult),
               reads=[bbeta_], writes=[bnbeta_])
            op("dve", lambda e: e.tensor_tensor(out=nbeg_[:], in0=nbeta_[:], in1=eg_[:], op=ALU.mult),
               reads=[bnbeta_, beg_], writes=[bnbeg_])

            qTn, bqTn = qkn[0]
            kTn, bkTn = qkn[1]

            def g4(nm):
                return G4[nm][0], G4[nm][1]
            dg, bdg = g4("dg"); t1, bt1 = g4("t1"); E1, bE1 = g4("E1"); E2, bE2 = g4("E2"); Erow, bErow = g4("Erow")
            M, bM = g4("M"); MT, bMT = g4("MT"); X, bX = g4("X"); XT, bXT = g4("XT"); AqkT, bAqkT = g4("AqkT")
            kdec, bkdec = g4("kdec"); ATm, bATm = g4("ATm"); qd, bqd = g4("qd"); QeffT, bQeffT = g4("QeffT")
            zs, bzs = g4("zs"); yb, byb = g4("yb"); junk, bjunk = g4("junk")
            R_, bR = G4["R"]

            def v4(bank):
                return pb[bank][:].rearrange("p (s c) -> p s c", s=4)
            for s in range(4):
                op("dve", lambda e, s=s: e.tensor_scalar(out=dg[:, s, :], in0=identf[:], scalar1=gc_[:, s:s + 1], scalar2=None,
                                                         op0=ALU.mult), reads=[bident, bgc_], writes=[bdg])
            for s in range(4):
                op("pe", lambda e, s=s: e.matmul(pb[3][:, s * 128:(s + 1) * 128], lhsT=dg[:, s, :], rhs=onesf[:],
                                                 start=(s == 0), stop=False, skip_group_check=True),
                   reads=[bdg, bones], writes=[pq[3][0]])
            for s in range(4):
                op("pe", lambda e, s=s: e.matmul(pb[3][:, s * 128:(s + 1) * 128], lhsT=nonesf[:], rhs=dg[:, s, :],
                                                 start=False, stop=(s == 3), skip_group_check=True),
                   reads=[bdg, bnones], writes=[pq[3][0]])
            for s in range(4):
                op("pe", lambda e, s=s: e.matmul(pb[4][:, s * 128:(s + 1) * 128], lhsT=onesf[:], rhs=dg[:, s, :],
                                                 start=(s == 0), stop=(s == 3), skip_group_check=True),
                   reads=[bdg, bones], writes=[pq[4][0]])
            op("dve", lambda e: e.tensor_scalar(out=t1[:], in0=v4(3), scalar1=0.0, scalar2=None, op0=ALU.min),
               reads=[pq[3][0]], writes=[bt1])
            op("act", lambda e: e.activation(out=E1[:], in_=t1[:], func=AF.Exp), reads=[bt1], writes=[bE1])
            op("pool", lambda e: e.affine_select(out=E1[:], in_=E1[:], pattern=[[0, 4], [-1, 128]], compare_op=ALU.is_gt,
                                                 fill=0.0, base=0, channel_multiplier=1), reads=[bE1], writes=[bE1])
            op("dve", lambda e: e.tensor_scalar(out=t1[:], in0=v4(3), scalar1=0.0, scalar2=None, op0=ALU.max),
               reads=[pq[3][0]], writes=[bt1])
            op("act", lambda e: e.activation(out=E2[:], in_=t1[:], func=AF.Exp, scale=-1.0), reads=[bt1], writes=[bE2])
            op("pool", lambda e: e.affine_select(out=E2[:], in_=E2[:], pattern=[[0, 4], [1, 128]], compare_op=ALU.is_ge,
                                                 fill=0.0, base=0, channel_multiplier=-1), reads=[bE2], writes=[bE2])
            op("act", lambda e: e.activation(out=Erow[:], in_=v4(4), func=AF.Exp), reads=[pq[4][0]], writes=[bErow])
            for s in range(4):
                cs = slice(s * 128, (s + 1) * 128)
                op("pe", lambda e, s=s, cs=cs: e.matmul(pb[5][:, cs], lhsT=kTn[:, cs], rhs=kTn[:, cs], start=(s == 0),
                                                        stop=(s == 3), skip_group_check=True), reads=[bkTn], writes=[pq[5][0]])
            for s in range(4):
                cs = slice(s * 128, (s + 1) * 128)
                op("pe", lambda e, s=s, cs=cs: e.matmul(pb[6][:, cs], lhsT=kTn[:, cs], rhs=qTn[:, cs], start=(s == 0),
                                                        stop=(s == 3), skip_group_check=True), reads=[bkTn, bqTn], writes=[pq[6][0]])
            for s in range(4):
                op("dve", lambda e, s=s: e.scalar_tensor_tensor(out=M[:, s, :], in0=pb[5][:, s * 128:(s + 1) * 128],
                                                                scalar=nbeta_[:, s:s + 1], in1=E1[:, s, :], op0=ALU.mult,
                                                                op1=ALU.mult), reads=[pq[5][0], bnbeta_, bE1], writes=[bM])
            op("dve", lambda e: e.tensor_tensor(out=AqkT[:], in0=v4(6), in1=E2[:], op=ALU.mult), reads=[pq[6][0], bE2], writes=[bAqkT])
            for s in range(4):
                op("pe", lambda e, s=s: e.transpose(pb[7][:, s * 128:(s + 1) * 128], M[:, s, :], identf[:]),
                   reads=[bM, bident], writes=[pq[7][0]])
            op("act", lambda e: e.activation(out=MT[:], in_=v4(7), func=AF.Copy), reads=[pq[7][0]], writes=[bMT])
            for s in range(4):
                cs = slice(s * 128, (s + 1) * 128)
                op("pe", lambda e, cs=cs: e.transpose(pb[3][:, cs], kTn[:, cs], identf[:]), reads=[bkTn, bident], writes=[pq[3][0]])
            for s in range(4):
                cs = slice(s * 128, (s + 1) * 128)
                op("pe", lambda e, cs=cs: e.transpose(pb[4][:, cs], cout[:, 2, cs], identf[:]), reads=[bcout, bident], writes=[pq[4][0]])
            for s in range(4):
                cs = slice(s * 128, (s + 1) * 128)
                op("dve", lambda e, s=s, cs=cs: e.tensor_scalar(out=kdec[:, s, :], in0=pb[3][:, cs], scalar1=ekd_[:, s:s + 1],
                                                                scalar2=None, op0=ALU.mult), reads=[pq[3][0], bekd_], writes=[bkdec])
                op("dve", lambda e, s=s, cs=cs: e.tensor_scalar(out=R_[:, s, 128:256], in0=pb[3][:, cs], scalar1=nbeg_[:, s:s + 1],
                                                                scalar2=None, op0=ALU.mult), reads=[pq[3][0], bnbeg_], writes=[bR])
                op("dve", lambda e, s=s, cs=cs: e.tensor_scalar(out=R_[:, s, 0:128], in0=pb[4][:, cs], scalar1=beta_[:, s:s + 1],
                                                                scalar2=None, op0=ALU.mult), reads=[pq[4][0], bbeta_], writes=[bR])
            Xc, bXc, XTc, bXTc = M, bM, MT, bMT
            for it in range(GDN_SQ + 1):
                for s in range(4):
                    bank = 5 + s // 2
                    op("pe", lambda e, s=s, bank=bank, XTc=XTc: e.matmul(
                        pb[bank][:, (s % 2) * 256:(s % 2 + 1) * 256], lhsT=XTc[:, s, :], rhs=R_[:, s, :],
                        start=(s % 2 == 0), stop=(s % 2 == 1), skip_group_check=True), reads=[bXTc, bR], writes=[pq[bank][0]])
                for hb_ in range(2):
                    op("dve", lambda e, hb_=hb_: e.tensor_tensor(
                        out=R_[:, 2 * hb_:2 * hb_ + 2, :], in0=R_[:, 2 * hb_:2 * hb_ + 2, :],
                        in1=pb[5 + hb_][:].rearrange("p (s c) -> p s c", s=2), op=ALU.add),
                        reads=[bR, pq[5 + hb_][0]], writes=[bR])
                if it < GDN_SQ:
                    for s in range(4):
                        op("pe", lambda e, s=s, Xc=Xc, XTc=XTc: e.matmul(
                            pb[7][:, s * 128:(s + 1) * 128], lhsT=XTc[:, s, :], rhs=Xc[:, s, :], start=(s == 0), stop=(s == 3),
                            skip_group_check=True), reads=[bXc, bXTc], writes=[pq[7][0]])
                    for s in range(4):
                        op("pe", lambda e, s=s, Xc=Xc, XTc=XTc: e.matmul(
                            pb[3][:, s * 128:(s + 1) * 128], lhsT=Xc[:, s, :], rhs=XTc[:, s, :], start=(s == 0), stop=(s == 3),
                            skip_group_check=True), reads=[bXc, bXTc], writes=[pq[3][0]])
                    if it % 2 == 0:
                        Xn, bXn, XTn, bXTn = X, bX, XT, bXT
                    else:
                        Xn, bXn, XTn, bXTn = M, bM, MT, bMT
                    op("act", lambda e, Xn=Xn: e.activation(out=Xn[:], in_=v4(7), func=AF.Copy), reads=[pq[7][0]], writes=[bXn])
                    op("dve", lambda e, XTn=XTn: e.tensor_copy(out=XTn[:], in_=v4(3)), reads=[pq[3][0]], writes=[bXTn])
                    Xc, bXc, XTc, bXTc = Xn, bXn, XTn, bXTn
            for s in range(4):
                op("pe", lambda e, s=s: e.matmul(pb[4][:, s * 128:(s + 1) * 128], lhsT=R_[:, s, 128:256], rhs=kdec[:, s, :],
                                                 start=(s == 0), stop=(s == 3), skip_group_check=True),
                   reads=[bR, bkdec], writes=[pq[4][0]])
            for s in range(4):
                op("pe", lambda e, s=s: e.matmul(pb[7][:, s * 128:(s + 1) * 128], lhsT=R_[:, s, 128:256], rhs=AqkT[:, s, :],
                                                 start=(s == 0), stop=(s == 3), skip_group_check=True),
                   reads=[bR, bAqkT], writes=[pq[7][0]])
            for s in range(4):
                op("dve", lambda e, s=s: e.scalar_tensor_tensor(out=ATm[:, s, :], in0=identf[:], scalar=glast_[:, s:s + 1],
                                                                in1=pb[4][:, s * 128:(s + 1) * 128], op0=ALU.mult, op1=ALU.add),
                   reads=[bident, bglast_, pq[4][0]], writes=[bATm])
            op("pool", lambda e: e.tensor_tensor(out=qd[:], in0=qTn[:].rearrange("p (s c) -> p s c", s=4), in1=Erow[:],
                                                 op=ALU.mult), reads=[bqTn, bErow], writes=[bqd])
            op("dve", lambda e: e.tensor_tensor(out=QeffT[:], in0=qd[:], in1=v4(7), op=ALU.add), reads=[bqd, pq[7][0]], writes=[bQeffT])
            op("act", lambda e: e.activation(out=zs[:], in_=tm[:, :, 0:128], func=AF.Silu), reads=[btm], writes=[bzs])
            op("pool", lambda e: e.tensor_tensor(out=zs[:], in0=zs[:], in1=nrm4[:], op=ALU.mult), reads=[bzs, bnrm4], writes=[bzs])
            ss, bss = sc["ss4"]
            for s in range(4):
                n = 4 * j + s
                Scur, bScur = Sst[n % 2]
                Snxt, bSnxt = Sst[(n + 1) % 2]
                op("pe", lambda e, s=s, Scur=Scur: e.matmul(pb[5][:, s * 128:(s + 1) * 128], lhsT=QeffT[:, s, :], rhs=Scur[:],
                                                            start=(s == 0), stop=False, skip_group_check=True),
                   reads=[bQeffT, bScur], writes=[pq[5][0]])
                op("pe", lambda e, s=s: e.matmul(pb[5][:, s * 128:(s + 1) * 128], lhsT=AqkT[:, s, :], rhs=R_[:, s, 0:128],
                                                 start=False, stop=True, skip_group_check=True),
                   reads=[bAqkT, bR], writes=[pq[5][0]])
                op("pe", lambda e, s=s: e.matmul(pb[6][:, 0:128], lhsT=kdec[:, s, :], rhs=R_[:, s, 0:128], start=True, stop=False),
                   reads=[bkdec, bR], writes=[pq[6][0]])
                op("pe", lambda e, s=s, Scur=Scur: e.matmul(pb[6][:, 0:128], lhsT=ATm[:, s, :], rhs=Scur[:], start=False, stop=True),
                   reads=[bATm, bScur], writes=[pq[6][0]])
                op("act", lambda e, Snxt=Snxt: e.activation(out=Snxt[:], in_=pb[6][:, 0:128], func=AF.Copy),
                   reads=[pq[6][0]], writes=[bSnxt])
            for s in range(4):
                op("act", lambda e, s=s: e.activation(out=junk[:, 0, :], in_=pb[5][:, s * 128:(s + 1) * 128], func=AF.Square,
                                                      accum_out=ss[:, s:s + 1]), reads=[pq[5][0]], writes=[bjunk, bss])
            rr, brr = sc["rr4"]
            op("dve", lambda e: e.tensor_scalar(out=rr[:], in0=ss[:], scalar1=1.0 / 128, scalar2=1e-6, op0=ALU.mult, op1=ALU.add),
               reads=[bss], writes=[brr])
            op("act", lambda e: e.activation(out=rr[:], in_=rr[:], func=AF.Ln), reads=[brr], writes=[brr])
            op("act", lambda e: e.activation(out=rr[:], in_=rr[:], func=AF.Exp, scale=-0.5), reads=[brr], writes=[brr])
            for s in range(4):
                op("dve", lambda e, s=s: e.scalar_tensor_tensor(out=yb[:, s, :], in0=pb[5][:, s * 128:(s + 1) * 128],
                                                                scalar=rr[:, s:s + 1], in1=zs[:, s, :], op0=ALU.mult, op1=ALU.mult),
                   reads=[pq[5][0], brr, bzs], writes=[byb])
            for s in range(4):
                op("pe", lambda e, s=s: e.transpose(pb[3][:, s * 128:(s + 1) * 128], yb[:, s, :], identf[:]),
                   reads=[byb, bident], writes=[pq[3][0]])
            op("act", lambda e: e.activation(out=ystage[1][0][:], in_=pb[3][:], func=AF.Copy), reads=[pq[3][0]], writes=[ystage[1][1]])
            S.dma("sp", lambda e, t0=t0: e.dma_start(out=dr["ybT"][:, t0:t0 + 512], in_=ystage[1][0][:]),
                  reads=[ystage[1][1]], writes=[b_out], sem_buf=ystage[1][1], same_gen=True)

        if do_attn:
            oacc = {}
            slots = [(4, 0), (4, 129), (4, 258), (5, 0), (5, 129), (5, 258), (6, 0), (6, 129)]
            for m in range(2):
                for s in range(4):
                    oacc[(m, s)] = slots[m * 4 + s]
            n_k = 4 * j + 4
            it = 0
            for i in range(n_k):
                s0 = max(0, i - 4 * j)
                q0 = s0 * 128
                for m in range(2):
                    bank = 7 if (it % 2 == 0) else 3
                    it += 1
                    pst, bpst = preg(bank, q0, 512 - q0)
                    ms = slice(m * 64, (m + 1) * 64)
                    near = [s for s in range(s0, 4) if (4 * j + s) - i <= 1]
                    op("pe", lambda e, pst=pst, ms=ms, i=i, q0=q0, QTc=QTc, near=near: e.matmul(
                        pst, lhsT=KT[ms, i * 128:(i + 1) * 128], rhs=QTc[ms, q0:512], start=True, stop=(len(near) == 0),
                        skip_group_check=True),
                        reads=[bKT[i // 4], bQTc], writes=bpst)
                    for idx, s in enumerate(near):
                        typ = 0 if (4 * j + s) == i else 1
                        op("pe", lambda e, bank=bank, s=s, typ=typ, last=(idx == len(near) - 1): e.matmul(
                            pb[bank][:, s * 128:(s + 1) * 128], lhsT=identf[:], rhs=BT[:, typ, :], start=False, stop=last,
                            skip_group_check=True), reads=[bident, bBT], writes=bpst)
                    ptt, bptt = PT[pt_ctr[0] % 4]
                    pt_ctr[0] += 1
                    op("act", lambda e, pst=pst, ptt=ptt, q0=q0: e.activation(out=ptt[:, q0:512], in_=pst, func=AF.Exp,
                                                                              bias=misc[:, 2:3]),
                       reads=bpst + [bmisc], writes=[bptt])
                    for s in range(s0, 4):
                        bk, c0 = oacc[(m, s)]
                        po, bpo = preg(bk, c0, 129)
                        last_k = 4 * j + s
                        op("pe", lambda e, po=po, ptt=ptt, s=s, i=i, last_k=last_k, c0=c0: e.matmul(
                            po, lhsT=ptt[:, s * 128:(s + 1) * 128], rhs=VA[:, i, 0:129], start=(i == 0 and c0 == 0), stop=(i == last_k),
                            skip_group_check=True), reads=[bptt, bVA[i]], writes=bpo)
            for s in range(4):
                b1, c1 = oacc[(0, s)]
                b2, c2 = oacc[(1, s)]
                po1, bpo1 = preg(b1, c1, 129)
                po2, bpo2 = preg(b2, c2, 129)
                r1, br1 = rr_t["r1"]; r2, br2 = rr_t["r2"]; ss2, bss2 = rr_t["ss2"]; rr2, brr2 = rr_t["rr2"]
                op("dve", lambda e, po1=po1: e.reciprocal(out=r1[:], in_=po1[:, 128:129]), reads=bpo1, writes=[br1])
                op("dve", lambda e, po2=po2: e.reciprocal(out=r2[:], in_=po2[:, 128:129]), reads=bpo2, writes=[br2])
                op("dve", lambda e: e.tensor_tensor(out=r2[:], in0=r2[:], in1=lam[:], op=ALU.mult), reads=[br2, blam], writes=[br2])
                op("dve", lambda e, po2=po2: e.tensor_scalar(out=at_t2[:], in0=po2[:, 0:128], scalar1=r2[:, 0:1], scalar2=None,
                                                             op0=ALU.mult), reads=bpo2 + [br2], writes=[bat_t2])
                op("dve", lambda e, po1=po1: e.scalar_tensor_tensor(out=at_a[:], in0=po1[:, 0:128], scalar=r1[:, 0:1],
                                                                    in1=at_t2[:], op0=ALU.mult, op1=ALU.subtract),
                   reads=bpo1 + [br1, bat_t2], writes=[bat_a])
                op("act", lambda e: e.activation(out=at_junk[:], in_=at_a[:], func=AF.Square, accum_out=ss2[:]),
                   reads=[bat_a], writes=[bat_junk, bss2])
                op("dve", lambda e: e.tensor_scalar(out=rr2[:], in0=ss2[:], scalar1=1.0 / 128, scalar2=1e-6, op0=ALU.mult,
                                                    op1=ALU.add), reads=[bss2], writes=[brr2])
                op("act", lambda e: e.activation(out=rr2[:], in_=rr2[:], func=AF.Ln), reads=[brr2], writes=[brr2])
                op("act", lambda e: e.activation(out=rr2[:], in_=rr2[:], func=AF.Exp, scale=-0.5), reads=[brr2], writes=[brr2])
                op("dve", lambda e: e.scalar_tensor_tensor(out=at_y[:], in0=at_a[:], scalar=rr2[:, 0:1], in1=nrm[:, 1, :],
                                                           op0=ALU.mult, op1=ALU.mult), reads=[bat_a, brr2, bnrm], writes=[bat_y])
                pY, bpY = preg(6, 258, 128)
                op("pe", lambda e, pY=pY: e.transpose(pY, at_y[:], identf[:]), reads=[bat_y, bident], writes=bpY)
                op("act", lambda e, pY=pY, s=s: e.activation(out=ystage[0][0][:, s * 128:(s + 1) * 128], in_=pY, func=AF.Copy),
                   reads=bpY, writes=[ystage[0][1]])
            S.dma("sp", lambda e, t0=t0: e.dma_start(out=dr["yaT"][:, t0:t0 + 512], in_=ystage[0][0][:]),
                  reads=[ystage[0][1]], writes=[b_out], sem_buf=ystage[0][1], same_gen=True)
    return [b_out]


def host_prep_a(inp):
    x = inp["x"][0]
    w_in = inp["w_in"][0]
    xT = np.ascontiguousarray(x.T)
    conv_w = inp["conv_w"][0]
    table = inp["rel_bias_table"]
    nb = 16
    ki = np.arange(128)[:, None]
    qi = np.arange(128)[None, :]

    def bucket(rel):
        base = np.where(rel > 0, nb, 0)
        n = np.abs(rel)
        max_exact = nb // 2
        nf = np.maximum(n, 1).astype(np.float32)
        large = max_exact + (np.log(nf / np.float32(max_exact)) / np.float32(np.log(128 / max_exact))
                             * np.float32(nb - max_exact)).astype(np.int32)
        large = np.minimum(large, nb - 1)
        return base + np.where(n < max_exact, n, large)
    bk = np.stack([bucket(ki - qi), bucket(ki - 128 - qi)], axis=1)
    maps = []
    for h in range(NCORE):
        cols = np.concatenate([
            np.arange(h * 128, h * 128 + 128),
            1024 + np.arange(h * 128, h * 128 + 128),
            3072 + np.arange(h * 128, h * 128 + 128),
            4096 + np.arange(h * 128, h * 128 + 128),
            5120 + np.arange(h * 128, h * 128 + 128),
            2048 + np.arange(h * 128, h * 128 + 128),
            6144 + np.arange(h * 128, h * 128 + 128),
            np.array([7168 + h, 7176 + h]),
        ])
        wA = np.ascontiguousarray(w_in[:, cols])
        misc = np.zeros((128, 8), np.float32)
        misc[:, 0] = inp["gdn_a_log"][0, h]
        misc[:, 1] = inp["gdn_dt_bias"][0, h]
        misc[:, 2] = table[15, h]
        cw = np.stack([conv_w[:, i * 1024 + h * 128:i * 1024 + (h + 1) * 128].T for i in range(3)], axis=1)
        nrmw = np.stack([np.broadcast_to(inp["gdn_norm_w"][0], (128, 128)),
                         np.broadcast_to(inp["diff_subln_w"][0], (128, 128))], axis=1)
        lamr = np.broadcast_to(inp["diff_lambda"][0].reshape(1, 2, 2, 64), (128, 2, 2, 64))
        biasT = table[:, h][bk]
        maps.append({"xT": xT, "wA": wA, "miscA": misc, "convw": np.ascontiguousarray(cw, dtype=np.float32),
                     "nrmw": np.ascontiguousarray(nrmw, dtype=np.float32),
                     "lam": np.ascontiguousarray(lamr, dtype=np.float32),
                     "biasT": np.ascontiguousarray(biasT, dtype=np.float32)})
    return maps


def build_nc_a(n_tiles=NT_A, do_gdn=True, do_attn=True, max_ops=10 ** 9):
    nc = bass.Bass("TRN2", target_bir_lowering=False)
    dr = {}
    dr["xT"] = nc.dram_tensor("xT", [D, T], F32, kind="ExternalInput").ap()
    dr["wA"] = nc.dram_tensor("wA", [D, 898], F32, kind="ExternalInput").ap()
    dr["miscA"] = nc.dram_tensor("miscA", [128, 8], F32, kind="ExternalInput").ap()
    dr["convw"] = nc.dram_tensor("convw", [128, 3, 4], F32, kind="ExternalInput").ap()
    dr["nrmw"] = nc.dram_tensor("nrmw", [128, 2, 128], F32, kind="ExternalInput").ap()
    dr["lam"] = nc.dram_tensor("lam", [128, 2, 2, 64], F32, kind="ExternalInput").ap()
    dr["biasT"] = nc.dram_tensor("biasT", [128, 2, 128], F32, kind="ExternalInput").ap()
    dr["yaT"] = nc.dram_tensor("yaT", [128, T], BF16, kind="ExternalOutput").ap()
    dr["ybT"] = nc.dram_tensor("ybT", [128, T], BF16, kind="ExternalOutput").ap()
    S = Sched(nc)
    S.max_ops = max_ops
    outs = build_phase_a(nc, S, dr, n_tiles=n_tiles, do_gdn=do_gdn, do_attn=do_attn)
    S.wait_final("sp", outs)
    S.emit()
    return nc, S


TPC = T // NCORE
ALPHA = 2.0 ** 0.25
N_EXP = 64


def build_phase_b(nc, S, dr, n_exp=N_EXP, yall_bufs=()):
    C = Ctx(nc, S)
    op = S.op
    yall_bufs = list(yall_bufs)
    ARENA, _ = C.sb([128, 40960], BF16, "arena")
    bA = [Buf("arena0"), Buf("arena1"), Buf("arena2")]
    MT, _ = C.sb([128, 16, 1024], BF16, "MT")
    bMT = [Buf(f"MT{i}") for i in range(16)]
    ACC, _ = C.sb([128, 8, 2048], F32, "ACC")
    bACC = [Buf(f"ACC{i}") for i in range(8)]
    WGT = [C.sb([128, 4096], BF16, f"wgt{i}") for i in range(2)]
    tmpf = [C.sb([128, 512], F32, f"tmpf{i}") for i in range(2)]
    Gt, bGt = C.sb([128, 8, 64], F32, "Gt")
    identf, bident = C.sb([128, 128], F32, "identfB")
    onesB, bonesB = C.sb([128, 128], F32, "onesB")
    op("pool", lambda e: e.memset(onesB[:], 1.0), writes=[bonesB])
    op("pool", lambda e: e.affine_select(out=identf[:], in_=onesB[:], pattern=[[-1, 128]], compare_op=ALU.is_equal,
                                         fill=0.0, base=0, channel_multiplier=1), reads=[bonesB], writes=[bident])
    rb, brb = C.sb([128, 64], F32, "rbias")
    S.dma("sp", lambda e: e.dma_start(out=rb[:], in_=dr["rbias"]), writes=[brb], sem_buf=brb)
    wr, bwr = C.sb([128, 16, 64], F32, "wr")
    S.dma("sp", lambda e: e.dma_start(out=wr[:], in_=dr["w_router"].rearrange("(c p) n -> p c n", p=128)),
          writes=[bwr], sem_buf=bwr)
    epsc, bepsc = C.sb([128, 2], F32, "epscB")
    op("pool", lambda e: e.memset(epsc[:, 0:1], 1e-5), writes=[bepsc])
    small = {nm: C.sb([128, 8], F32, "smB_" + nm) for nm in ["m8", "gs", "g8", "gm", "t8", "den", "mv", "rstd", "nmr"]}
    st6, bst6 = C.sb([128, 4, 6], F32, "st6")
    ch, bch = C.sb([128, 64], F32, "choice")
    scs, bscs = C.sb([128, 64], F32, "scores")
    mc, bmc = C.sb([128, 64], F32, "mchoice")

    pb = [nc.alloc_psum_tensor(f"pbB{i}", [128, 512], F32) for i in range(8)]
    pq = [Buf(f"pbB{i}", excl=True) for i in range(8)]

    def AR(c0, n):
        return ARENA[:, c0:c0 + n]

    ACCb = ACC[:].rearrange("p t d -> p (t d)").bitcast(BF16)

    def XH(c0, n):
        return ACCb[:, c0:c0 + n]

    wba_v = dr["w_branch_a"].rearrange("(c p) n -> p c n", p=128)
    wbb_v = dr["w_branch_b"].rearrange("(c p) n -> p c n", p=128)
    for c in range(8):
        S.dma("pool", lambda e, c=c: e.dma_start(out=AR(c * 2048, 2048), in_=wba_v[:, c, :]),
              writes=[bA[0]], sem_buf=bA[0], same_gen=True)
    for c in range(8):
        S.dma("pool", lambda e, c=c: e.dma_start(out=AR(16384 + c * 2048, 2048), in_=wbb_v[:, c, :]),
              writes=[bA[1]], sem_buf=bA[1], same_gen=True)
    xTs = dr["xTs"].rearrange("(c p) t -> p c t", p=128)
    yall = dr["yall"].rearrange("(r p) t -> p r t", p=128)
    tok0 = dr["tok0"]
    bX = Buf("xhalf")
    bY = Buf("yhalf")
    for half in range(2):
        for c in range(16):
            S.dma("pool", lambda e, c=c, half=half: e.dma_start(
                out=XH(c * 512, 512), in_=xTs[:, c, half * 512:(half + 1) * 512]),
                writes=[bX], sem_buf=bX, same_gen=(c > 0))
        for r in range(16):
            S.dma("sp", lambda e, r=r, half=half: e.dma_start(
                out=XH(8192 + r * 512, 512), in_=yall[:, r, tok0 + half * 512:tok0 + (half + 1) * 512]),
                reads=yall_bufs, writes=[bY], sem_buf=bY, same_gen=(r > 0))
        for m in range(16):
            wg, bwg = WGT[m % 2]
            S.dma("pool", lambda e, wg=wg, m=m: e.dma_start(out=wg[:], in_=dr["wgates"][m]),
                  writes=[bwg], sem_buf=bwg)
            for ab in range(2):
                for c in range(16):
                    op("pe", lambda e, ab=ab, c=c, wg=wg: e.matmul(
                        pb[ab][:], lhsT=wg[:, c * 256 + ab * 128:c * 256 + ab * 128 + 128], rhs=XH(c * 512, 512),
                        start=(c == 0), stop=(c == 15)), reads=[bwg, bX], writes=[pq[ab]])
            for ab in range(2):
                for hh in range(8):
                    op("pe", lambda e, ab=ab, hh=hh, m=m: e.matmul(
                        pb[2 + ab][:], lhsT=AR(ab * 16384 + hh * 2048 + m * 128, 128),
                        rhs=XH(8192 + (2 * hh + ab) * 512, 512), start=(hh == 0), stop=(hh == 7)),
                        reads=[bA[ab], bY], writes=[pq[2 + ab]])
            t0_, bt0 = tmpf[0]; t1_, bt1 = tmpf[1]
            op("act", lambda e: e.activation(out=t0_[:], in_=pb[0][:], func=AF.Sigmoid), reads=[pq[0]], writes=[bt0])
            op("act", lambda e: e.activation(out=t1_[:], in_=pb[1][:], func=AF.Sigmoid), reads=[pq[1]], writes=[bt1])
            op("dve", lambda e: e.tensor_tensor(out=t0_[:], in0=t0_[:], in1=pb[2][:], op=ALU.mult),
               reads=[bt0, pq[2]], writes=[bt0])
            op("dve", lambda e: e.tensor_tensor(out=t1_[:], in0=t1_[:], in1=pb[3][:], op=ALU.mult),
               reads=[bt1, pq[3]], writes=[bt1])
            op("pool", lambda e, m=m, half=half: e.tensor_tensor(out=MT[:, m, half * 512:(half + 1) * 512], in0=t0_[:],
                                                                 in1=t1_[:], op=ALU.add), reads=[bt0, bt1], writes=[bMT[m]])

    wout_v = dr["w_out"].rearrange("(c p) n -> p c n", p=128)
    bWO = Buf("wout")
    for c in range(16):
        S.dma("pool", lambda e, c=c: e.dma_start(out=AR(c * 2048, 2048), in_=wout_v[:, c, :]),
              reads=[], writes=[bA[0], bA[1], bWO], sem_buf=bWO, same_gen=(c > 0))
    lnp = ARENA[:, 32768:40960].bitcast(F32).rearrange("p (a d) -> p a d", a=2)
    blnp = Buf("lnp")
    S.dma("sp", lambda e: e.dma_start(out=lnp, in_=dr["ln1"]), reads=[], writes=[blnp], sem_buf=blnp)
    xrows = dr["xrows"].rearrange("(t p) d -> p t d", p=128)
    for tt in range(8):
        S.dma("sp", lambda e, tt=tt: e.dma_start(out=ACC[:, tt, :], in_=xrows[:, tt, :]),
              writes=[bACC[tt], bX, bY], sem_buf=bACC[tt], same_gen=(tt > 0))
    hb = 0
    for tt in range(8):
        for dg in range(4):
            bank = 4 + (hb % 2)
            hb += 1
            for m in range(16):
                op("pe", lambda e, bank=bank, m=m, tt=tt, dg=dg: e.matmul(
                    pb[bank][:], lhsT=MT[:, m, tt * 128:(tt + 1) * 128], rhs=AR(m * 2048 + dg * 512, 512),
                    start=(m == 0), stop=(m == 15)), reads=[bMT[m], bWO], writes=[pq[bank]])
            op("dve", lambda e, bank=bank, tt=tt, dg=dg: e.scalar_tensor_tensor(
                out=ACC[:, tt, dg * 512:(dg + 1) * 512], in0=ACC[:, tt, dg * 512:(dg + 1) * 512], scalar=ALPHA,
                in1=pb[bank][:], op0=ALU.mult, op1=ALU.add), reads=[bACC[tt], pq[bank]], writes=[bACC[tt]])

    def layer_norm(tt, prm, bprm):
        mv, bmv = small["mv"]; rstd, brstd = small["rstd"]
        for q in range(4):
            op("dve", lambda e, q=q, tt=tt: e.bn_stats(out=st6[:, q, :], in_=ACC[:, tt, q * 512:(q + 1) * 512]),
               reads=[bACC[tt]], writes=[bst6])
        op("dve", lambda e: e.bn_aggr(out=mv[:, 0:2], in_=st6[:].rearrange("p a b -> p (a b)")), reads=[bst6], writes=[bmv])
        op("act", lambda e: e.activation(out=rstd[:, 0:1], in_=mv[:, 1:2], func=AF.Ln, bias=epsc[:, 0:1]),
           reads=[bmv, bepsc], writes=[brstd])
        op("act", lambda e: e.activation(out=rstd[:, 0:1], in_=rstd[:, 0:1], func=AF.Exp, scale=-0.5),
           reads=[brstd], writes=[brstd])
        op("dve", lambda e, tt=tt: e.tensor_scalar(out=ACC[:, tt, :], in0=ACC[:, tt, :], scalar1=mv[:, 0:1],
                                                   scalar2=rstd[:, 0:1], op0=ALU.subtract, op1=ALU.mult),
           reads=[bACC[tt], bmv, brstd], writes=[bACC[tt]])
        op("pool", lambda e, tt=tt: e.tensor_tensor(out=ACC[:, tt, :], in0=ACC[:, tt, :], in1=prm[:, 0, :], op=ALU.mult),
           reads=[bACC[tt], bprm], writes=[bACC[tt]])
        op("pool", lambda e, tt=tt: e.tensor_tensor(out=ACC[:, tt, :], in0=ACC[:, tt, :], in1=prm[:, 1, :], op=ALU.add),
           reads=[bACC[tt], bprm], writes=[bACC[tt]])

    for tt in range(8):
        layer_norm(tt, lnp, blnp)
        xf, bX1F = WGT[tt % 2]
        X1F = xf[:].bitcast(F32)
        for c4 in range(4):
            bank = 6 + (c4 % 2)
            for k in range(4):
                c = c4 * 4 + k
                op("pe", lambda e, bank=bank, k=k, c=c, tt=tt: e.transpose(
                    pb[bank][:, k * 128:(k + 1) * 128], ACC[:, tt, c * 128:(c + 1) * 128], identf[:]),
                    reads=[bACC[tt], bident], writes=[pq[bank]])
            op("act", lambda e, bank=bank, c4=c4, tt=tt: e.activation(
                out=MT[:, c4 * 4:(c4 + 1) * 4, tt * 128:(tt + 1) * 128],
                in_=pb[bank][:].rearrange("p (k t) -> p k t", k=4), func=AF.Copy),
                reads=[pq[bank]], writes=[bMT[c4 * 4 + k] for k in range(4)])
            op("dve", lambda e, bank=bank, c4=c4, X1F=X1F: e.tensor_copy(out=X1F[:, c4 * 512:(c4 + 1) * 512], in_=pb[bank][:]),
               reads=[pq[bank]], writes=[bX1F])
        for c in range(16):
            op("pe", lambda e, c=c, X1F=X1F: e.matmul(pb[0][:, 0:64], lhsT=X1F[:, c * 128:(c + 1) * 128], rhs=wr[:, c, :],
                                             start=(c == 0), stop=(c == 15)), reads=[bX1F, bwr], writes=[pq[0]])
        m8, bm8 = small["m8"]; gs, bgs = small["gs"]; g8, bg8 = small["g8"]; gm, bgm = small["gm"]
        t8, bt8 = small["t8"]; den, bden = small["den"]
        op("act", lambda e: e.activation(out=scs[:], in_=pb[0][:, 0:64], func=AF.Sigmoid), reads=[pq[0]], writes=[bscs])
        op("dve", lambda e: e.tensor_tensor(out=ch[:], in0=scs[:], in1=rb[:], op=ALU.add), reads=[bscs, brb], writes=[bch])
        for g in range(8):
            op("dve", lambda e, g=g: e.max(out=m8[:], in_=ch[:, g * 8:(g + 1) * 8]), reads=[bch], writes=[bm8])
            op("dve", lambda e, g=g: e.tensor_tensor(out=gs[:, g:g + 1], in0=m8[:, 0:1], in1=m8[:, 1:2], op=ALU.add),
               reads=[bm8], writes=[bgs])
        op("dve", lambda e: e.max(out=g8[:], in_=gs[:]), reads=[bgs], writes=[bg8])
        op("dve", lambda e: e.tensor_scalar(out=gm[:], in0=gs[:], scalar1=g8[:, 3:4], scalar2=None, op0=ALU.is_ge),
           reads=[bgs, bg8], writes=[bgm])
        op("dve", lambda e: e.tensor_scalar(out=gm[:], in0=gm[:], scalar1=-1.0, scalar2=1e30, op0=ALU.add, op1=ALU.mult),
           reads=[bgm], writes=[bgm])
        for g in range(8):
            op("dve", lambda e, g=g: e.tensor_scalar(out=mc[:, g * 8:(g + 1) * 8], in0=ch[:, g * 8:(g + 1) * 8],
                                                     scalar1=gm[:, g:g + 1], scalar2=None, op0=ALU.add),
               reads=[bch, bgm], writes=[bmc])
        op("dve", lambda e: e.max(out=t8[:], in_=mc[:]), reads=[bmc], writes=[bt8])
        op("dve", lambda e: e.tensor_scalar(out=mc[:], in0=mc[:], scalar1=t8[:, 7:8], scalar2=None, op0=ALU.is_ge),
           reads=[bmc, bt8], writes=[bmc])
        op("dve", lambda e: e.tensor_tensor(out=mc[:], in0=mc[:], in1=scs[:], op=ALU.mult), reads=[bmc, bscs], writes=[bmc])
        op("dve", lambda e: e.tensor_reduce(out=den[:, 0:1], in_=mc[:], axis=AX.X, op=ALU.add), reads=[bmc], writes=[bden])
        op("dve", lambda e: e.reciprocal(out=den[:, 0:1], in_=den[:, 0:1]), reads=[bden], writes=[bden])
        op("dve", lambda e, tt=tt: e.tensor_scalar(out=Gt[:, tt, :], in0=mc[:], scalar1=den[:, 0:1], scalar2=2.5,
                                                   op0=ALU.mult, op1=ALU.mult), reads=[bmc, bden], writes=[bGt])
        op("act", lambda e, tt=tt: e.activation(out=ACC[:, tt, :], in_=ACC[:, tt, :], func=AF.Copy, scale=ALPHA),
           reads=[bACC[tt]], writes=[bACC[tt]])

    bEW = [Buf("ew0"), Buf("ew1")]
    bWD = Buf("ewd")
    yb_ctr = 0
    for e_i in range(n_exp + 1):
        base = (e_i % 2) * 16384
        bew = bEW[e_i % 2]
        if e_i < n_exp:
            srcs = [dr["w_gate"][e_i].rearrange("(c p) f -> p c f", p=128), dr["w_up"][e_i].rearrange("(c p) f -> p c f", p=128),
                    dr["w_down"][e_i].rearrange("(c p) n -> p c n", p=128)]
        else:
            srcs = [dr["ws_gate"].rearrange("(c p) f -> p c f", p=128), dr["ws_up"].rearrange("(c p) f -> p c f", p=128),
                    dr["ws_down"].rearrange("(c p) n -> p c n", p=128)]
        extra_w = [bA[0], bA[1], bWO, blnp] if e_i < 2 else []
        for wi in range(2):
            dstv = ARENA[:, base + wi * 8192:base + (wi + 1) * 8192].rearrange("p (c f) -> p c f", c=16)
            S.dma("pool", lambda e, dstv=dstv, src=srcs[wi]: e.dma_start(out=dstv, in_=src),
                  writes=[bew] + extra_w, sem_buf=bew, same_gen=(wi > 0))
        dstv = ARENA[:, 32768:40960].rearrange("p (c n) -> p c n", c=4)
        S.dma("pool", lambda e, dstv=dstv, src=srcs[2]: e.dma_start(out=dstv, in_=src),
              writes=[bWD] + extra_w, sem_buf=bWD)
        hT, bhT = WGT[e_i % 2]
        sg_, bsg = tmpf[0]
        for half in range(2):
            for fc in range(4):
                for gu in range(2):
                    bank = gu * 2 + (fc % 2)
                    for c in range(16):
                        op("pe", lambda e, bank=bank, gu=gu, fc=fc, c=c, base=base, half=half: e.matmul(
                            pb[bank][:], lhsT=AR(base + gu * 8192 + c * 512 + fc * 128, 128),
                            rhs=MT[:, c, half * 512:(half + 1) * 512], start=(c == 0), stop=(c == 15)),
                            reads=[bew, bMT[c]], writes=[pq[bank]])
                bg_, bu_ = fc % 2, 2 + (fc % 2)
                op("act", lambda e, bg_=bg_: e.activation(out=sg_[:], in_=pb[bg_][:], func=AF.Silu), reads=[pq[bg_]], writes=[bsg])
                op("dve", lambda e, bu_=bu_, hT=hT, fc=fc, half=half: e.tensor_tensor(
                    out=hT[:, fc * 1024 + half * 512:fc * 1024 + (half + 1) * 512], in0=sg_[:], in1=pb[bu_][:], op=ALU.mult),
                    reads=[bsg, pq[bu_]], writes=[bhT])
        for tt in range(8):
            for dg in range(4):
                bank = 4 + (yb_ctr % 4)
                yb_ctr += 1
                for fc in range(4):
                    op("pe", lambda e, bank=bank, fc=fc, tt=tt, dg=dg, hT=hT, base=base: e.matmul(
                        pb[bank][:], lhsT=hT[:, fc * 1024 + tt * 128:fc * 1024 + (tt + 1) * 128],
                        rhs=AR(32768 + fc * 2048 + dg * 512, 512), start=(fc == 0), stop=(fc == 3)),
                        reads=[bhT, bWD], writes=[pq[bank]])
                if e_i < n_exp:
                    op("dve", lambda e, bank=bank, tt=tt, dg=dg, e_i=e_i: e.scalar_tensor_tensor(
                        out=ACC[:, tt, dg * 512:(dg + 1) * 512], in0=pb[bank][:], scalar=Gt[:, tt, e_i:e_i + 1],
                        in1=ACC[:, tt, dg * 512:(dg + 1) * 512], op0=ALU.mult, op1=ALU.add),
                        reads=[pq[bank], bGt, bACC[tt]], writes=[bACC[tt]])
                else:
                    op("dve", lambda e, bank=bank, tt=tt, dg=dg: e.tensor_tensor(
                        out=ACC[:, tt, dg * 512:(dg + 1) * 512], in0=pb[bank][:], in1=ACC[:, tt, dg * 512:(dg + 1) * 512],
                        op=ALU.add), reads=[pq[bank], bACC[tt]], writes=[bACC[tt]])

    ln2base = ((n_exp + 1) % 2) * 16384
    lnp2 = ARENA[:, ln2base:ln2base + 8192].bitcast(F32).rearrange("p (a d) -> p a d", a=2)
    blnp2 = Buf("lnp2")
    S.dma("sp", lambda e: e.dma_start(out=lnp2, in_=dr["ln2"]), writes=[bEW[(n_exp + 1) % 2], blnp2], sem_buf=blnp2)
    b_out = Buf("outB")
    outv = dr["out"].rearrange("(t p) d -> p t d", p=128)
    for tt in range(8):
        layer_norm(tt, lnp2, blnp2)
        S.dma("sp", lambda e, tt=tt: e.dma_start(out=outv[:, tt, :], in_=ACC[:, tt, :]), reads=[bACC[tt]], writes=[b_out],
              sem_buf=bACC[tt], same_gen=True)
    return [b_out]


def host_prep_b(inp, yall):
    x = inp["x"][0]
    w_in = inp["w_in"][0]
    ga = w_in[:, 7184:7184 + 2048].reshape(16, 128, 16, 128)
    gb = w_in[:, 9232:9232 + 2048].reshape(16, 128, 16, 128)
    wg = np.stack([ga, gb], axis=3)
    wgates = np.ascontiguousarray(wg.transpose(2, 1, 0, 3, 4).reshape(16, 128, 16 * 256))
    common = {
        "wgates": wgates, "yall": yall,
        "w_branch_a": inp["w_branch_a"][0], "w_branch_b": inp["w_branch_b"][0], "w_out": inp["w_out"][0],
        "ln1": np.ascontiguousarray(np.broadcast_to(np.stack([inp["ln1_g"][0], inp["ln1_b"][0]])[None], (128, 2, D))),
        "ln2": np.ascontiguousarray(np.broadcast_to(np.stack([inp["ln2_g"][0], inp["ln2_b"][0]])[None], (128, 2, D))),
        "rbias": np.ascontiguousarray(np.broadcast_to(inp["router_bias"][0][None], (128, 64))),
        "w_router": inp["w_router"][0],
        "w_gate": inp["w_gate"][0], "w_up": inp["w_up"][0], "w_down": inp["w_down"][0],
        "ws_gate": inp["ws_gate"][0], "ws_up": inp["ws_up"][0], "ws_down": inp["ws_down"][0],
    }
    maps = []
    xT = None
    for c in range(NCORE):
        xr = np.ascontiguousarray(x[c * TPC:(c + 1) * TPC])
        m = dict(common)
        m["xrows"] = xr
        m["xTs"] = np.ascontiguousarray(xr.T)
        maps.append(m)
    return maps


def declare_b(nc, dr, n_exp=N_EXP, fused=False):
    def din(name, shape, dt=F32):
        dr[name] = nc.dram_tensor(name, list(shape), dt, kind="ExternalInput").ap()
    din("wgates", [16, 128, 4096])
    din("w_branch_a", [1024, D]); din("w_branch_b", [1024, D]); din("w_out", [D, D])
    din("ln1", [128, 2, D]); din("ln2", [128, 2, D]); din("rbias", [128, 64]); din("w_router", [D, 64])
    din("w_gate", [64, D, 512]); din("w_up", [64, D, 512]); din("w_down", [64, 512, D])
    din("ws_gate", [D, 512]); din("ws_up", [D, 512]); din("ws_down", [512, D])
    din("xrows", [TPC, D]); din("xTs", [D, TPC])
    dr["out"] = nc.dram_tensor("out", [TPC, D], F32, kind="ExternalOutput").ap()


def build_nc_b(n_exp=N_EXP):
    nc = bass.Bass("TRN2", target_bir_lowering=False)
    dr = {}
    declare_b(nc, dr, n_exp)
    dr["yall"] = nc.dram_tensor("yall", [2048, TPC], BF16, kind="ExternalInput").ap()
    dr["tok0"] = 0
    S = Sched(nc)
    outs = build_phase_b(nc, S, dr, n_exp=n_exp)
    S.wait_final("sp", outs)
    S.emit()
    return nc, S


def _run_two_launch(inputs):
    maps_a = host_prep_a(inputs)
    nc_a, _ = build_nc_a()
    res_a = run_bass_kernel_spmd(nc_a, maps_a, core_ids=list(range(NCORE)))
    rows = []
    for h in range(NCORE):
        rows.append(np.asarray(res_a.results[h]["yaT"]))
        rows.append(np.asarray(res_a.results[h]["ybT"]))
    yall = np.concatenate(rows, axis=0)
    maps_b = host_prep_b(inputs, None)
    for c in range(NCORE):
        maps_b[c]["yall"] = np.ascontiguousarray(yall[:, c * TPC:(c + 1) * TPC])
    nc_b, _ = build_nc_b()
    res_b = run_bass_kernel_spmd(nc_b, maps_b, core_ids=list(range(NCORE)))
    out = np.concatenate([np.asarray(res_b.results[c]["out"]) for c in range(NCORE)], axis=0)
    return out.reshape(1, T, D).astype(np.float32)


def kernel(**inputs):
    inputs = {k: np.asarray(v) for k, v in inputs.items()}
    return _run_two_launch(inputs)
```

```python
import numpy as np
import ml_dtypes
import concourse.bass as bass
import concourse.mybir as mybir
from concourse.bass_utils import run_bass_kernel_spmd

F32 = mybir.dt.float32
BF16 = mybir.dt.bfloat16
I32 = mybir.dt.int32
AF = mybir.ActivationFunctionType
ALU = mybir.AluOpType
AX = mybir.AxisListType

T = 8192
D = 2048
NCORE = 8
ENGS = ["pe", "act", "dve", "pool", "sp"]


class Buf:
    __slots__ = ("name", "writes", "reads", "sem", "cnt", "excl")

    def __init__(self, name, excl=False):
        self.name = name
        self.excl = excl
        self.writes = []
        self.reads = []
        self.sem = None
        self.cnt = 0


class Sched:
    def __init__(self, nc):
        self.nc = nc
        self.prog = {e: [] for e in ENGS}
        self.sem = {e: nc.alloc_semaphore(name=f"s_{e}") for e in ENGS if e != "sp"}
        self.cnt = {e: 0 for e in ENGS}
        self.seen = {e: {} for e in ENGS}
        self.n_sems = 4

    def _waits(self, eng, reads, writes, same_gen=False):
        toks = []
        for b in reads:
            toks.extend(b.writes)
        for b in writes:
            toks.extend(b.reads)
            if not same_gen:
                toks.extend(b.writes)
        need = {}
        for (sem, val, src) in toks:
            if src == "pe" and eng == "pe":
                continue
            k = id(sem)
            if self.seen[eng].get(k, 0) >= val:
                continue
            if k not in need or need[k][1] < val:
                need[k] = (sem, val)
        out = []
        for k, (sem, val) in need.items():
            self.seen[eng][k] = val
            out.append((sem, val))
        return out

    def _commit(self, tok, reads, writes, same_gen=False):
        for b in reads:
            b.reads.append(tok)
            if len(b.reads) > 64:
                b.reads = b.reads[-64:] if False else b.reads
        for b in writes:
            if same_gen:
                b.writes.append(tok)
            else:
                b.writes = [tok]
                b.reads = []

    def op(self, eng, fn, reads=(), writes=()):
        self.nops = getattr(self, "nops", 0) + 1
        if self.nops > getattr(self, "max_ops", 10 ** 9):
            return None
        ex = [b for b in reads if b.excl]
        if ex:
            reads = [b for b in reads if not b.excl]
            writes = list(writes) + ex
        waits = self._waits(eng, reads, writes)
        self.cnt[eng] += 1
        tok = (self.sem[eng], self.cnt[eng], eng)
        self.prog[eng].append((waits, fn, (self.sem[eng], 1)))
        self._commit(tok, reads, writes)
        return tok

    def dma(self, q, fn, reads=(), writes=(), sem_buf=None, same_gen=False, inc=16):
        self.nops = getattr(self, "nops", 0) + 1
        if self.nops > getattr(self, "max_ops", 10 ** 9):
            return None
        waits = self._waits(q, reads, writes, same_gen=same_gen)
        if sem_buf.sem is None:
            sem_buf.sem = self.nc.alloc_semaphore(name=f"d_{sem_buf.name}")
            self.n_sems += 1
        sem_buf.cnt += inc
        tok = (sem_buf.sem, sem_buf.cnt, "dma")
        self.prog[q].append((waits, fn, (sem_buf.sem, inc)))
        self._commit(tok, reads, writes, same_gen=same_gen)
        return tok

    def wait_final(self, eng, bufs):
        need = {}
        for b in bufs:
            for (sem, val, src) in list(b.writes) + list(b.reads):
                k = id(sem)
                if k not in need or need[k][1] < val:
                    need[k] = (sem, val)
        self.prog[eng].append((list(need.values()), None, None))

    def emit(self):
        nc = self.nc
        handles = {"pe": "tensor", "act": "scalar", "dve": "vector", "pool": "gpsimd", "sp": "sync"}
        with nc.Block() as block:
            for e in ENGS:
                prog = self.prog[e]
                if not prog:
                    continue

                def body(eng, prog=prog):
                    for waits, fn, inc in prog:
                        for (sem, val) in waits:
                            eng.wait_ge(sem, val)
                        if fn is not None:
                            fn(eng).then_inc(inc[0], inc[1])

                getattr(block, handles[e])(body)


class Ctx:
    def __init__(self, nc, S):
        self.nc = nc
        self.S = S
        self.n = 0

    def sb(self, shape, dt, name=None):
        self.n += 1
        name = name or f"t{self.n}"
        t = self.nc.alloc_sbuf_tensor("sb_" + name, list(shape), dt)
        return t, Buf(name)


NT_A = 16
GDN_SQ = 6


def build_phase_a(nc, S, dr, n_tiles=NT_A, do_gdn=True, do_attn=True):
    C = Ctx(nc, S)
    cur = [None]

    def op(eng, fn, reads=(), writes=()):
        cur[0].append((0, eng, fn, tuple(reads), tuple(writes), None))

    def dma(q, fn, **kw):
        cur[0].append((1, q, fn, None, None, kw))

    def flush(lst):
        for (k, e, fn, r, w, kw) in lst:
            if k == 0:
                S.op(e, fn, reads=r, writes=w)
            else:
                Sched.dma(S, e, fn, **kw)

    def merge(a, b):
        out = []
        ia = ib = 0
        na, nb = len(a), len(b)
        while ia < na or ib < nb:
            if ib >= nb or (ia < na and ia * max(nb, 1) <= ib * max(na, 1)):
                out.append(a[ia]); ia += 1
            else:
                out.append(b[ib]); ib += 1
        return out
    setup_ops = []
    cur[0] = setup_ops

    xT = dr["xT"].rearrange("(c p) t -> p c t", p=128)
    W, bW = C.sb([128, 16, 898], BF16, "W")
    wa_v = dr["wA"].rearrange("(c p) n -> p c n", p=128)
    for c4 in range(4):
        b = Buf(f"Wl{c4}")
        dma("pool", lambda e, c4=c4: e.dma_start(out=W[:, c4 * 4:(c4 + 1) * 4, :], in_=wa_v[:, c4 * 4:(c4 + 1) * 4, :]),
              writes=[bW], sem_buf=bW, same_gen=True)
    misc, bmisc = C.sb([128, 8], F32, "misc")
    dma("sp", lambda e: e.dma_start(out=misc[:], in_=dr["miscA"]), writes=[bmisc], sem_buf=bmisc)
    cw, bcw = C.sb([128, 3, 4], F32, "cw")
    dma("sp", lambda e: e.dma_start(out=cw[:], in_=dr["convw"]), writes=[bcw], sem_buf=bcw)
    nrm, bnrm = C.sb([128, 2, 128], F32, "nrm")
    dma("sp", lambda e: e.dma_start(out=nrm[:], in_=dr["nrmw"]), writes=[bnrm], sem_buf=bnrm)
    lamt, blamt = C.sb([128, 2, 2, 64], F32, "lamt")
    dma("sp", lambda e: e.dma_start(out=lamt[:], in_=dr["lam"]), writes=[blamt], sem_buf=blamt)
    BT, bBT = C.sb([128, 2, 128], F32, "BT")
    dma("sp", lambda e: e.dma_start(out=BT[:], in_=dr["biasT"]), writes=[bBT], sem_buf=bBT)

    onesf, bones = C.sb([128, 128], F32, "onesf")
    op("pool", lambda e: e.memset(onesf[:], 1.0), writes=[bones])
    identf, bident = C.sb([128, 128], F32, "identf")
    op("pool", lambda e: e.affine_select(out=identf[:], in_=onesf[:], pattern=[[-1, 128]], compare_op=ALU.is_equal,
                                         fill=0.0, base=0, channel_multiplier=1), reads=[bones], writes=[bident])
    triu, btriu = C.sb([128, 128], F32, "triu")
    op("pool", lambda e: e.affine_select(out=triu[:], in_=onesf[:], pattern=[[1, 128]], compare_op=ALU.is_ge,
                                         fill=0.0, base=0, channel_multiplier=-1), reads=[bones], writes=[btriu])
    identb, bidentb = C.sb([128, 128], BF16, "identb")
    op("dve", lambda e: e.tensor_copy(out=identb[:], in_=identf[:]), reads=[bident], writes=[bidentb])

    epsc, bepsc = C.sb([128, 1], F32, "epsc")
    op("pool", lambda e: e.memset(epsc[:], 1e-6), writes=[bepsc])
    negA, bnegA = C.sb([128, 1], F32, "negA")
    op("act", lambda e: e.activation(out=negA[:], in_=misc[:, 0:1], func=AF.Exp), reads=[bmisc], writes=[bnegA])
    op("dve", lambda e: e.tensor_scalar(out=negA[:], in0=negA[:], scalar1=-1.0, scalar2=None, op0=ALU.mult),
       reads=[bnegA], writes=[bnegA])
    lprod, blprod = C.sb([128, 2, 64], F32, "lprod")
    op("dve", lambda e: e.tensor_tensor(out=lprod[:], in0=lamt[:, :, 0, :], in1=lamt[:, :, 1, :], op=ALU.mult),
       reads=[blamt], writes=[blprod])
    lsum, blsum = C.sb([128, 2], F32, "lsum")
    op("dve", lambda e: e.tensor_reduce(out=lsum[:], in_=lprod[:], axis=AX.X, op=ALU.add), reads=[blprod], writes=[blsum])
    op("act", lambda e: e.activation(out=lsum[:], in_=lsum[:], func=AF.Exp), reads=[blsum], writes=[blsum])
    lam, blam = C.sb([128, 1], F32, "lam")
    op("dve", lambda e: e.scalar_tensor_tensor(out=lam[:], in0=lsum[:, 0:1], scalar=0.2, in1=lsum[:, 1:2],
                                               op0=ALU.add, op1=ALU.subtract), reads=[blsum], writes=[blam])
    op("dve", lambda e: e.tensor_scalar(out=BT[:], in0=BT[:], scalar1=misc[:, 2:3], scalar2=None, op0=ALU.subtract),
       reads=[bBT, bmisc], writes=[bBT])
    op("dve", lambda e: e.memset(BT[64:128, 0, 0:64], -30000.0), writes=[bBT])
    op("dve", lambda e: e.tensor_scalar(out=nrm[:, 1, :], in0=nrm[:, 1, :], scalar1=0.8, scalar2=None, op0=ALU.mult),
       reads=[bnrm], writes=[bnrm])

    KT, _ = C.sb([128, T], BF16, "KT")
    bKT = [Buf(f"KT{j}") for j in range(NT_A)]
    VA, _ = C.sb([128, 64, 130], BF16, "VA")
    bVA = [Buf(f"VA{j}") for j in range(64)]
    op("pool", lambda e: e.memset(VA[:, :, 128:130], 1.0), writes=bVA)
    QT = [C.sb([128, 512], BF16, f"QT{i}") for i in range(2)]
    xt = [C.sb([128, 16, 512], BF16, f"xt{i}") for i in range(2)]
    cins = [C.sb([128, 3, 515], F32, f"cin{i}") for i in range(2)]
    cout, bcout = C.sb([128, 3, 512], F32, "cout")
    sqt, bsqt = C.sb([128, 512], F32, "sqt")
    qkn = [C.sb([128, 512], F32, f"qkn{i}") for i in range(2)]
    tms = [C.sb([128, 4, 130], F32, f"tm{i}") for i in range(2)]
    sc = {nm: C.sb([128, 4], F32, "sc_" + nm) for nm in
          ["x", "ax", "e", "l", "g", "beta", "gc", "gtot", "eg", "nbeg", "ekd", "glast", "nbeta", "tmp", "ss4", "rr4"]}
    Sst = [C.sb([128, 128], F32, f"Sst{i}") for i in range(2)]
    op("pool", lambda e: e.memset(Sst[0][0][:], 0.0), writes=[Sst[0][1]])
    ystage = [C.sb([128, 512], BF16, f"ystg{i}") for i in range(2)]

    pb = [nc.alloc_psum_tensor(f"pb{i}", [128, 512], F32) for i in range(8)]
    pq = [[Buf(f"pb{i}q{k}", excl=True) for k in range(4)] for i in range(8)]

    stream = ["proj"]
    BMAP = {"proj": {0: 0, 1: 0, 2: 0}, "attn": {7: 1, 3: 1, 4: 2, 5: 3, 6: 4},
            "gdn": {0: 7, 1: 7, 3: 5, 4: 6, 5: 7, 6: 5, 7: 6}}

    def preg(bank, c0, n):
        bank = BMAP[stream[0]][bank]
        return pb[bank][:, c0:c0 + n], [pq[bank][0]]
    g3, g4b, g5, g6, g7 = 5, 6, 7, 5, 6

    def mk(name, shape=(128, 128), dt=F32):
        return C.sb(list(shape), dt, name)
    G4 = {nm: mk("g4_" + nm, (128, 4, 128)) for nm in ["dg", "t1", "E1", "E2", "Erow", "M", "MT", "X", "XT", "AqkT", "kdec",
                                                       "ATm", "qd", "QeffT", "zs", "yb", "junk"]}
    G4["R"] = mk("g4_R", (128, 4, 256))
    nonesf, bnones = C.sb([128, 128], F32, "nonesf")
    op("pool", lambda e: e.memset(nonesf[:], -1.0), writes=[bnones])
    nrm4, bnrm4 = C.sb([128, 4, 128], F32, "nrm4")
    for s4 in range(4):
        op("pool", lambda e, s4=s4: e.tensor_copy(out=nrm4[:, s4, :], in_=nrm[:, 0, :]), reads=[bnrm], writes=[bnrm4])
    rr_t = {nm: C.sb([128, 1], F32, "rr_" + nm) for nm in ["ss", "rr", "r1", "r2", "ss2", "rr2"]}
    PT = [C.sb([128, 512], BF16, f"PT{i}") for i in range(4)]
    at_t2, bat_t2 = C.sb([128, 128], F32, "at_t2")
    at_a, bat_a = C.sb([128, 128], F32, "at_a")
    at_y, bat_y = C.sb([128, 128], F32, "at_y")
    at_junk, bat_junk = C.sb([128, 128], F32, "at_junk")

    b_out = Buf("outA")
    pt_ctr = [0]

    halo, bhalo = C.sb([128, 3, 3], F32, "halo")
    op("pool", lambda e: e.memset(halo[:], 0.0), writes=[bhalo])
    flush(setup_ops)
    Pl, Gl, Al = [], [], []
    for j in range(n_tiles):
        t0 = j * 512
        xtt, bxt = xt[j % 2]
        cin, bcin = cins[j % 2]
        tm, btm = tms[j % 2]
        Pl.append([]); Gl.append([]); Al.append([])
        cur[0] = Pl[j]
        stream[0] = "proj"
        for half in range(2):
            dma("pool", lambda e, xtt=xtt, half=half, t0=t0: e.dma_start(
                out=xtt[:, half * 8:(half + 1) * 8, :], in_=xT[:, half * 8:(half + 1) * 8, t0:t0 + 512]),
                writes=[bxt], sem_buf=bxt, same_gen=(half == 1))
        QTc, bQTc = QT[j % 2]
        for g in range(5):
            bank = g % 2
            ps, pbufs = preg(bank, 0, 512)
            for c in range(16):
                op("pe", lambda e, ps=ps, g=g, c=c, xtt=xtt: e.matmul(ps, lhsT=W[:, c, g * 128:(g + 1) * 128],
                                                                      rhs=xtt[:, c, :], start=(c == 0), stop=(c == 15)),
                   reads=[bW, bxt], writes=pbufs)
            if g == 0:
                op("act", lambda e, ps=ps, QTc=QTc: e.activation(out=QTc[:], in_=ps, func=AF.Copy, scale=0.125),
                   reads=pbufs, writes=[bQTc])
            elif g == 1:
                op("act", lambda e, ps=ps, t0=t0: e.activation(out=KT[:, t0:t0 + 512], in_=ps, func=AF.Copy),
                   reads=pbufs, writes=[bKT[j]])
            else:
                op("act", lambda e, ps=ps, g=g, cin=cin: e.activation(out=cin[:, g - 2, 3:515], in_=ps, func=AF.Copy),
                   reads=pbufs, writes=[bcin])
        for s in range(4):
            ps, pbufs = preg(2, 0, 258)
            for c in range(16):
                op("pe", lambda e, ps=ps, c=c, s=s, xtt=xtt: e.matmul(ps, lhsT=xtt[:, c, s * 128:(s + 1) * 128],
                                                                      rhs=W[:, c, 640:898], start=(c == 0), stop=(c == 15)),
                   reads=[bW, bxt], writes=pbufs)
            op("act", lambda e, s=s, j=j: e.activation(out=VA[:, 4 * j + s, 0:128], in_=pb[0][:, 0:128], func=AF.Copy),
               reads=pbufs, writes=[bVA[4 * j + s]])
            op("dve", lambda e, s=s, tm=tm: e.tensor_copy(out=tm[:, s, :], in_=pb[0][:, 128:258]), reads=pbufs, writes=[btm])

        cur[0] = Gl[j]
        stream[0] = "gdn"
        if do_gdn:
            op("dve", lambda e, cin=cin: e.tensor_copy(out=cin[:, :, 0:3], in_=halo[:]), reads=[bhalo], writes=[bcin])
            for i in range(3):
                op("dve", lambda e, i=i, cin=cin: e.tensor_scalar(out=cout[:, i, :], in0=cin[:, i, 0:512], scalar1=cw[:, i, 0:1],
                                                         scalar2=None, op0=ALU.mult), reads=[bcin, bcw], writes=[bcout])
                for jj in range(1, 4):
                    op("dve", lambda e, i=i, jj=jj, cin=cin: e.scalar_tensor_tensor(
                        out=cout[:, i, :], in0=cin[:, i, jj:jj + 512], scalar=cw[:, i, jj:jj + 1], in1=cout[:, i, :],
                        op0=ALU.mult, op1=ALU.add), reads=[bcin, bcw, bcout], writes=[bcout])
            op("dve", lambda e, cin=cin: e.tensor_copy(out=halo[:], in_=cin[:, :, 512:515]), reads=[bcin], writes=[bhalo])
            op("act", lambda e: e.activation(out=cout[:], in_=cout[:], func=AF.Silu), reads=[bcout], writes=[bcout])
            for i in range(2):
                qk, bqk = qkn[i]
                op("act", lambda e, i=i: e.activation(out=sqt[:], in_=cout[:, i, :], func=AF.Square),
                   reads=[bcout], writes=[bsqt])
                ps, pbufs = preg(i, 0, 512)
                op("pe", lambda e, ps=ps: e.matmul(ps, lhsT=onesf[:], rhs=sqt[:], start=True, stop=True),
                   reads=[bones, bsqt], writes=pbufs)
                op("act", lambda e, ps=ps: e.activation(out=sqt[:], in_=ps, func=AF.Ln, bias=epsc[:, 0:1]), reads=pbufs + [bepsc], writes=[bsqt])
                op("act", lambda e: e.activation(out=sqt[:], in_=sqt[:], func=AF.Exp, scale=-0.5), reads=[bsqt], writes=[bsqt])
                scl = (128.0 ** -0.5) if i == 0 else 1.0
                op("dve", lambda e, i=i, qk=qk, scl=scl: e.scalar_tensor_tensor(
                    out=qk[:], in0=cout[:, i, :], scalar=scl, in1=sqt[:], op0=ALU.mult, op1=ALU.mult),
                    reads=[bcout, bsqt], writes=[bqk])
            def sct(nm):
                return sc[nm][0], sc[nm][1]
            x_, bx_ = sct("x"); ax_, bax_ = sct("ax"); e_, be_ = sct("e"); l_, bl_ = sct("l"); g_, bg_ = sct("g")
            beta_, bbeta_ = sct("beta"); gc_, bgc_ = sct("gc"); gtot_, bgtot_ = sct("gtot"); eg_, beg_ = sct("eg")
            nbeg_, bnbeg_ = sct("nbeg"); ekd_, bekd_ = sct("ekd"); glast_, bglast_ = sct("glast")
            nbeta_, bnbeta_ = sct("nbeta"); tmp_, btmp_ = sct("tmp")
            op("dve", lambda e, tm=tm: e.tensor_scalar(out=x_[:], in0=tm[:, :, 128], scalar1=misc[:, 1:2], scalar2=None,
                                                op0=ALU.add), reads=[btm, bmisc], writes=[bx_])
            op("dve", lambda e: e.scalar_tensor_tensor(out=ax_[:], in0=x_[:], scalar=-1.0, in1=x_[:], op0=ALU.mult,
                                                       op1=ALU.max), reads=[bx_], writes=[bax_])
            op("act", lambda e: e.activation(out=e_[:], in_=ax_[:], func=AF.Exp, scale=-1.0), reads=[bax_], writes=[be_])
            op("act", lambda e: e.activation(out=l_[:], in_=e_[:], func=AF.Ln, bias=onesf[:, 0:1]), reads=[be_, bones], writes=[bl_])
            op("dve", lambda e: e.scalar_tensor_tensor(out=g_[:], in0=x_[:], scalar=0.0, in1=l_[:], op0=ALU.max,
                                                       op1=ALU.add), reads=[bx_, bl_], writes=[bg_])
            op("dve", lambda e: e.tensor_scalar(out=g_[:], in0=g_[:], scalar1=negA[:, 0:1], scalar2=None, op0=ALU.mult),
               reads=[bg_, bnegA], writes=[bg_])
            op("act", lambda e, tm=tm: e.activation(out=beta_[:], in_=tm[:, :, 129], func=AF.Sigmoid), reads=[btm], writes=[bbeta_])
            psg, pgb = preg(6, 384, 8)
            op("pe", lambda e: e.matmul(psg[:, 0:4], lhsT=triu[:], rhs=g_[:], start=True, stop=True),
               reads=[btriu, bg_], writes=pgb)
            op("pe", lambda e: e.matmul(psg[:, 4:8], lhsT=onesf[:], rhs=g_[:], start=True, stop=True),
               reads=[bones, bg_], writes=pgb)
            op("dve", lambda e: e.tensor_copy(out=gc_[:], in_=psg[:, 0:4]), reads=pgb, writes=[bgc_])
            op("dve", lambda e: e.tensor_copy(out=gtot_[:], in_=psg[:, 4:8]), reads=pgb, writes=[bgtot_])
            op("act", lambda e: e.activation(out=eg_[:], in_=gc_[:], func=AF.Exp), reads=[bgc_], writes=[beg_])
            op("act", lambda e: e.activation(out=glast_[:], in_=gtot_[:], func=AF.Exp), reads=[bgtot_], writes=[bglast_])
            op("dve", lambda e: e.tensor_tensor(out=tmp_[:], in0=gtot_[:], in1=gc_[:], op=ALU.subtract),
               reads=[bgtot_, bgc_], writes=[btmp_])
            op("act", lambda e: e.activation(out=ekd_[:], in_=tmp_[:], func=AF.Exp), reads=[btmp_], writes=[bekd_])
            op("dve", lambda e: e.tensor_scalar(out=nbeta_[:], in0=beta_[:], scalar1=-1.0, scalar2=None, op0=ALU.mult),
               reads=[bbeta_], writes=[bnbeta_])
            op("dve", lambda e: e.tensor_tensor(out=nbeg_[:], in0=nbeta_[:], in1=eg_[:], op=ALU.mult),
               reads=[bnbeta_, beg_], writes=[bnbeg_])

            qTn, bqTn = qkn[0]
            kTn, bkTn = qkn[1]

            def g4(nm):
                return G4[nm][0], G4[nm][1]
            dg, bdg = g4("dg"); t1, bt1 = g4("t1"); E1, bE1 = g4("E1"); E2, bE2 = g4("E2"); Erow, bErow = g4("Erow")
            M, bM = g4("M"); MT, bMT = g4("MT"); X, bX = g4("X"); XT, bXT = g4("XT"); AqkT, bAqkT = g4("AqkT")
            kdec, bkdec = g4("kdec"); ATm, bATm = g4("ATm"); qd, bqd = g4("qd"); QeffT, bQeffT = g4("QeffT")
            zs, bzs = g4("zs"); yb, byb = g4("yb"); junk, bjunk = g4("junk")
            R_, bR = G4["R"]

            def v4(bank):
                return pb[bank][:].rearrange("p (s c) -> p s c", s=4)
            for s in range(4):
                op("dve", lambda e, s=s: e.tensor_scalar(out=dg[:, s, :], in0=identf[:], scalar1=gc_[:, s:s + 1], scalar2=None,
                                                         op0=ALU.mult), reads=[bident, bgc_], writes=[bdg])
            for s in range(4):
                op("pe", lambda e, s=s: e.matmul(pb[g3][:, s * 128:(s + 1) * 128], lhsT=dg[:, s, :], rhs=onesf[:],
                                                 start=(s == 0), stop=False, skip_group_check=True),
                   reads=[bdg, bones], writes=[pq[g3][0]])
            for s in range(4):
                op("pe", lambda e, s=s: e.matmul(pb[g3][:, s * 128:(s + 1) * 128], lhsT=nonesf[:], rhs=dg[:, s, :],
                                                 start=False, stop=(s == 3), skip_group_check=True),
                   reads=[bdg, bnones], writes=[pq[g3][0]])
            for s in range(4):
                op("pe", lambda e, s=s: e.matmul(pb[g4b][:, s * 128:(s + 1) * 128], lhsT=onesf[:], rhs=dg[:, s, :],
                                                 start=(s == 0), stop=(s == 3), skip_group_check=True),
                   reads=[bdg, bones], writes=[pq[g4b][0]])
            op("dve", lambda e: e.tensor_scalar(out=t1[:], in0=v4(g3), scalar1=0.0, scalar2=None, op0=ALU.min),
               reads=[pq[g3][0]], writes=[bt1])
            op("act", lambda e: e.activation(out=E1[:], in_=t1[:], func=AF.Exp), reads=[bt1], writes=[bE1])
            op("pool", lambda e: e.affine_select(out=E1[:], in_=E1[:], pattern=[[0, 4], [-1, 128]], compare_op=ALU.is_gt,
                                                 fill=0.0, base=0, channel_multiplier=1), reads=[bE1], writes=[bE1])
            op("dve", lambda e: e.tensor_scalar(out=t1[:], in0=v4(g3), scalar1=0.0, scalar2=None, op0=ALU.max),
               reads=[pq[g3][0]], writes=[bt1])
            op("act", lambda e: e.activation(out=E2[:], in_=t1[:], func=AF.Exp, scale=-1.0), reads=[bt1], writes=[bE2])
            op("pool", lambda e: e.affine_select(out=E2[:], in_=E2[:], pattern=[[0, 4], [1, 128]], compare_op=ALU.is_ge,
                                                 fill=0.0, base=0, channel_multiplier=-1), reads=[bE2], writes=[bE2])
            op("act", lambda e: e.activation(out=Erow[:], in_=v4(g4b), func=AF.Exp), reads=[pq[g4b][0]], writes=[bErow])
            for s in range(4):
                cs = slice(s * 128, (s + 1) * 128)
                op("pe", lambda e, s=s, cs=cs: e.matmul(pb[g5][:, cs], lhsT=kTn[:, cs], rhs=kTn[:, cs], start=(s == 0),
                                                        stop=(s == 3), skip_group_check=True), reads=[bkTn], writes=[pq[g5][0]])
            for s in range(4):
                cs = slice(s * 128, (s + 1) * 128)
                op("pe", lambda e, s=s, cs=cs: e.matmul(pb[g6][:, cs], lhsT=kTn[:, cs], rhs=qTn[:, cs], start=(s == 0),
                                                        stop=(s == 3), skip_group_check=True), reads=[bkTn, bqTn], writes=[pq[g6][0]])
            for s in range(4):
                op("dve", lambda e, s=s: e.scalar_tensor_tensor(out=M[:, s, :], in0=pb[g5][:, s * 128:(s + 1) * 128],
                                                                scalar=nbeta_[:, s:s + 1], in1=E1[:, s, :], op0=ALU.mult,
                                                                op1=ALU.mult), reads=[pq[g5][0], bnbeta_, bE1], writes=[bM])
            op("dve", lambda e: e.tensor_tensor(out=AqkT[:], in0=v4(g6), in1=E2[:], op=ALU.mult), reads=[pq[g6][0], bE2], writes=[bAqkT])
            for s in range(4):
                op("pe", lambda e, s=s: e.transpose(pb[g7][:, s * 128:(s + 1) * 128], M[:, s, :], identf[:]),
                   reads=[bM, bident], writes=[pq[g7][0]])
            op("act", lambda e: e.activation(out=MT[:], in_=v4(g7), func=AF.Copy), reads=[pq[g7][0]], writes=[bMT])
            for s in range(4):
                cs = slice(s * 128, (s + 1) * 128)
                op("pe", lambda e, cs=cs: e.transpose(pb[g3][:, cs], kTn[:, cs], identf[:]), reads=[bkTn, bident], writes=[pq[g3][0]])
            for s in range(4):
                cs = slice(s * 128, (s + 1) * 128)
                op("pe", lambda e, cs=cs: e.transpose(pb[g4b][:, cs], cout[:, 2, cs], identf[:]), reads=[bcout, bident], writes=[pq[g4b][0]])
            for s in range(4):
                cs = slice(s * 128, (s + 1) * 128)
                op("dve", lambda e, s=s, cs=cs: e.tensor_scalar(out=kdec[:, s, :], in0=pb[g3][:, cs], scalar1=ekd_[:, s:s + 1],
                                                                scalar2=None, op0=ALU.mult), reads=[pq[g3][0], bekd_], writes=[bkdec])
                op("dve", lambda e, s=s, cs=cs: e.tensor_scalar(out=R_[:, s, 128:256], in0=pb[g3][:, cs], scalar1=nbeg_[:, s:s + 1],
                                                                scalar2=None, op0=ALU.mult), reads=[pq[g3][0], bnbeg_], writes=[bR])
                op("dve", lambda e, s=s, cs=cs: e.tensor_scalar(out=R_[:, s, 0:128], in0=pb[g4b][:, cs], scalar1=beta_[:, s:s + 1],
                                                                scalar2=None, op0=ALU.mult), reads=[pq[g4b][0], bbeta_], writes=[bR])
            Xc, bXc, XTc, bXTc = M, bM, MT, bMT
            for it in range(GDN_SQ + 1):
                for s in range(4):
                    bank = (g5, g6)[s // 2]
                    op("pe", lambda e, s=s, bank=bank, XTc=XTc: e.matmul(
                        pb[bank][:, (s % 2) * 256:(s % 2 + 1) * 256], lhsT=XTc[:, s, :], rhs=R_[:, s, :],
                        start=(s % 2 == 0), stop=(s % 2 == 1), skip_group_check=True), reads=[bXTc, bR], writes=[pq[bank][0]])
                for hb_ in range(2):
                    op("dve", lambda e, hb_=hb_: e.tensor_tensor(
                        out=R_[:, 2 * hb_:2 * hb_ + 2, :], in0=R_[:, 2 * hb_:2 * hb_ + 2, :],
                        in1=pb[(g5, g6)[hb_]][:].rearrange("p (s c) -> p s c", s=2), op=ALU.add),
                        reads=[bR, pq[(g5, g6)[hb_]][0]], writes=[bR])
                if it < GDN_SQ:
                    if it % 2 == 0:
                        Xn, bXn, XTn, bXTn = X, bX, XT, bXT
                    else:
                        Xn, bXn, XTn, bXTn = M, bM, MT, bMT
                    for s in range(4):
                        op("pe", lambda e, s=s, Xc=Xc, XTc=XTc: e.matmul(
                            pb[g7][:, s * 128:(s + 1) * 128], lhsT=XTc[:, s, :], rhs=Xc[:, s, :], start=(s == 0), stop=(s == 3),
                            skip_group_check=True), reads=[bXc, bXTc], writes=[pq[g7][0]])
                    op("act", lambda e, Xn=Xn: e.activation(out=Xn[:], in_=v4(g7), func=AF.Copy), reads=[pq[g7][0]], writes=[bXn])
                    for s in range(4):
                        op("pe", lambda e, s=s, Xc=Xc, XTc=XTc: e.matmul(
                            pb[g7][:, s * 128:(s + 1) * 128], lhsT=Xc[:, s, :], rhs=XTc[:, s, :], start=(s == 0), stop=(s == 3),
                            skip_group_check=True), reads=[bXc, bXTc], writes=[pq[g7][0]])
                    op("dve", lambda e, XTn=XTn: e.tensor_copy(out=XTn[:], in_=v4(g7)), reads=[pq[g7][0]], writes=[bXTn])
                    Xc, bXc, XTc, bXTc = Xn, bXn, XTn, bXTn
            for s in range(4):
                op("pe", lambda e, s=s: e.matmul(pb[g4b][:, s * 128:(s + 1) * 128], lhsT=R_[:, s, 128:256], rhs=kdec[:, s, :],
                                                 start=(s == 0), stop=(s == 3), skip_group_check=True),
                   reads=[bR, bkdec], writes=[pq[g4b][0]])
            for s in range(4):
                op("pe", lambda e, s=s: e.matmul(pb[g7][:, s * 128:(s + 1) * 128], lhsT=R_[:, s, 128:256], rhs=AqkT[:, s, :],
                                                 start=(s == 0), stop=(s == 3), skip_group_check=True),
                   reads=[bR, bAqkT], writes=[pq[g7][0]])
            for s in range(4):
                op("dve", lambda e, s=s: e.scalar_tensor_tensor(out=ATm[:, s, :], in0=identf[:], scalar=glast_[:, s:s + 1],
                                                                in1=pb[g4b][:, s * 128:(s + 1) * 128], op0=ALU.mult, op1=ALU.add),
                   reads=[bident, bglast_, pq[g4b][0]], writes=[bATm])
            op("pool", lambda e: e.tensor_tensor(out=qd[:], in0=qTn[:].rearrange("p (s c) -> p s c", s=4), in1=Erow[:],
                                                 op=ALU.mult), reads=[bqTn, bErow], writes=[bqd])
            op("dve", lambda e: e.tensor_tensor(out=QeffT[:], in0=qd[:], in1=v4(g7), op=ALU.add), reads=[bqd, pq[g7][0]], writes=[bQeffT])
            op("act", lambda e, tm=tm: e.activation(out=zs[:], in_=tm[:, :, 0:128], func=AF.Silu), reads=[btm], writes=[bzs])
            op("pool", lambda e: e.tensor_tensor(out=zs[:], in0=zs[:], in1=nrm4[:], op=ALU.mult), reads=[bzs, bnrm4], writes=[bzs])
            ss, bss = sc["ss4"]
            for s in range(4):
                n = 4 * j + s
                Scur, bScur = Sst[n % 2]
                Snxt, bSnxt = Sst[(n + 1) % 2]
                op("pe", lambda e, s=s, Scur=Scur: e.matmul(pb[g5][:, s * 128:(s + 1) * 128], lhsT=QeffT[:, s, :], rhs=Scur[:],
                                                            start=(s == 0), stop=False, skip_group_check=True),
                   reads=[bQeffT, bScur], writes=[pq[g5][0]])
                op("pe", lambda e, s=s: e.matmul(pb[g5][:, s * 128:(s + 1) * 128], lhsT=AqkT[:, s, :], rhs=R_[:, s, 0:128],
                                                 start=False, stop=True, skip_group_check=True),
                   reads=[bAqkT, bR], writes=[pq[g5][0]])
                op("pe", lambda e, s=s: e.matmul(pb[g6][:, 0:128], lhsT=kdec[:, s, :], rhs=R_[:, s, 0:128], start=True, stop=False),
                   reads=[bkdec, bR], writes=[pq[g6][0]])
                op("pe", lambda e, s=s, Scur=Scur: e.matmul(pb[g6][:, 0:128], lhsT=ATm[:, s, :], rhs=Scur[:], start=False, stop=True),
                   reads=[bATm, bScur], writes=[pq[g6][0]])
                op("act", lambda e, Snxt=Snxt: e.activation(out=Snxt[:], in_=pb[g6][:, 0:128], func=AF.Copy),
                   reads=[pq[g6][0]], writes=[bSnxt])
            for s in range(4):
                op("act", lambda e, s=s: e.activation(out=junk[:, 0, :], in_=pb[g5][:, s * 128:(s + 1) * 128], func=AF.Square,
                                                      accum_out=ss[:, s:s + 1]), reads=[pq[g5][0]], writes=[bjunk, bss])
            rr, brr = sc["rr4"]
            op("dve", lambda e: e.tensor_scalar(out=rr[:], in0=ss[:], scalar1=1.0 / 128, scalar2=1e-6, op0=ALU.mult, op1=ALU.add),
               reads=[bss], writes=[brr])
            op("act", lambda e: e.activation(out=rr[:], in_=rr[:], func=AF.Ln), reads=[brr], writes=[brr])
            op("act", lambda e: e.activation(out=rr[:], in_=rr[:], func=AF.Exp, scale=-0.5), reads=[brr], writes=[brr])
            for s in range(4):
                op("dve", lambda e, s=s: e.scalar_tensor_tensor(out=yb[:, s, :], in0=pb[g5][:, s * 128:(s + 1) * 128],
                                                                scalar=rr[:, s:s + 1], in1=zs[:, s, :], op0=ALU.mult, op1=ALU.mult),
                   reads=[pq[g5][0], brr, bzs], writes=[byb])
            for s in range(4):
                op("pe", lambda e, s=s: e.transpose(pb[g3][:, s * 128:(s + 1) * 128], yb[:, s, :], identf[:]),
                   reads=[byb, bident], writes=[pq[g3][0]])
            op("act", lambda e: e.activation(out=ystage[1][0][:], in_=pb[g3][:], func=AF.Copy), reads=[pq[g3][0]], writes=[ystage[1][1]])
            dma("sp", lambda e, t0=t0: e.dma_start(out=dr["ybT"][:, t0:t0 + 512], in_=ystage[1][0][:]),
                  reads=[ystage[1][1]], writes=[b_out], sem_buf=ystage[1][1], same_gen=True)

        cur[0] = Al[j]
        stream[0] = "attn"
        if do_attn:
            oacc = {}
            slots = [(4, 0), (4, 129), (4, 258), (5, 0), (5, 129), (5, 258), (6, 0), (6, 129)]
            for m in range(2):
                for s in range(4):
                    oacc[(m, s)] = slots[m * 4 + s]
            n_k = 4 * j + 4
            it = 0
            for i in range(n_k):
                s0 = max(0, i - 4 * j)
                q0 = s0 * 128
                for m in range(2):
                    bank = 7 if (it % 2 == 0) else 3
                    it += 1
                    pst, bpst = preg(bank, q0, 512 - q0)
                    ms = slice(m * 64, (m + 1) * 64)
                    near = [s for s in range(s0, 4) if (4 * j + s) - i <= 1]
                    op("pe", lambda e, pst=pst, ms=ms, i=i, q0=q0, QTc=QTc, near=near: e.matmul(
                        pst, lhsT=KT[ms, i * 128:(i + 1) * 128], rhs=QTc[ms, q0:512], start=True, stop=(len(near) == 0),
                        skip_group_check=True),
                        reads=[bKT[i // 4], bQTc], writes=bpst)
                    for idx, s in enumerate(near):
                        typ = 0 if (4 * j + s) == i else 1
                        op("pe", lambda e, bank=BMAP["attn"][bank], s=s, typ=typ, last=(idx == len(near) - 1): e.matmul(
                            pb[bank][:, s * 128:(s + 1) * 128], lhsT=identf[:], rhs=BT[:, typ, :], start=False, stop=last,
                            skip_group_check=True), reads=[bident, bBT], writes=bpst)
                    ptt, bptt = PT[pt_ctr[0] % 4]
                    pt_ctr[0] += 1
                    op("act", lambda e, pst=pst, ptt=ptt, q0=q0: e.activation(out=ptt[:, q0:512], in_=pst, func=AF.Exp,
                                                                              bias=misc[:, 2:3]),
                       reads=bpst + [bmisc], writes=[bptt])
                    for s in range(s0, 4):
                        bk, c0 = oacc[(m, s)]
                        po, bpo = preg(bk, c0, 129)
                        last_k = 4 * j + s
                        op("pe", lambda e, po=po, ptt=ptt, s=s, i=i, last_k=last_k, c0=c0: e.matmul(
                            po, lhsT=ptt[:, s * 128:(s + 1) * 128], rhs=VA[:, i, 0:129], start=(i == 0 and c0 == 0), stop=(i == last_k),
                            skip_group_check=True), reads=[bptt, bVA[i]], writes=bpo)
            for s in range(4):
                b1, c1 = oacc[(0, s)]
                b2, c2 = oacc[(1, s)]
                po1, bpo1 = preg(b1, c1, 129)
                po2, bpo2 = preg(b2, c2, 129)
                r1, br1 = rr_t["r1"]; r2, br2 = rr_t["r2"]; ss2, bss2 = rr_t["ss2"]; rr2, brr2 = rr_t["rr2"]
                op("dve", lambda e, po1=po1: e.reciprocal(out=r1[:], in_=po1[:, 128:129]), reads=bpo1, writes=[br1])
                op("dve", lambda e, po2=po2: e.reciprocal(out=r2[:], in_=po2[:, 128:129]), reads=bpo2, writes=[br2])
                op("dve", lambda e: e.tensor_tensor(out=r2[:], in0=r2[:], in1=lam[:], op=ALU.mult), reads=[br2, blam], writes=[br2])
                op("dve", lambda e, po2=po2: e.tensor_scalar(out=at_t2[:], in0=po2[:, 0:128], scalar1=r2[:, 0:1], scalar2=None,
                                                             op0=ALU.mult), reads=bpo2 + [br2], writes=[bat_t2])
                op("dve", lambda e, po1=po1: e.scalar_tensor_tensor(out=at_a[:], in0=po1[:, 0:128], scalar=r1[:, 0:1],
                                                                    in1=at_t2[:], op0=ALU.mult, op1=ALU.subtract),
                   reads=bpo1 + [br1, bat_t2], writes=[bat_a])
                op("act", lambda e: e.activation(out=at_junk[:], in_=at_a[:], func=AF.Square, accum_out=ss2[:]),
                   reads=[bat_a], writes=[bat_junk, bss2])
                op("dve", lambda e: e.tensor_scalar(out=rr2[:], in0=ss2[:], scalar1=1.0 / 128, scalar2=1e-6, op0=ALU.mult,
                                                    op1=ALU.add), reads=[bss2], writes=[brr2])
                op("act", lambda e: e.activation(out=rr2[:], in_=rr2[:], func=AF.Ln), reads=[brr2], writes=[brr2])
                op("act", lambda e: e.activation(out=rr2[:], in_=rr2[:], func=AF.Exp, scale=-0.5), reads=[brr2], writes=[brr2])
                op("dve", lambda e: e.scalar_tensor_tensor(out=at_y[:], in0=at_a[:], scalar=rr2[:, 0:1], in1=nrm[:, 1, :],
                                                           op0=ALU.mult, op1=ALU.mult), reads=[bat_a, brr2, bnrm], writes=[bat_y])
                pY, bpY = preg(6, 258, 128)
                op("pe", lambda e, pY=pY: e.transpose(pY, at_y[:], identf[:]), reads=[bat_y, bident], writes=bpY)
                op("act", lambda e, pY=pY, s=s: e.activation(out=ystage[0][0][:, s * 128:(s + 1) * 128], in_=pY, func=AF.Copy),
                   reads=bpY, writes=[ystage[0][1]])
            dma("sp", lambda e, t0=t0: e.dma_start(out=dr["yaT"][:, t0:t0 + 512], in_=ystage[0][0][:]),
                  reads=[ystage[0][1]], writes=[b_out], sem_buf=ystage[0][1], same_gen=True)
    flush(Pl[0])
    for j in range(n_tiles):
        nxt = Pl[j + 1] if j + 1 < n_tiles else []
        flush(merge(Gl[j], Al[j] + nxt))
    return [b_out]


def host_prep_a(inp):
    x = inp["x"][0]
    w_in = inp["w_in"][0]
    xT = np.ascontiguousarray(x.T)
    conv_w = inp["conv_w"][0]
    table = inp["rel_bias_table"]
    nb = 16
    ki = np.arange(128)[:, None]
    qi = np.arange(128)[None, :]

    def bucket(rel):
        base = np.where(rel > 0, nb, 0)
        n = np.abs(rel)
        max_exact = nb // 2
        nf = np.maximum(n, 1).astype(np.float32)
        large = max_exact + (np.log(nf / np.float32(max_exact)) / np.float32(np.log(128 / max_exact))
                             * np.float32(nb - max_exact)).astype(np.int32)
        large = np.minimum(large, nb - 1)
        return base + np.where(n < max_exact, n, large)
    bk = np.stack([bucket(ki - qi), bucket(ki - 128 - qi)], axis=1)
    maps = []
    for h in range(NCORE):
        cols = np.concatenate([
            np.arange(h * 128, h * 128 + 128),
            1024 + np.arange(h * 128, h * 128 + 128),
            3072 + np.arange(h * 128, h * 128 + 128),
            4096 + np.arange(h * 128, h * 128 + 128),
            5120 + np.arange(h * 128, h * 128 + 128),
            2048 + np.arange(h * 128, h * 128 + 128),
            6144 + np.arange(h * 128, h * 128 + 128),
            np.array([7168 + h, 7176 + h]),
        ])
        wA = np.ascontiguousarray(w_in[:, cols])
        misc = np.zeros((128, 8), np.float32)
        misc[:, 0] = inp["gdn_a_log"][0, h]
        misc[:, 1] = inp["gdn_dt_bias"][0, h]
        misc[:, 2] = table[15, h]
        cw = np.stack([conv_w[:, i * 1024 + h * 128:i * 1024 + (h + 1) * 128].T for i in range(3)], axis=1)
        nrmw = np.stack([np.broadcast_to(inp["gdn_norm_w"][0], (128, 128)),
                         np.broadcast_to(inp["diff_subln_w"][0], (128, 128))], axis=1)
        lamr = np.broadcast_to(inp["diff_lambda"][0].reshape(1, 2, 2, 64), (128, 2, 2, 64))
        biasT = table[:, h][bk]
        maps.append({"xT": xT, "wA": wA, "miscA": misc, "convw": np.ascontiguousarray(cw, dtype=np.float32),
                     "nrmw": np.ascontiguousarray(nrmw, dtype=np.float32),
                     "lam": np.ascontiguousarray(lamr, dtype=np.float32),
                     "biasT": np.ascontiguousarray(biasT, dtype=np.float32)})
    return maps


def build_nc_a(n_tiles=NT_A, do_gdn=True, do_attn=True, max_ops=10 ** 9):
    nc = bass.Bass("TRN2", target_bir_lowering=False)
    dr = {}
    dr["xT"] = nc.dram_tensor("xT", [D, T], F32, kind="ExternalInput").ap()
    dr["wA"] = nc.dram_tensor("wA", [D, 898], F32, kind="ExternalInput").ap()
    dr["miscA"] = nc.dram_tensor("miscA", [128, 8], F32, kind="ExternalInput").ap()
    dr["convw"] = nc.dram_tensor("convw", [128, 3, 4], F32, kind="ExternalInput").ap()
    dr["nrmw"] = nc.dram_tensor("nrmw", [128, 2, 128], F32, kind="ExternalInput").ap()
    dr["lam"] = nc.dram_tensor("lam", [128, 2, 2, 64], F32, kind="ExternalInput").ap()
    dr["biasT"] = nc.dram_tensor("biasT", [128, 2, 128], F32, kind="ExternalInput").ap()
    dr["yaT"] = nc.dram_tensor("yaT", [128, T], BF16, kind="ExternalOutput").ap()
    dr["ybT"] = nc.dram_tensor("ybT", [128, T], BF16, kind="ExternalOutput").ap()
    S = Sched(nc)
    S.max_ops = max_ops
    outs = build_phase_a(nc, S, dr, n_tiles=n_tiles, do_gdn=do_gdn, do_attn=do_attn)
    S.wait_final("sp", outs)
    S.emit()
    return nc, S


TPC = T // NCORE
ALPHA = 2.0 ** 0.25
N_EXP = 64


def build_phase_b(nc, S, dr, n_exp=N_EXP, yall_bufs=()):
    C = Ctx(nc, S)
    op = S.op
    yall_bufs = list(yall_bufs)
    ARENA, _ = C.sb([128, 40960], BF16, "arena")
    bA = [Buf("arena0"), Buf("arena1"), Buf("arena2")]
    MT, _ = C.sb([128, 16, 1024], BF16, "MT")
    bMT = [Buf(f"MT{i}") for i in range(16)]
    ACC, _ = C.sb([128, 8, 2048], F32, "ACC")
    bACC = [Buf(f"ACC{i}") for i in range(8)]
    WGT = [C.sb([128, 4096], BF16, f"wgt{i}") for i in range(2)]
    tmpf = [C.sb([128, 512], F32, f"tmpf{i}") for i in range(2)]
    Gt, bGt = C.sb([128, 8, 64], F32, "Gt")
    identf, bident = C.sb([128, 128], F32, "identfB")
    onesB, bonesB = C.sb([128, 128], F32, "onesB")
    op("pool", lambda e: e.memset(onesB[:], 1.0), writes=[bonesB])
    op("pool", lambda e: e.affine_select(out=identf[:], in_=onesB[:], pattern=[[-1, 128]], compare_op=ALU.is_equal,
                                         fill=0.0, base=0, channel_multiplier=1), reads=[bonesB], writes=[bident])
    rb, brb = C.sb([128, 64], F32, "rbias")
    S.dma("sp", lambda e: e.dma_start(out=rb[:], in_=dr["rbias"]), writes=[brb], sem_buf=brb)
    wr, bwr = C.sb([128, 16, 64], F32, "wr")
    S.dma("sp", lambda e: e.dma_start(out=wr[:], in_=dr["w_router"].rearrange("(c p) n -> p c n", p=128)),
          writes=[bwr], sem_buf=bwr)
    epsc, bepsc = C.sb([128, 2], F32, "epscB")
    op("pool", lambda e: e.memset(epsc[:, 0:1], 1e-5), writes=[bepsc])
    small = {nm: C.sb([128, 8], F32, "smB_" + nm) for nm in ["m8", "gs", "g8", "gm", "t8", "den", "mv", "rstd", "nmr"]}
    st6, bst6 = C.sb([128, 4, 6], F32, "st6")
    ch, bch = C.sb([128, 64], F32, "choice")
    scs, bscs = C.sb([128, 64], F32, "scores")
    mc, bmc = C.sb([128, 64], F32, "mchoice")

    pb = [nc.alloc_psum_tensor(f"pbB{i}", [128, 512], F32) for i in range(8)]
    pq = [Buf(f"pbB{i}", excl=True) for i in range(8)]

    def AR(c0, n):
        return ARENA[:, c0:c0 + n]

    ACCb = ACC[:].rearrange("p t d -> p (t d)").bitcast(BF16)

    def XH(c0, n):
        return ACCb[:, c0:c0 + n]

    wba_v = dr["w_branch_a"].rearrange("(c p) n -> p c n", p=128)
    wbb_v = dr["w_branch_b"].rearrange("(c p) n -> p c n", p=128)
    for c in range(8):
        S.dma("pool", lambda e, c=c: e.dma_start(out=AR(c * 2048, 2048), in_=wba_v[:, c, :]),
              writes=[bA[0]], sem_buf=bA[0], same_gen=True)
    for c in range(8):
        S.dma("pool", lambda e, c=c: e.dma_start(out=AR(16384 + c * 2048, 2048), in_=wbb_v[:, c, :]),
              writes=[bA[1]], sem_buf=bA[1], same_gen=True)
    xTs = dr["xTs"].rearrange("(c p) t -> p c t", p=128)
    yall = dr["yall"].rearrange("(r p) t -> p r t", p=128)
    tok0 = dr["tok0"]
    bX = Buf("xhalf")
    bY = Buf("yhalf")
    for half in range(2):
        for c in range(16):
            S.dma("pool", lambda e, c=c, half=half: e.dma_start(
                out=XH(c * 512, 512), in_=xTs[:, c, half * 512:(half + 1) * 512]),
                writes=[bX], sem_buf=bX, same_gen=(c > 0))
        for r in range(16):
            S.dma("sp", lambda e, r=r, half=half: e.dma_start(
                out=XH(8192 + r * 512, 512), in_=yall[:, r, tok0 + half * 512:tok0 + (half + 1) * 512]),
                reads=yall_bufs, writes=[bY], sem_buf=bY, same_gen=(r > 0))
        for m in range(16):
            wg, bwg = WGT[m % 2]
            S.dma("pool", lambda e, wg=wg, m=m: e.dma_start(out=wg[:], in_=dr["wgates"][m]),
                  writes=[bwg], sem_buf=bwg)
            for ab in range(2):
                for c in range(16):
                    op("pe", lambda e, ab=ab, c=c, wg=wg: e.matmul(
                        pb[ab][:], lhsT=wg[:, c * 256 + ab * 128:c * 256 + ab * 128 + 128], rhs=XH(c * 512, 512),
                        start=(c == 0), stop=(c == 15)), reads=[bwg, bX], writes=[pq[ab]])
            for ab in range(2):
                for hh in range(8):
                    op("pe", lambda e, ab=ab, hh=hh, m=m: e.matmul(
                        pb[2 + ab][:], lhsT=AR(ab * 16384 + hh * 2048 + m * 128, 128),
                        rhs=XH(8192 + (2 * hh + ab) * 512, 512), start=(hh == 0), stop=(hh == 7)),
                        reads=[bA[ab], bY], writes=[pq[2 + ab]])
            t0_, bt0 = tmpf[0]; t1_, bt1 = tmpf[1]
            op("act", lambda e: e.activation(out=t0_[:], in_=pb[0][:], func=AF.Sigmoid), reads=[pq[0]], writes=[bt0])
            op("act", lambda e: e.activation(out=t1_[:], in_=pb[1][:], func=AF.Sigmoid), reads=[pq[1]], writes=[bt1])
            op("dve", lambda e: e.tensor_tensor(out=t0_[:], in0=t0_[:], in1=pb[2][:], op=ALU.mult),
               reads=[bt0, pq[2]], writes=[bt0])
            op("dve", lambda e: e.tensor_tensor(out=t1_[:], in0=t1_[:], in1=pb[3][:], op=ALU.mult),
               reads=[bt1, pq[3]], writes=[bt1])
            op("pool", lambda e, m=m, half=half: e.tensor_tensor(out=MT[:, m, half * 512:(half + 1) * 512], in0=t0_[:],
                                                                 in1=t1_[:], op=ALU.add), reads=[bt0, bt1], writes=[bMT[m]])

    wout_v = dr["w_out"].rearrange("(c p) n -> p c n", p=128)
    bWO = Buf("wout")
    for c in range(16):
        S.dma("pool", lambda e, c=c: e.dma_start(out=AR(c * 2048, 2048), in_=wout_v[:, c, :]),
              reads=[], writes=[bA[0], bA[1], bWO], sem_buf=bWO, same_gen=(c > 0))
    lnp = ARENA[:, 32768:40960].bitcast(F32).rearrange("p (a d) -> p a d", a=2)
    blnp = Buf("lnp")
    S.dma("sp", lambda e: e.dma_start(out=lnp, in_=dr["ln1"]), reads=[], writes=[blnp], sem_buf=blnp)
    xrows = dr["xrows"].rearrange("(t p) d -> p t d", p=128)
    for tt in range(8):
        S.dma("sp", lambda e, tt=tt: e.dma_start(out=ACC[:, tt, :], in_=xrows[:, tt, :]),
              writes=[bACC[tt], bX, bY], sem_buf=bACC[tt], same_gen=(tt > 0))
    hb = 0
    for tt in range(8):
        for dg in range(4):
            bank = 4 + (hb % 2)
            hb += 1
            for m in range(16):
                op("pe", lambda e, bank=bank, m=m, tt=tt, dg=dg: e.matmul(
                    pb[bank][:], lhsT=MT[:, m, tt * 128:(tt + 1) * 128], rhs=AR(m * 2048 + dg * 512, 512),
                    start=(m == 0), stop=(m == 15)), reads=[bMT[m], bWO], writes=[pq[bank]])
            op("dve", lambda e, bank=bank, tt=tt, dg=dg: e.scalar_tensor_tensor(
                out=ACC[:, tt, dg * 512:(dg + 1) * 512], in0=ACC[:, tt, dg * 512:(dg + 1) * 512], scalar=ALPHA,
                in1=pb[bank][:], op0=ALU.mult, op1=ALU.add), reads=[bACC[tt], pq[bank]], writes=[bACC[tt]])

    def layer_norm(tt, prm, bprm):
        mv, bmv = small["mv"]; rstd, brstd = small["rstd"]
        for q in range(4):
            op("dve", lambda e, q=q, tt=tt: e.bn_stats(out=st6[:, q, :], in_=ACC[:, tt, q * 512:(q + 1) * 512]),
               reads=[bACC[tt]], writes=[bst6])
        op("dve", lambda e: e.bn_aggr(out=mv[:, 0:2], in_=st6[:].rearrange("p a b -> p (a b)")), reads=[bst6], writes=[bmv])
        op("act", lambda e: e.activation(out=rstd[:, 0:1], in_=mv[:, 1:2], func=AF.Ln, bias=epsc[:, 0:1]),
           reads=[bmv, bepsc], writes=[brstd])
        op("act", lambda e: e.activation(out=rstd[:, 0:1], in_=rstd[:, 0:1], func=AF.Exp, scale=-0.5),
           reads=[brstd], writes=[brstd])
        op("dve", lambda e, tt=tt: e.tensor_scalar(out=ACC[:, tt, :], in0=ACC[:, tt, :], scalar1=mv[:, 0:1],
                                                   scalar2=rstd[:, 0:1], op0=ALU.subtract, op1=ALU.mult),
           reads=[bACC[tt], bmv, brstd], writes=[bACC[tt]])
        op("pool", lambda e, tt=tt: e.tensor_tensor(out=ACC[:, tt, :], in0=ACC[:, tt, :], in1=prm[:, 0, :], op=ALU.mult),
           reads=[bACC[tt], bprm], writes=[bACC[tt]])
        op("pool", lambda e, tt=tt: e.tensor_tensor(out=ACC[:, tt, :], in0=ACC[:, tt, :], in1=prm[:, 1, :], op=ALU.add),
           reads=[bACC[tt], bprm], writes=[bACC[tt]])

    for tt in range(8):
        layer_norm(tt, lnp, blnp)
        xf, bX1F = WGT[tt % 2]
        X1F = xf[:].bitcast(F32)
        for c4 in range(4):
            bank = 6 + (c4 % 2)
            for k in range(4):
                c = c4 * 4 + k
                op("pe", lambda e, bank=bank, k=k, c=c, tt=tt: e.transpose(
                    pb[bank][:, k * 128:(k + 1) * 128], ACC[:, tt, c * 128:(c + 1) * 128], identf[:]),
                    reads=[bACC[tt], bident], writes=[pq[bank]])
            op("act", lambda e, bank=bank, c4=c4, tt=tt: e.activation(
                out=MT[:, c4 * 4:(c4 + 1) * 4, tt * 128:(tt + 1) * 128],
                in_=pb[bank][:].rearrange("p (k t) -> p k t", k=4), func=AF.Copy),
                reads=[pq[bank]], writes=[bMT[c4 * 4 + k] for k in range(4)])
            op("dve", lambda e, bank=bank, c4=c4, X1F=X1F: e.tensor_copy(out=X1F[:, c4 * 512:(c4 + 1) * 512], in_=pb[bank][:]),
               reads=[pq[bank]], writes=[bX1F])
        for c in range(16):
            op("pe", lambda e, c=c, X1F=X1F: e.matmul(pb[0][:, 0:64], lhsT=X1F[:, c * 128:(c + 1) * 128], rhs=wr[:, c, :],
                                             start=(c == 0), stop=(c == 15)), reads=[bX1F, bwr], writes=[pq[0]])
        m8, bm8 = small["m8"]; gs, bgs = small["gs"]; g8, bg8 = small["g8"]; gm, bgm = small["gm"]
        t8, bt8 = small["t8"]; den, bden = small["den"]
        op("act", lambda e: e.activation(out=scs[:], in_=pb[0][:, 0:64], func=AF.Sigmoid), reads=[pq[0]], writes=[bscs])
        op("dve", lambda e: e.tensor_tensor(out=ch[:], in0=scs[:], in1=rb[:], op=ALU.add), reads=[bscs, brb], writes=[bch])
        for g in range(8):
            op("dve", lambda e, g=g: e.max(out=m8[:], in_=ch[:, g * 8:(g + 1) * 8]), reads=[bch], writes=[bm8])
            op("dve", lambda e, g=g: e.tensor_tensor(out=gs[:, g:g + 1], in0=m8[:, 0:1], in1=m8[:, 1:2], op=ALU.add),
               reads=[bm8], writes=[bgs])
        op("dve", lambda e: e.max(out=g8[:], in_=gs[:]), reads=[bgs], writes=[bg8])
        op("dve", lambda e: e.tensor_scalar(out=gm[:], in0=gs[:], scalar1=g8[:, 3:4], scalar2=None, op0=ALU.is_ge),
           reads=[bgs, bg8], writes=[bgm])
        op("dve", lambda e: e.tensor_scalar(out=gm[:], in0=gm[:], scalar1=-1.0, scalar2=1e30, op0=ALU.add, op1=ALU.mult),
           reads=[bgm], writes=[bgm])
        for g in range(8):
            op("dve", lambda e, g=g: e.tensor_scalar(out=mc[:, g * 8:(g + 1) * 8], in0=ch[:, g * 8:(g + 1) * 8],
                                                     scalar1=gm[:, g:g + 1], scalar2=None, op0=ALU.add),
               reads=[bch, bgm], writes=[bmc])
        op("dve", lambda e: e.max(out=t8[:], in_=mc[:]), reads=[bmc], writes=[bt8])
        op("dve", lambda e: e.tensor_scalar(out=mc[:], in0=mc[:], scalar1=t8[:, 7:8], scalar2=None, op0=ALU.is_ge),
           reads=[bmc, bt8], writes=[bmc])
        op("dve", lambda e: e.tensor_tensor(out=mc[:], in0=mc[:], in1=scs[:], op=ALU.mult), reads=[bmc, bscs], writes=[bmc])
        op("dve", lambda e: e.tensor_reduce(out=den[:, 0:1], in_=mc[:], axis=AX.X, op=ALU.add), reads=[bmc], writes=[bden])
        op("dve", lambda e: e.reciprocal(out=den[:, 0:1], in_=den[:, 0:1]), reads=[bden], writes=[bden])
        op("dve", lambda e, tt=tt: e.tensor_scalar(out=Gt[:, tt, :], in0=mc[:], scalar1=den[:, 0:1], scalar2=2.5,
                                                   op0=ALU.mult, op1=ALU.mult), reads=[bmc, bden], writes=[bGt])
        op("act", lambda e, tt=tt: e.activation(out=ACC[:, tt, :], in_=ACC[:, tt, :], func=AF.Copy, scale=ALPHA),
           reads=[bACC[tt]], writes=[bACC[tt]])

    bEW = [Buf("ew0"), Buf("ew1")]
    bWD = Buf("ewd")
    yb_ctr = 0
    for e_i in range(n_exp + 1):
        base = (e_i % 2) * 16384
        bew = bEW[e_i % 2]
        if e_i < n_exp:
            srcs = [dr["w_gate"][e_i].rearrange("(c p) f -> p c f", p=128), dr["w_up"][e_i].rearrange("(c p) f -> p c f", p=128),
                    dr["w_down"][e_i].rearrange("(c p) n -> p c n", p=128)]
        else:
            srcs = [dr["ws_gate"].rearrange("(c p) f -> p c f", p=128), dr["ws_up"].rearrange("(c p) f -> p c f", p=128),
                    dr["ws_down"].rearrange("(c p) n -> p c n", p=128)]
        extra_w = [bA[0], bA[1], bWO, blnp] if e_i < 2 else []
        for wi in range(2):
            dstv = ARENA[:, base + wi * 8192:base + (wi + 1) * 8192].rearrange("p (c f) -> p c f", c=16)
            S.dma("pool", lambda e, dstv=dstv, src=srcs[wi]: e.dma_start(out=dstv, in_=src),
                  writes=[bew] + extra_w, sem_buf=bew, same_gen=(wi > 0))
        dstv = ARENA[:, 32768:40960].rearrange("p (c n) -> p c n", c=4)
        S.dma("pool", lambda e, dstv=dstv, src=srcs[2]: e.dma_start(out=dstv, in_=src),
              writes=[bWD] + extra_w, sem_buf=bWD)
        hT, bhT = WGT[e_i % 2]
        sg_, bsg = tmpf[0]
        for half in range(2):
            for fc in range(4):
                for gu in range(2):
                    bank = gu * 2 + (fc % 2)
                    for c in range(16):
                        op("pe", lambda e, bank=bank, gu=gu, fc=fc, c=c, base=base, half=half: e.matmul(
                            pb[bank][:], lhsT=AR(base + gu * 8192 + c * 512 + fc * 128, 128),
                            rhs=MT[:, c, half * 512:(half + 1) * 512], start=(c == 0), stop=(c == 15)),
                            reads=[bew, bMT[c]], writes=[pq[bank]])
                bg_, bu_ = fc % 2, 2 + (fc % 2)
                op("act", lambda e, bg_=bg_: e.activation(out=sg_[:], in_=pb[bg_][:], func=AF.Silu), reads=[pq[bg_]], writes=[bsg])
                op("dve", lambda e, bu_=bu_, hT=hT, fc=fc, half=half: e.tensor_tensor(
                    out=hT[:, fc * 1024 + half * 512:fc * 1024 + (half + 1) * 512], in0=sg_[:], in1=pb[bu_][:], op=ALU.mult),
                    reads=[bsg, pq[bu_]], writes=[bhT])
        for tt in range(8):
            for dg in range(4):
                bank = 4 + (yb_ctr % 4)
                yb_ctr += 1
                for fc in range(4):
                    op("pe", lambda e, bank=bank, fc=fc, tt=tt, dg=dg, hT=hT, base=base: e.matmul(
                        pb[bank][:], lhsT=hT[:, fc * 1024 + tt * 128:fc * 1024 + (tt + 1) * 128],
                        rhs=AR(32768 + fc * 2048 + dg * 512, 512), start=(fc == 0), stop=(fc == 3)),
                        reads=[bhT, bWD], writes=[pq[bank]])
                if e_i < n_exp:
                    op("dve", lambda e, bank=bank, tt=tt, dg=dg, e_i=e_i: e.scalar_tensor_tensor(
                        out=ACC[:, tt, dg * 512:(dg + 1) * 512], in0=pb[bank][:], scalar=Gt[:, tt, e_i:e_i + 1],
                        in1=ACC[:, tt, dg * 512:(dg + 1) * 512], op0=ALU.mult, op1=ALU.add),
                        reads=[pq[bank], bGt, bACC[tt]], writes=[bACC[tt]])
                else:
                    op("dve", lambda e, bank=bank, tt=tt, dg=dg: e.tensor_tensor(
                        out=ACC[:, tt, dg * 512:(dg + 1) * 512], in0=pb[bank][:], in1=ACC[:, tt, dg * 512:(dg + 1) * 512],
                        op=ALU.add), reads=[pq[bank], bACC[tt]], writes=[bACC[tt]])

    ln2base = ((n_exp + 1) % 2) * 16384
    lnp2 = ARENA[:, ln2base:ln2base + 8192].bitcast(F32).rearrange("p (a d) -> p a d", a=2)
    blnp2 = Buf("lnp2")
    S.dma("sp", lambda e: e.dma_start(out=lnp2, in_=dr["ln2"]), writes=[bEW[(n_exp + 1) % 2], blnp2], sem_buf=blnp2)
    b_out = Buf("outB")
    outv = dr["out"].rearrange("(t p) d -> p t d", p=128)
    for tt in range(8):
        layer_norm(tt, lnp2, blnp2)
        S.dma("sp", lambda e, tt=tt: e.dma_start(out=outv[:, tt, :], in_=ACC[:, tt, :]), reads=[bACC[tt]], writes=[b_out],
              sem_buf=bACC[tt], same_gen=True)
    return [b_out]


def host_prep_b(inp, yall):
    x = inp["x"][0]
    w_in = inp["w_in"][0]
    ga = w_in[:, 7184:7184 + 2048].reshape(16, 128, 16, 128)
    gb = w_in[:, 9232:9232 + 2048].reshape(16, 128, 16, 128)
    wg = np.stack([ga, gb], axis=3)
    wgates = np.ascontiguousarray(wg.transpose(2, 1, 0, 3, 4).reshape(16, 128, 16 * 256))
    common = {
        "wgates": wgates, "yall": yall,
        "w_branch_a": inp["w_branch_a"][0], "w_branch_b": inp["w_branch_b"][0], "w_out": inp["w_out"][0],
        "ln1": np.ascontiguousarray(np.broadcast_to(np.stack([inp["ln1_g"][0], inp["ln1_b"][0]])[None], (128, 2, D))),
        "ln2": np.ascontiguousarray(np.broadcast_to(np.stack([inp["ln2_g"][0], inp["ln2_b"][0]])[None], (128, 2, D))),
        "rbias": np.ascontiguousarray(np.broadcast_to(inp["router_bias"][0][None], (128, 64))),
        "w_router": inp["w_router"][0],
        "w_gate": inp["w_gate"][0], "w_up": inp["w_up"][0], "w_down": inp["w_down"][0],
        "ws_gate": inp["ws_gate"][0], "ws_up": inp["ws_up"][0], "ws_down": inp["ws_down"][0],
    }
    maps = []
    xT = None
    for c in range(NCORE):
        xr = np.ascontiguousarray(x[c * TPC:(c + 1) * TPC])
        m = dict(common)
        m["xrows"] = xr
        m["xTs"] = np.ascontiguousarray(xr.T)
        maps.append(m)
    return maps


def declare_b(nc, dr, n_exp=N_EXP, fused=False):
    def din(name, shape, dt=F32):
        dr[name] = nc.dram_tensor(name, list(shape), dt, kind="ExternalInput").ap()
    din("wgates", [16, 128, 4096])
    din("w_branch_a", [1024, D]); din("w_branch_b", [1024, D]); din("w_out", [D, D])
    din("ln1", [128, 2, D]); din("ln2", [128, 2, D]); din("rbias", [128, 64]); din("w_router", [D, 64])
    din("w_gate", [64, D, 512]); din("w_up", [64, D, 512]); din("w_down", [64, 512, D])
    din("ws_gate", [D, 512]); din("ws_up", [D, 512]); din("ws_down", [512, D])
    din("xrows", [TPC, D]); din("xTs", [D, TPC])
    dr["out"] = nc.dram_tensor("out", [TPC, D], F32, kind="ExternalOutput").ap()


def build_nc_b(n_exp=N_EXP):
    nc = bass.Bass("TRN2", target_bir_lowering=False)
    dr = {}
    declare_b(nc, dr, n_exp)
    dr["yall"] = nc.dram_tensor("yall", [2048, TPC], BF16, kind="ExternalInput").ap()
    dr["tok0"] = 0
    S = Sched(nc)
    outs = build_phase_b(nc, S, dr, n_exp=n_exp)
    S.wait_final("sp", outs)
    S.emit()
    return nc, S


def _run_two_launch(inputs):
    maps_a = host_prep_a(inputs)
    nc_a, _ = build_nc_a()
    res_a = run_bass_kernel_spmd(nc_a, maps_a, core_ids=list(range(NCORE)))
    rows = []
    for h in range(NCORE):
        rows.append(np.asarray(res_a.results[h]["yaT"]))
        rows.append(np.asarray(res_a.results[h]["ybT"]))
    yall = np.concatenate(rows, axis=0)
    maps_b = host_prep_b(inputs, None)
    for c in range(NCORE):
        maps_b[c]["yall"] = np.ascontiguousarray(yall[:, c * TPC:(c + 1) * TPC])
    nc_b, _ = build_nc_b()
    res_b = run_bass_kernel_spmd(nc_b, maps_b, core_ids=list(range(NCORE)))
    out = np.concatenate([np.asarray(res_b.results[c]["out"]) for c in range(NCORE)], axis=0)
    return out.reshape(1, T, D).astype(np.float32)


def kernel(**inputs):
    inputs = {k: np.asarray(v) for k, v in inputs.items()}
    return _run_two_launch(inputs)
```

```python
import numpy as np
import ml_dtypes
import concourse.bass as bass
import concourse.mybir as mybir
from concourse.bass_utils import run_bass_kernel_spmd

F32 = mybir.dt.float32
BF16 = mybir.dt.bfloat16
I32 = mybir.dt.int32
AF = mybir.ActivationFunctionType
ALU = mybir.AluOpType
AX = mybir.AxisListType

T = 8192
D = 2048
NCORE = 8
ENGS = ["pe", "act", "dve", "pool", "sp"]


class Buf:
    __slots__ = ("name", "writes", "reads", "sem", "cnt", "excl")

    def __init__(self, name, excl=False):
        self.name = name
        self.excl = excl
        self.writes = []
        self.reads = []
        self.sem = None
        self.cnt = 0


class Sched:
    def __init__(self, nc):
        self.nc = nc
        self.prog = {e: [] for e in ENGS}
        self.sem = {e: nc.alloc_semaphore(name=f"s_{e}") for e in ENGS if e != "sp"}
        self.cnt = {e: 0 for e in ENGS}
        self.seen = {e: {} for e in ENGS}
        self.n_sems = 4

    def _waits(self, eng, reads, writes, same_gen=False):
        toks = []
        for b in reads:
            toks.extend(b.writes)
        for b in writes:
            toks.extend(b.reads)
            if not same_gen:
                toks.extend(b.writes)
        need = {}
        for (sem, val, src) in toks:
            if src == "pe" and eng == "pe":
                continue
            k = id(sem)
            if self.seen[eng].get(k, 0) >= val:
                continue
            if k not in need or need[k][1] < val:
                need[k] = (sem, val)
        out = []
        for k, (sem, val) in need.items():
            self.seen[eng][k] = val
            out.append((sem, val))
        return out

    def _commit(self, tok, reads, writes, same_gen=False):
        for b in reads:
            b.reads.append(tok)
            if len(b.reads) > 64:
                b.reads = b.reads[-64:] if False else b.reads
        for b in writes:
            if same_gen:
                b.writes.append(tok)
            else:
                b.writes = [tok]
                b.reads = []

    def op(self, eng, fn, reads=(), writes=()):
        self.nops = getattr(self, "nops", 0) + 1
        if self.nops > getattr(self, "max_ops", 10 ** 9):
            return None
        ex = [b for b in reads if b.excl]
        if ex:
            reads = [b for b in reads if not b.excl]
            writes = list(writes) + ex
        waits = self._waits(eng, reads, writes)
        self.cnt[eng] += 1
        tok = (self.sem[eng], self.cnt[eng], eng)
        self.prog[eng].append((waits, fn, (self.sem[eng], 1)))
        self._commit(tok, reads, writes)
        return tok

    def dma(self, q, fn, reads=(), writes=(), sem_buf=None, same_gen=False, inc=16):
        self.nops = getattr(self, "nops", 0) + 1
        if self.nops > getattr(self, "max_ops", 10 ** 9):
            return None
        waits = self._waits(q, reads, writes, same_gen=same_gen)
        if sem_buf.sem is None:
            sem_buf.sem = self.nc.alloc_semaphore(name=f"d_{sem_buf.name}")
            self.n_sems += 1
        sem_buf.cnt += inc
        tok = (sem_buf.sem, sem_buf.cnt, "dma")
        self.prog[q].append((waits, fn, (sem_buf.sem, inc)))
        self._commit(tok, reads, writes, same_gen=same_gen)
        return tok

    def wait_final(self, eng, bufs):
        need = {}
        for b in bufs:
            for (sem, val, src) in list(b.writes) + list(b.reads):
                k = id(sem)
                if k not in need or need[k][1] < val:
                    need[k] = (sem, val)
        self.prog[eng].append((list(need.values()), None, None))

    def emit(self):
        nc = self.nc
        handles = {"pe": "tensor", "act": "scalar", "dve": "vector", "pool": "gpsimd", "sp": "sync"}
        with nc.Block() as block:
            for e in ENGS:
                prog = self.prog[e]
                if not prog:
                    continue

                def body(eng, prog=prog):
                    for waits, fn, inc in prog:
                        for (sem, val) in waits:
                            eng.wait_ge(sem, val)
                        if fn is not None:
                            fn(eng).then_inc(inc[0], inc[1])

                getattr(block, handles[e])(body)


class Ctx:
    def __init__(self, nc, S):
        self.nc = nc
        self.S = S
        self.n = 0

    def sb(self, shape, dt, name=None):
        self.n += 1
        name = name or f"t{self.n}"
        t = self.nc.alloc_sbuf_tensor("sb_" + name, list(shape), dt)
        return t, Buf(name)


NT_A = 16
GDN_SQ = 6


def build_phase_a(nc, S, dr, n_tiles=NT_A, do_gdn=True, do_attn=True):
    C = Ctx(nc, S)
    cur = [None]

    def op(eng, fn, reads=(), writes=()):
        cur[0].append((0, eng, fn, tuple(reads), tuple(writes), None))

    def dma(q, fn, **kw):
        cur[0].append((1, q, fn, None, None, kw))

    def flush(lst):
        for (k, e, fn, r, w, kw) in lst:
            if k == 0:
                S.op(e, fn, reads=r, writes=w)
            else:
                Sched.dma(S, e, fn, **kw)

    def merge(a, b):
        out = []
        ia = ib = 0
        na, nb = len(a), len(b)
        while ia < na or ib < nb:
            if ib >= nb or (ia < na and ia * max(nb, 1) <= ib * max(na, 1)):
                out.append(a[ia]); ia += 1
            else:
                out.append(b[ib]); ib += 1
        return out
    setup_ops = []
    cur[0] = setup_ops

    xT = dr["xT"].rearrange("(c p) t -> p c t", p=128)
    W, bW = C.sb([128, 16, 898], BF16, "W")
    wa_v = dr["wA"].rearrange("(c p) n -> p c n", p=128)
    for c4 in range(4):
        b = Buf(f"Wl{c4}")
        dma("pool", lambda e, c4=c4: e.dma_start(out=W[:, c4 * 4:(c4 + 1) * 4, :], in_=wa_v[:, c4 * 4:(c4 + 1) * 4, :]),
              writes=[bW], sem_buf=bW, same_gen=True)
    misc, bmisc = C.sb([128, 8], F32, "misc")
    dma("sp", lambda e: e.dma_start(out=misc[:], in_=dr["miscA"]), writes=[bmisc], sem_buf=bmisc)
    cw, bcw = C.sb([128, 3, 4], F32, "cw")
    dma("sp", lambda e: e.dma_start(out=cw[:], in_=dr["convw"]), writes=[bcw], sem_buf=bcw)
    nrm, bnrm = C.sb([128, 2, 128], F32, "nrm")
    dma("sp", lambda e: e.dma_start(out=nrm[:], in_=dr["nrmw"]), writes=[bnrm], sem_buf=bnrm)
    lamt, blamt = C.sb([128, 2, 2, 64], F32, "lamt")
    dma("sp", lambda e: e.dma_start(out=lamt[:], in_=dr["lam"]), writes=[blamt], sem_buf=blamt)
    BT, bBT = C.sb([128, 2, 128], F32, "BT")
    dma("sp", lambda e: e.dma_start(out=BT[:], in_=dr["biasT"]), writes=[bBT], sem_buf=bBT)

    onesf, bones = C.sb([128, 128], F32, "onesf")
    op("pool", lambda e: e.memset(onesf[:], 1.0), writes=[bones])
    identf, bident = C.sb([128, 128], F32, "identf")
    op("pool", lambda e: e.affine_select(out=identf[:], in_=onesf[:], pattern=[[-1, 128]], compare_op=ALU.is_equal,
                                         fill=0.0, base=0, channel_multiplier=1), reads=[bones], writes=[bident])
    triu, btriu = C.sb([128, 128], F32, "triu")
    op("pool", lambda e: e.affine_select(out=triu[:], in_=onesf[:], pattern=[[1, 128]], compare_op=ALU.is_ge,
                                         fill=0.0, base=0, channel_multiplier=-1), reads=[bones], writes=[btriu])
    identb, bidentb = C.sb([128, 128], BF16, "identb")
    op("dve", lambda e: e.tensor_copy(out=identb[:], in_=identf[:]), reads=[bident], writes=[bidentb])

    epsc, bepsc = C.sb([128, 1], F32, "epsc")
    op("pool", lambda e: e.memset(epsc[:], 1e-6), writes=[bepsc])
    negA, bnegA = C.sb([128, 1], F32, "negA")
    op("act", lambda e: e.activation(out=negA[:], in_=misc[:, 0:1], func=AF.Exp), reads=[bmisc], writes=[bnegA])
    op("dve", lambda e: e.tensor_scalar(out=negA[:], in0=negA[:], scalar1=-1.0, scalar2=None, op0=ALU.mult),
       reads=[bnegA], writes=[bnegA])
    lprod, blprod = C.sb([128, 2, 64], F32, "lprod")
    op("dve", lambda e: e.tensor_tensor(out=lprod[:], in0=lamt[:, :, 0, :], in1=lamt[:, :, 1, :], op=ALU.mult),
       reads=[blamt], writes=[blprod])
    lsum, blsum = C.sb([128, 2], F32, "lsum")
    op("dve", lambda e: e.tensor_reduce(out=lsum[:], in_=lprod[:], axis=AX.X, op=ALU.add), reads=[blprod], writes=[blsum])
    op("act", lambda e: e.activation(out=lsum[:], in_=lsum[:], func=AF.Exp), reads=[blsum], writes=[blsum])
    lam, blam = C.sb([128, 1], F32, "lam")
    op("dve", lambda e: e.scalar_tensor_tensor(out=lam[:], in0=lsum[:, 0:1], scalar=0.2, in1=lsum[:, 1:2],
                                               op0=ALU.add, op1=ALU.subtract), reads=[blsum], writes=[blam])
    op("dve", lambda e: e.tensor_scalar(out=BT[:], in0=BT[:], scalar1=misc[:, 2:3], scalar2=None, op0=ALU.subtract),
       reads=[bBT, bmisc], writes=[bBT])
    op("dve", lambda e: e.memset(BT[64:128, 0, 0:64], -30000.0), writes=[bBT])
    op("dve", lambda e: e.tensor_scalar(out=nrm[:, 1, :], in0=nrm[:, 1, :], scalar1=0.8, scalar2=None, op0=ALU.mult),
       reads=[bnrm], writes=[bnrm])

    KT, _ = C.sb([128, T], BF16, "KT")
    bKT = [Buf(f"KT{j}") for j in range(NT_A)]
    VA, _ = C.sb([128, 64, 130], BF16, "VA")
    bVA = [Buf(f"VA{j}") for j in range(64)]
    op("pool", lambda e: e.memset(VA[:, :, 128:130], 1.0), writes=bVA)
    QT = [C.sb([128, 512], BF16, f"QT{i}") for i in range(2)]
    xt = [C.sb([128, 16, 512], BF16, f"xt{i}") for i in range(2)]
    cins = [C.sb([128, 3, 515], F32, f"cin{i}") for i in range(2)]
    cout, bcout = C.sb([128, 3, 512], F32, "cout")
    sqt, bsqt = C.sb([128, 512], F32, "sqt")
    qkn = [C.sb([128, 512], F32, f"qkn{i}") for i in range(2)]
    tms = [C.sb([128, 4, 130], F32, f"tm{i}") for i in range(2)]
    sc = {nm: C.sb([128, 4], F32, "sc_" + nm) for nm in
          ["x", "ax", "e", "l", "g", "beta", "gc", "gtot", "eg", "nbeg", "ekd", "glast", "nbeta", "tmp", "ss4", "rr4"]}
    Sst = [C.sb([128, 128], F32, f"Sst{i}") for i in range(2)]
    op("pool", lambda e: e.memset(Sst[0][0][:], 0.0), writes=[Sst[0][1]])
    ystage = [C.sb([128, 512], BF16, f"ystg{i}") for i in range(2)]

    pb = [nc.alloc_psum_tensor(f"pb{i}", [128, 512], F32) for i in range(8)]
    pq = [[Buf(f"pb{i}q{k}", excl=True) for k in range(4)] for i in range(8)]

    stream = ["proj"]
    BMAP = {"proj": {0: 0, 1: 0, 2: 0}, "attn": {7: 1, 3: 1, 4: 2, 5: 3, 6: 4},
            "gdn": {0: 7, 1: 7, 3: 5, 4: 6, 5: 7, 6: 5, 7: 6}}

    def preg(bank, c0, n):
        bank = BMAP[stream[0]][bank]
        return pb[bank][:, c0:c0 + n], [pq[bank][0]]
    g3, g4b, g5, g6, g7 = 5, 6, 7, 5, 6

    def mk(name, shape=(128, 128), dt=F32):
        return C.sb(list(shape), dt, name)
    G4 = {nm: mk("g4_" + nm, (128, 4, 128)) for nm in ["dg", "t1", "E1", "E2", "Erow", "M", "MT", "X", "XT", "AqkT", "kdec",
                                                       "ATm", "qd", "QeffT", "zs", "yb", "junk"]}
    G4["R"] = mk("g4_R", (128, 4, 256))
    nonesf, bnones = C.sb([128, 128], F32, "nonesf")
    op("pool", lambda e: e.memset(nonesf[:], -1.0), writes=[bnones])
    nrm4, bnrm4 = C.sb([128, 4, 128], F32, "nrm4")
    for s4 in range(4):
        op("pool", lambda e, s4=s4: e.tensor_copy(out=nrm4[:, s4, :], in_=nrm[:, 0, :]), reads=[bnrm], writes=[bnrm4])
    rr_t = {nm: C.sb([128, 1], F32, "rr_" + nm) for nm in ["ss", "rr", "r1", "r2", "ss2", "rr2"]}
    PT = [C.sb([128, 512], BF16, f"PT{i}") for i in range(4)]
    at_t2, bat_t2 = C.sb([128, 128], F32, "at_t2")
    at_a, bat_a = C.sb([128, 128], F32, "at_a")
    at_y, bat_y = C.sb([128, 128], F32, "at_y")
    at_junk, bat_junk = C.sb([128, 128], F32, "at_junk")

    b_out = Buf("outA")
    pt_ctr = [0]

    halo, bhalo = C.sb([128, 3, 3], F32, "halo")
    op("pool", lambda e: e.memset(halo[:], 0.0), writes=[bhalo])
    flush(setup_ops)
    Pl, Gl, Al = [], [], []
    for j in range(n_tiles):
        t0 = j * 512
        xtt, bxt = xt[j % 2]
        cin, bcin = cins[j % 2]
        tm, btm = tms[j % 2]
        Pl.append([]); Gl.append([]); Al.append([])
        cur[0] = Pl[j]
        stream[0] = "proj"
        for half in range(2):
            dma("pool", lambda e, xtt=xtt, half=half, t0=t0: e.dma_start(
                out=xtt[:, half * 8:(half + 1) * 8, :], in_=xT[:, half * 8:(half + 1) * 8, t0:t0 + 512]),
                writes=[bxt], sem_buf=bxt, same_gen=(half == 1))
        QTc, bQTc = QT[j % 2]
        for g in range(5):
            bank = g % 2
            ps, pbufs = preg(bank, 0, 512)
            for c in range(16):
                op("pe", lambda e, ps=ps, g=g, c=c, xtt=xtt: e.matmul(ps, lhsT=W[:, c, g * 128:(g + 1) * 128],
                                                                      rhs=xtt[:, c, :], start=(c == 0), stop=(c == 15)),
                   reads=[bW, bxt], writes=pbufs)
            if g == 0:
                op("act", lambda e, ps=ps, QTc=QTc: e.activation(out=QTc[:], in_=ps, func=AF.Copy, scale=0.125),
                   reads=pbufs, writes=[bQTc])
            elif g == 1:
                op("act", lambda e, ps=ps, t0=t0: e.activation(out=KT[:, t0:t0 + 512], in_=ps, func=AF.Copy),
                   reads=pbufs, writes=[bKT[j]])
            else:
                op("act", lambda e, ps=ps, g=g, cin=cin: e.activation(out=cin[:, g - 2, 3:515], in_=ps, func=AF.Copy),
                   reads=pbufs, writes=[bcin])
        for s in range(4):
            ps, pbufs = preg(2, 0, 258)
            for c in range(16):
                op("pe", lambda e, ps=ps, c=c, s=s, xtt=xtt: e.matmul(ps, lhsT=xtt[:, c, s * 128:(s + 1) * 128],
                                                                      rhs=W[:, c, 640:898], start=(c == 0), stop=(c == 15)),
                   reads=[bW, bxt], writes=pbufs)
            op("act", lambda e, s=s, j=j: e.activation(out=VA[:, 4 * j + s, 0:128], in_=pb[0][:, 0:128], func=AF.Copy),
               reads=pbufs, writes=[bVA[4 * j + s]])
            op("dve", lambda e, s=s, tm=tm: e.tensor_copy(out=tm[:, s, :], in_=pb[0][:, 128:258]), reads=pbufs, writes=[btm])

        cur[0] = Gl[j]
        stream[0] = "gdn"
        if do_gdn:
            op("dve", lambda e, cin=cin: e.tensor_copy(out=cin[:, :, 0:3], in_=halo[:]), reads=[bhalo], writes=[bcin])
            for i in range(3):
                op("dve", lambda e, i=i, cin=cin: e.tensor_scalar(out=cout[:, i, :], in0=cin[:, i, 0:512], scalar1=cw[:, i, 0:1],
                                                         scalar2=None, op0=ALU.mult), reads=[bcin, bcw], writes=[bcout])
                for jj in range(1, 4):
                    op("dve", lambda e, i=i, jj=jj, cin=cin: e.scalar_tensor_tensor(
                        out=cout[:, i, :], in0=cin[:, i, jj:jj + 512], scalar=cw[:, i, jj:jj + 1], in1=cout[:, i, :],
                        op0=ALU.mult, op1=ALU.add), reads=[bcin, bcw, bcout], writes=[bcout])
            op("dve", lambda e, cin=cin: e.tensor_copy(out=halo[:], in_=cin[:, :, 512:515]), reads=[bcin], writes=[bhalo])
            op("act", lambda e: e.activation(out=cout[:], in_=cout[:], func=AF.Silu), reads=[bcout], writes=[bcout])
            for i in range(2):
                qk, bqk = qkn[i]
                op("act", lambda e, i=i: e.activation(out=sqt[:], in_=cout[:, i, :], func=AF.Square),
                   reads=[bcout], writes=[bsqt])
                ps, pbufs = preg(i, 0, 512)
                op("pe", lambda e, ps=ps: e.matmul(ps, lhsT=onesf[:], rhs=sqt[:], start=True, stop=True),
                   reads=[bones, bsqt], writes=pbufs)
                op("act", lambda e, ps=ps: e.activation(out=sqt[:], in_=ps, func=AF.Ln, bias=epsc[:, 0:1]), reads=pbufs + [bepsc], writes=[bsqt])
                op("act", lambda e: e.activation(out=sqt[:], in_=sqt[:], func=AF.Exp, scale=-0.5), reads=[bsqt], writes=[bsqt])
                scl = (128.0 ** -0.5) if i == 0 else 1.0
                op("dve", lambda e, i=i, qk=qk, scl=scl: e.scalar_tensor_tensor(
                    out=qk[:], in0=cout[:, i, :], scalar=scl, in1=sqt[:], op0=ALU.mult, op1=ALU.mult),
                    reads=[bcout, bsqt], writes=[bqk])
            def sct(nm):
                return sc[nm][0], sc[nm][1]
            x_, bx_ = sct("x"); ax_, bax_ = sct("ax"); e_, be_ = sct("e"); l_, bl_ = sct("l"); g_, bg_ = sct("g")
            beta_, bbeta_ = sct("beta"); gc_, bgc_ = sct("gc"); gtot_, bgtot_ = sct("gtot"); eg_, beg_ = sct("eg")
            nbeg_, bnbeg_ = sct("nbeg"); ekd_, bekd_ = sct("ekd"); glast_, bglast_ = sct("glast")
            nbeta_, bnbeta_ = sct("nbeta"); tmp_, btmp_ = sct("tmp")
            op("dve", lambda e, tm=tm: e.tensor_scalar(out=x_[:], in0=tm[:, :, 128], scalar1=misc[:, 1:2], scalar2=None,
                                                op0=ALU.add), reads=[btm, bmisc], writes=[bx_])
            op("dve", lambda e: e.scalar_tensor_tensor(out=ax_[:], in0=x_[:], scalar=-1.0, in1=x_[:], op0=ALU.mult,
                                                       op1=ALU.max), reads=[bx_], writes=[bax_])
            op("act", lambda e: e.activation(out=e_[:], in_=ax_[:], func=AF.Exp, scale=-1.0), reads=[bax_], writes=[be_])
            op("act", lambda e: e.activation(out=l_[:], in_=e_[:], func=AF.Ln, bias=onesf[:, 0:1]), reads=[be_, bones], writes=[bl_])
            op("dve", lambda e: e.scalar_tensor_tensor(out=g_[:], in0=x_[:], scalar=0.0, in1=l_[:], op0=ALU.max,
                                                       op1=ALU.add), reads=[bx_, bl_], writes=[bg_])
            op("dve", lambda e: e.tensor_scalar(out=g_[:], in0=g_[:], scalar1=negA[:, 0:1], scalar2=None, op0=ALU.mult),
               reads=[bg_, bnegA], writes=[bg_])
            op("act", lambda e, tm=tm: e.activation(out=beta_[:], in_=tm[:, :, 129], func=AF.Sigmoid), reads=[btm], writes=[bbeta_])
            psg, pgb = preg(6, 384, 8)
            op("pe", lambda e: e.matmul(psg[:, 0:4], lhsT=triu[:], rhs=g_[:], start=True, stop=True),
               reads=[btriu, bg_], writes=pgb)
            op("pe", lambda e: e.matmul(psg[:, 4:8], lhsT=onesf[:], rhs=g_[:], start=True, stop=True),
               reads=[bones, bg_], writes=pgb)
            op("dve", lambda e: e.tensor_copy(out=gc_[:], in_=psg[:, 0:4]), reads=pgb, writes=[bgc_])
            op("dve", lambda e: e.tensor_copy(out=gtot_[:], in_=psg[:, 4:8]), reads=pgb, writes=[bgtot_])
            op("act", lambda e: e.activation(out=eg_[:], in_=gc_[:], func=AF.Exp), reads=[bgc_], writes=[beg_])
            op("act", lambda e: e.activation(out=glast_[:], in_=gtot_[:], func=AF.Exp), reads=[bgtot_], writes=[bglast_])
            op("dve", lambda e: e.tensor_tensor(out=tmp_[:], in0=gtot_[:], in1=gc_[:], op=ALU.subtract),
               reads=[bgtot_, bgc_], writes=[btmp_])
            op("act", lambda e: e.activation(out=ekd_[:], in_=tmp_[:], func=AF.Exp), reads=[btmp_], writes=[bekd_])
            op("dve", lambda e: e.tensor_scalar(out=nbeta_[:], in0=beta_[:], scalar1=-1.0, scalar2=None, op0=ALU.mult),
               reads=[bbeta_], writes=[bnbeta_])
            op("dve", lambda e: e.tensor_tensor(out=nbeg_[:], in0=nbeta_[:], in1=eg_[:], op=ALU.mult),
               reads=[bnbeta_, beg_], writes=[bnbeg_])

            qTn, bqTn = qkn[0]
            kTn, bkTn = qkn[1]

            def g4(nm):
                return G4[nm][0], G4[nm][1]
            dg, bdg = g4("dg"); t1, bt1 = g4("t1"); E1, bE1 = g4("E1"); E2, bE2 = g4("E2"); Erow, bErow = g4("Erow")
            M, bM = g4("M"); MT, bMT = g4("MT"); X, bX = g4("X"); XT, bXT = g4("XT"); AqkT, bAqkT = g4("AqkT")
            kdec, bkdec = g4("kdec"); ATm, bATm = g4("ATm"); qd, bqd = g4("qd"); QeffT, bQeffT = g4("QeffT")
            zs, bzs = g4("zs"); yb, byb = g4("yb"); junk, bjunk = g4("junk")
            R_, bR = G4["R"]

            def v4(bank):
                return pb[bank][:].rearrange("p (s c) -> p s c", s=4)
            for s in range(4):
                op("dve", lambda e, s=s: e.tensor_scalar(out=dg[:, s, :], in0=identf[:], scalar1=gc_[:, s:s + 1], scalar2=None,
                                                         op0=ALU.mult), reads=[bident, bgc_], writes=[bdg])
            for s in range(4):
                op("pe", lambda e, s=s: e.matmul(pb[g3][:, s * 128:(s + 1) * 128], lhsT=dg[:, s, :], rhs=onesf[:],
                                                 start=(s == 0), stop=False, skip_group_check=True),
                   reads=[bdg, bones], writes=[pq[g3][0]])
            for s in range(4):
                op("pe", lambda e, s=s: e.matmul(pb[g3][:, s * 128:(s + 1) * 128], lhsT=nonesf[:], rhs=dg[:, s, :],
                                                 start=False, stop=(s == 3), skip_group_check=True),
                   reads=[bdg, bnones], writes=[pq[g3][0]])
            for s in range(4):
                op("pe", lambda e, s=s: e.matmul(pb[g4b][:, s * 128:(s + 1) * 128], lhsT=onesf[:], rhs=dg[:, s, :],
                                                 start=(s == 0), stop=(s == 3), skip_group_check=True),
                   reads=[bdg, bones], writes=[pq[g4b][0]])
            op("dve", lambda e: e.tensor_scalar(out=t1[:], in0=v4(g3), scalar1=0.0, scalar2=None, op0=ALU.min),
               reads=[pq[g3][0]], writes=[bt1])
            op("act", lambda e: e.activation(out=E1[:], in_=t1[:], func=AF.Exp), reads=[bt1], writes=[bE1])
            op("pool", lambda e: e.affine_select(out=E1[:], in_=E1[:], pattern=[[0, 4], [-1, 128]], compare_op=ALU.is_gt,
                                                 fill=0.0, base=0, channel_multiplier=1), reads=[bE1], writes=[bE1])
            op("dve", lambda e: e.tensor_scalar(out=t1[:], in0=v4(g3), scalar1=0.0, scalar2=None, op0=ALU.max),
               reads=[pq[g3][0]], writes=[bt1])
            op("act", lambda e: e.activation(out=E2[:], in_=t1[:], func=AF.Exp, scale=-1.0), reads=[bt1], writes=[bE2])
            op("pool", lambda e: e.affine_select(out=E2[:], in_=E2[:], pattern=[[0, 4], [1, 128]], compare_op=ALU.is_ge,
                                                 fill=0.0, base=0, channel_multiplier=-1), reads=[bE2], writes=[bE2])
            op("act", lambda e: e.activation(out=Erow[:], in_=v4(g4b), func=AF.Exp), reads=[pq[g4b][0]], writes=[bErow])
            for s in range(4):
                cs = slice(s * 128, (s + 1) * 128)
                op("pe", lambda e, s=s, cs=cs: e.matmul(pb[g5][:, cs], lhsT=kTn[:, cs], rhs=kTn[:, cs], start=(s == 0),
                                                        stop=(s == 3), skip_group_check=True), reads=[bkTn], writes=[pq[g5][0]])
            for s in range(4):
                cs = slice(s * 128, (s + 1) * 128)
                op("pe", lambda e, s=s, cs=cs: e.matmul(pb[g6][:, cs], lhsT=kTn[:, cs], rhs=qTn[:, cs], start=(s == 0),
                                                        stop=(s == 3), skip_group_check=True), reads=[bkTn, bqTn], writes=[pq[g6][0]])
            for s in range(4):
                op("dve", lambda e, s=s: e.scalar_tensor_tensor(out=M[:, s, :], in0=pb[g5][:, s * 128:(s + 1) * 128],
                                                                scalar=nbeta_[:, s:s + 1], in1=E1[:, s, :], op0=ALU.mult,
                                                                op1=ALU.mult), reads=[pq[g5][0], bnbeta_, bE1], writes=[bM])
            op("dve", lambda e: e.tensor_tensor(out=AqkT[:], in0=v4(g6), in1=E2[:], op=ALU.mult), reads=[pq[g6][0], bE2], writes=[bAqkT])
            for s in range(4):
                op("pe", lambda e, s=s: e.transpose(pb[g7][:, s * 128:(s + 1) * 128], M[:, s, :], identf[:]),
                   reads=[bM, bident], writes=[pq[g7][0]])
            op("act", lambda e: e.activation(out=MT[:], in_=v4(g7), func=AF.Copy), reads=[pq[g7][0]], writes=[bMT])
            for s in range(4):
                cs = slice(s * 128, (s + 1) * 128)
                op("pe", lambda e, cs=cs: e.transpose(pb[g3][:, cs], kTn[:, cs], identf[:]), reads=[bkTn, bident], writes=[pq[g3][0]])
            for s in range(4):
                cs = slice(s * 128, (s + 1) * 128)
                op("pe", lambda e, cs=cs: e.transpose(pb[g4b][:, cs], cout[:, 2, cs], identf[:]), reads=[bcout, bident], writes=[pq[g4b][0]])
            for s in range(4):
                cs = slice(s * 128, (s + 1) * 128)
                op("dve", lambda e, s=s, cs=cs: e.tensor_scalar(out=kdec[:, s, :], in0=pb[g3][:, cs], scalar1=ekd_[:, s:s + 1],
                                                                scalar2=None, op0=ALU.mult), reads=[pq[g3][0], bekd_], writes=[bkdec])
                op("dve", lambda e, s=s, cs=cs: e.tensor_scalar(out=R_[:, s, 128:256], in0=pb[g3][:, cs], scalar1=nbeg_[:, s:s + 1],
                                                                scalar2=None, op0=ALU.mult), reads=[pq[g3][0], bnbeg_], writes=[bR])
                op("dve", lambda e, s=s, cs=cs: e.tensor_scalar(out=R_[:, s, 0:128], in0=pb[g4b][:, cs], scalar1=beta_[:, s:s + 1],
                                                                scalar2=None, op0=ALU.mult), reads=[pq[g4b][0], bbeta_], writes=[bR])
            Xc, bXc, XTc, bXTc = M, bM, MT, bMT
            for it in range(GDN_SQ + 1):
                for s in range(4):
                    bank = (g5, g6)[s // 2]
                    op("pe", lambda e, s=s, bank=bank, XTc=XTc: e.matmul(
                        pb[bank][:, (s % 2) * 256:(s % 2 + 1) * 256], lhsT=XTc[:, s, :], rhs=R_[:, s, :],
                        start=(s % 2 == 0), stop=(s % 2 == 1), skip_group_check=True), reads=[bXTc, bR], writes=[pq[bank][0]])
                for hb_ in range(2):
                    op("dve", lambda e, hb_=hb_: e.tensor_tensor(
                        out=R_[:, 2 * hb_:2 * hb_ + 2, :], in0=R_[:, 2 * hb_:2 * hb_ + 2, :],
                        in1=pb[(g5, g6)[hb_]][:].rearrange("p (s c) -> p s c", s=2), op=ALU.add),
                        reads=[bR, pq[(g5, g6)[hb_]][0]], writes=[bR])
                if it < GDN_SQ:
                    if it % 2 == 0:
                        Xn, bXn, XTn, bXTn = X, bX, XT, bXT
                    else:
                        Xn, bXn, XTn, bXTn = M, bM, MT, bMT
                    for s in range(4):
                        op("pe", lambda e, s=s, Xc=Xc, XTc=XTc: e.matmul(
                            pb[g7][:, s * 128:(s + 1) * 128], lhsT=XTc[:, s, :], rhs=Xc[:, s, :], start=(s == 0), stop=(s == 3),
                            skip_group_check=True), reads=[bXc, bXTc], writes=[pq[g7][0]])
                    op("act", lambda e, Xn=Xn: e.activation(out=Xn[:], in_=v4(g7), func=AF.Copy), reads=[pq[g7][0]], writes=[bXn])
                    for s in range(4):
                        op("pe", lambda e, s=s, Xc=Xc, XTc=XTc: e.matmul(
                            pb[g7][:, s * 128:(s + 1) * 128], lhsT=Xc[:, s, :], rhs=XTc[:, s, :], start=(s == 0), stop=(s == 3),
                            skip_group_check=True), reads=[bXc, bXTc], writes=[pq[g7][0]])
                    op("dve", lambda e, XTn=XTn: e.tensor_copy(out=XTn[:], in_=v4(g7)), reads=[pq[g7][0]], writes=[bXTn])
                    Xc, bXc, XTc, bXTc = Xn, bXn, XTn, bXTn
            for s in range(4):
                op("pe", lambda e, s=s: e.matmul(pb[g4b][:, s * 128:(s + 1) * 128], lhsT=R_[:, s, 128:256], rhs=kdec[:, s, :],
                                                 start=(s == 0), stop=(s == 3), skip_group_check=True),
                   reads=[bR, bkdec], writes=[pq[g4b][0]])
            for s in range(4):
                op("dve", lambda e, s=s: e.scalar_tensor_tensor(out=ATm[:, s, :], in0=identf[:], scalar=glast_[:, s:s + 1],
                                                                in1=pb[g4b][:, s * 128:(s + 1) * 128], op0=ALU.mult, op1=ALU.add),
                   reads=[bident, bglast_, pq[g4b][0]], writes=[bATm])
            for s in range(4):
                op("pe", lambda e, s=s: e.matmul(pb[g7][:, s * 128:(s + 1) * 128], lhsT=R_[:, s, 128:256], rhs=AqkT[:, s, :],
                                                 start=(s == 0), stop=(s == 3), skip_group_check=True),
                   reads=[bR, bAqkT], writes=[pq[g7][0]])
            op("pool", lambda e: e.tensor_tensor(out=qd[:], in0=qTn[:].rearrange("p (s c) -> p s c", s=4), in1=Erow[:],
                                                 op=ALU.mult), reads=[bqTn, bErow], writes=[bqd])
            op("dve", lambda e: e.tensor_tensor(out=QeffT[:], in0=qd[:], in1=v4(g7), op=ALU.add), reads=[bqd, pq[g7][0]], writes=[bQeffT])
            op("act", lambda e, tm=tm: e.activation(out=zs[:], in_=tm[:, :, 0:128], func=AF.Silu), reads=[btm], writes=[bzs])
            op("pool", lambda e: e.tensor_tensor(out=zs[:], in0=zs[:], in1=nrm4[:], op=ALU.mult), reads=[bzs, bnrm4], writes=[bzs])
            ss, bss = sc["ss4"]
            for s in range(4):
                n = 4 * j + s
                Scur, bScur = Sst[n % 2]
                Snxt, bSnxt = Sst[(n + 1) % 2]
                op("pe", lambda e, s=s, Scur=Scur: e.matmul(pb[g5][:, s * 128:(s + 1) * 128], lhsT=QeffT[:, s, :], rhs=Scur[:],
                                                            start=(s == 0), stop=False, skip_group_check=True),
                   reads=[bQeffT, bScur], writes=[pq[g5][0]])
                op("pe", lambda e, s=s: e.matmul(pb[g5][:, s * 128:(s + 1) * 128], lhsT=AqkT[:, s, :], rhs=R_[:, s, 0:128],
                                                 start=False, stop=True, skip_group_check=True),
                   reads=[bAqkT, bR], writes=[pq[g5][0]])
                op("pe", lambda e, s=s: e.matmul(pb[g6][:, 0:128], lhsT=kdec[:, s, :], rhs=R_[:, s, 0:128], start=True, stop=False),
                   reads=[bkdec, bR], writes=[pq[g6][0]])
                op("pe", lambda e, s=s, Scur=Scur: e.matmul(pb[g6][:, 0:128], lhsT=ATm[:, s, :], rhs=Scur[:], start=False, stop=True),
                   reads=[bATm, bScur], writes=[pq[g6][0]])
                op("act", lambda e, Snxt=Snxt: e.activation(out=Snxt[:], in_=pb[g6][:, 0:128], func=AF.Copy),
                   reads=[pq[g6][0]], writes=[bSnxt])
            for s in range(4):
                op("act", lambda e, s=s: e.activation(out=junk[:, 0, :], in_=pb[g5][:, s * 128:(s + 1) * 128], func=AF.Square,
                                                      accum_out=ss[:, s:s + 1]), reads=[pq[g5][0]], writes=[bjunk, bss])
            rr, brr = sc["rr4"]
            op("dve", lambda e: e.tensor_scalar(out=rr[:], in0=ss[:], scalar1=1.0 / 128, scalar2=1e-6, op0=ALU.mult, op1=ALU.add),
               reads=[bss], writes=[brr])
            op("act", lambda e: e.activation(out=rr[:], in_=rr[:], func=AF.Ln), reads=[brr], writes=[brr])
            op("act", lambda e: e.activation(out=rr[:], in_=rr[:], func=AF.Exp, scale=-0.5), reads=[brr], writes=[brr])
            for s in range(4):
                op("dve", lambda e, s=s: e.scalar_tensor_tensor(out=yb[:, s, :], in0=pb[g5][:, s * 128:(s + 1) * 128],
                                                                scalar=rr[:, s:s + 1], in1=zs[:, s, :], op0=ALU.mult, op1=ALU.mult),
                   reads=[pq[g5][0], brr, bzs], writes=[byb])
            for s in range(4):
                op("pe", lambda e, s=s: e.transpose(pb[g3][:, s * 128:(s + 1) * 128], yb[:, s, :], identf[:]),
                   reads=[byb, bident], writes=[pq[g3][0]])
            op("act", lambda e: e.activation(out=ystage[1][0][:], in_=pb[g3][:], func=AF.Copy), reads=[pq[g3][0]], writes=[ystage[1][1]])
            dma("sp", lambda e, t0=t0: e.dma_start(out=dr["ybT"][:, t0:t0 + 512], in_=ystage[1][0][:]),
                  reads=[ystage[1][1]], writes=[b_out], sem_buf=ystage[1][1], same_gen=True)

        cur[0] = Al[j]
        stream[0] = "attn"
        if do_attn:
            oacc = {}
            slots = [(4, 0), (4, 129), (4, 258), (5, 0), (5, 129), (5, 258), (6, 0), (6, 129)]
            for m in range(2):
                for s in range(4):
                    oacc[(m, s)] = slots[m * 4 + s]
            n_k = 4 * j + 4
            it = 0
            for i in range(n_k):
                s0 = max(0, i - 4 * j)
                q0 = s0 * 128
                for m in range(2):
                    bank = 7 if (it % 2 == 0) else 3
                    it += 1
                    pst, bpst = preg(bank, q0, 512 - q0)
                    ms = slice(m * 64, (m + 1) * 64)
                    near = [s for s in range(s0, 4) if (4 * j + s) - i <= 1]
                    op("pe", lambda e, pst=pst, ms=ms, i=i, q0=q0, QTc=QTc, near=near: e.matmul(
                        pst, lhsT=KT[ms, i * 128:(i + 1) * 128], rhs=QTc[ms, q0:512], start=True, stop=(len(near) == 0),
                        skip_group_check=True),
                        reads=[bKT[i // 4], bQTc], writes=bpst)
                    for idx, s in enumerate(near):
                        typ = 0 if (4 * j + s) == i else 1
                        op("pe", lambda e, bank=BMAP["attn"][bank], s=s, typ=typ, last=(idx == len(near) - 1): e.matmul(
                            pb[bank][:, s * 128:(s + 1) * 128], lhsT=identf[:], rhs=BT[:, typ, :], start=False, stop=last,
                            skip_group_check=True), reads=[bident, bBT], writes=bpst)
                    ptt, bptt = PT[pt_ctr[0] % 4]
                    pt_ctr[0] += 1
                    op("act", lambda e, pst=pst, ptt=ptt, q0=q0: e.activation(out=ptt[:, q0:512], in_=pst, func=AF.Exp,
                                                                              bias=misc[:, 2:3]),
                       reads=bpst + [bmisc], writes=[bptt])
                    for s in range(s0, 4):
                        bk, c0 = oacc[(m, s)]
                        po, bpo = preg(bk, c0, 129)
                        last_k = 4 * j + s
                        op("pe", lambda e, po=po, ptt=ptt, s=s, i=i, last_k=last_k, c0=c0: e.matmul(
                            po, lhsT=ptt[:, s * 128:(s + 1) * 128], rhs=VA[:, i, 0:129], start=(i == 0 and c0 == 0), stop=(i == last_k),
                            skip_group_check=True), reads=[bptt, bVA[i]], writes=bpo)
            for s in range(4):
                b1, c1 = oacc[(0, s)]
                b2, c2 = oacc[(1, s)]
                po1, bpo1 = preg(b1, c1, 129)
                po2, bpo2 = preg(b2, c2, 129)
                r1, br1 = rr_t["r1"]; r2, br2 = rr_t["r2"]; ss2, bss2 = rr_t["ss2"]; rr2, brr2 = rr_t["rr2"]
                op("dve", lambda e, po1=po1: e.reciprocal(out=r1[:], in_=po1[:, 128:129]), reads=bpo1, writes=[br1])
                op("dve", lambda e, po2=po2: e.reciprocal(out=r2[:], in_=po2[:, 128:129]), reads=bpo2, writes=[br2])
                op("dve", lambda e: e.tensor_tensor(out=r2[:], in0=r2[:], in1=lam[:], op=ALU.mult), reads=[br2, blam], writes=[br2])
                op("dve", lambda e, po2=po2: e.tensor_scalar(out=at_t2[:], in0=po2[:, 0:128], scalar1=r2[:, 0:1], scalar2=None,
                                                             op0=ALU.mult), reads=bpo2 + [br2], writes=[bat_t2])
                op("dve", lambda e, po1=po1: e.scalar_tensor_tensor(out=at_a[:], in0=po1[:, 0:128], scalar=r1[:, 0:1],
                                                                    in1=at_t2[:], op0=ALU.mult, op1=ALU.subtract),
                   reads=bpo1 + [br1, bat_t2], writes=[bat_a])
                op("act", lambda e: e.activation(out=at_junk[:], in_=at_a[:], func=AF.Square, accum_out=ss2[:]),
                   reads=[bat_a], writes=[bat_junk, bss2])
                op("dve", lambda e: e.tensor_scalar(out=rr2[:], in0=ss2[:], scalar1=1.0 / 128, scalar2=1e-6, op0=ALU.mult,
                                                    op1=ALU.add), reads=[bss2], writes=[brr2])
                op("act", lambda e: e.activation(out=rr2[:], in_=rr2[:], func=AF.Ln), reads=[brr2], writes=[brr2])
                op("act", lambda e: e.activation(out=rr2[:], in_=rr2[:], func=AF.Exp, scale=-0.5), reads=[brr2], writes=[brr2])
                op("dve", lambda e: e.scalar_tensor_tensor(out=at_y[:], in0=at_a[:], scalar=rr2[:, 0:1], in1=nrm[:, 1, :],
                                                           op0=ALU.mult, op1=ALU.mult), reads=[bat_a, brr2, bnrm], writes=[bat_y])
                pY, bpY = preg(6, 258, 128)
                op("pe", lambda e, pY=pY: e.transpose(pY, at_y[:], identf[:]), reads=[bat_y, bident], writes=bpY)
                op("act", lambda e, pY=pY, s=s: e.activation(out=ystage[0][0][:, s * 128:(s + 1) * 128], in_=pY, func=AF.Copy),
                   reads=bpY, writes=[ystage[0][1]])
            dma("sp", lambda e, t0=t0: e.dma_start(out=dr["yaT"][:, t0:t0 + 512], in_=ystage[0][0][:]),
                  reads=[ystage[0][1]], writes=[b_out], sem_buf=ystage[0][1], same_gen=True)
    flush(Pl[0])
    for j in range(n_tiles):
        nxt = Pl[j + 1] if j + 1 < n_tiles else []
        flush(merge(Gl[j], Al[j] + nxt))
    return [b_out]


def host_prep_a(inp):
    x = inp["x"][0]
    w_in = inp["w_in"][0]
    xT = np.ascontiguousarray(x.T)
    conv_w = inp["conv_w"][0]
    table = inp["rel_bias_table"]
    nb = 16
    ki = np.arange(128)[:, None]
    qi = np.arange(128)[None, :]

    def bucket(rel):
        base = np.where(rel > 0, nb, 0)
        n = np.abs(rel)
        max_exact = nb // 2
        nf = np.maximum(n, 1).astype(np.float32)
        large = max_exact + (np.log(nf / np.float32(max_exact)) / np.float32(np.log(128 / max_exact))
                             * np.float32(nb - max_exact)).astype(np.int32)
        large = np.minimum(large, nb - 1)
        return base + np.where(n < max_exact, n, large)
    bk = np.stack([bucket(ki - qi), bucket(ki - 128 - qi)], axis=1)
    maps = []
    for h in range(NCORE):
        cols = np.concatenate([
            np.arange(h * 128, h * 128 + 128),
            1024 + np.arange(h * 128, h * 128 + 128),
            3072 + np.arange(h * 128, h * 128 + 128),
            4096 + np.arange(h * 128, h * 128 + 128),
            5120 + np.arange(h * 128, h * 128 + 128),
            2048 + np.arange(h * 128, h * 128 + 128),
            6144 + np.arange(h * 128, h * 128 + 128),
            np.array([7168 + h, 7176 + h]),
        ])
        wA = np.ascontiguousarray(w_in[:, cols])
        misc = np.zeros((128, 8), np.float32)
        misc[:, 0] = inp["gdn_a_log"][0, h]
        misc[:, 1] = inp["gdn_dt_bias"][0, h]
        misc[:, 2] = table[15, h]
        cw = np.stack([conv_w[:, i * 1024 + h * 128:i * 1024 + (h + 1) * 128].T for i in range(3)], axis=1)
        nrmw = np.stack([np.broadcast_to(inp["gdn_norm_w"][0], (128, 128)),
                         np.broadcast_to(inp["diff_subln_w"][0], (128, 128))], axis=1)
        lamr = np.broadcast_to(inp["diff_lambda"][0].reshape(1, 2, 2, 64), (128, 2, 2, 64))
        biasT = table[:, h][bk]
        maps.append({"xT": xT, "wA": wA, "miscA": misc, "convw": np.ascontiguousarray(cw, dtype=np.float32),
                     "nrmw": np.ascontiguousarray(nrmw, dtype=np.float32),
                     "lam": np.ascontiguousarray(lamr, dtype=np.float32),
                     "biasT": np.ascontiguousarray(biasT, dtype=np.float32)})
    return maps


def build_nc_a(n_tiles=NT_A, do_gdn=True, do_attn=True, max_ops=10 ** 9):
    nc = bass.Bass("TRN2", target_bir_lowering=False)
    dr = {}
    dr["xT"] = nc.dram_tensor("xT", [D, T], F32, kind="ExternalInput").ap()
    dr["wA"] = nc.dram_tensor("wA", [D, 898], F32, kind="ExternalInput").ap()
    dr["miscA"] = nc.dram_tensor("miscA", [128, 8], F32, kind="ExternalInput").ap()
    dr["convw"] = nc.dram_tensor("convw", [128, 3, 4], F32, kind="ExternalInput").ap()
    dr["nrmw"] = nc.dram_tensor("nrmw", [128, 2, 128], F32, kind="ExternalInput").ap()
    dr["lam"] = nc.dram_tensor("lam", [128, 2, 2, 64], F32, kind="ExternalInput").ap()
    dr["biasT"] = nc.dram_tensor("biasT", [128, 2, 128], F32, kind="ExternalInput").ap()
    dr["yaT"] = nc.dram_tensor("yaT", [128, T], BF16, kind="ExternalOutput").ap()
    dr["ybT"] = nc.dram_tensor("ybT", [128, T], BF16, kind="ExternalOutput").ap()
    S = Sched(nc)
    S.max_ops = max_ops
    outs = build_phase_a(nc, S, dr, n_tiles=n_tiles, do_gdn=do_gdn, do_attn=do_attn)
    S.wait_final("sp", outs)
    S.emit()
    return nc, S


TPC = T // NCORE
ALPHA = 2.0 ** 0.25
N_EXP = 64


def build_phase_b(nc, S, dr, n_exp=N_EXP, yall_bufs=()):
    C = Ctx(nc, S)
    op = S.op
    yall_bufs = list(yall_bufs)
    ARENA, _ = C.sb([128, 40960], BF16, "arena")
    bA = [Buf("arena0"), Buf("arena1"), Buf("arena2")]
    MT, _ = C.sb([128, 16, 1024], BF16, "MT")
    bMT = [Buf(f"MT{i}") for i in range(16)]
    ACC, _ = C.sb([128, 8, 2048], F32, "ACC")
    bACC = [Buf(f"ACC{i}") for i in range(8)]
    WGT = [C.sb([128, 4096], BF16, f"wgt{i}") for i in range(2)]
    tmpf = [C.sb([128, 512], F32, f"tmpf{i}") for i in range(2)]
    Gt, bGt = C.sb([128, 8, 64], F32, "Gt")
    identf, bident = C.sb([128, 128], F32, "identfB")
    onesB, bonesB = C.sb([128, 128], F32, "onesB")
    op("pool", lambda e: e.memset(onesB[:], 1.0), writes=[bonesB])
    op("pool", lambda e: e.affine_select(out=identf[:], in_=onesB[:], pattern=[[-1, 128]], compare_op=ALU.is_equal,
                                         fill=0.0, base=0, channel_multiplier=1), reads=[bonesB], writes=[bident])
    rb, brb = C.sb([128, 64], F32, "rbias")
    S.dma("sp", lambda e: e.dma_start(out=rb[:], in_=dr["rbias"]), writes=[brb], sem_buf=brb)
    wr, bwr = C.sb([128, 16, 64], F32, "wr")
    S.dma("sp", lambda e: e.dma_start(out=wr[:], in_=dr["w_router"].rearrange("(c p) n -> p c n", p=128)),
          writes=[bwr], sem_buf=bwr)
    epsc, bepsc = C.sb([128, 2], F32, "epscB")
    op("pool", lambda e: e.memset(epsc[:, 0:1], 1e-5), writes=[bepsc])
    small = {nm: C.sb([128, 8], F32, "smB_" + nm) for nm in ["m8", "gs", "g8", "gm", "t8", "den", "mv", "rstd", "nmr"]}
    st6, bst6 = C.sb([128, 4, 6], F32, "st6")
    ch, bch = C.sb([128, 64], F32, "choice")
    scs, bscs = C.sb([128, 64], F32, "scores")
    mc, bmc = C.sb([128, 64], F32, "mchoice")

    pb = [nc.alloc_psum_tensor(f"pbB{i}", [128, 512], F32) for i in range(8)]
    pq = [Buf(f"pbB{i}", excl=True) for i in range(8)]

    def AR(c0, n):
        return ARENA[:, c0:c0 + n]

    ACCb = ACC[:].rearrange("p t d -> p (t d)").bitcast(BF16)

    def XH(c0, n):
        return ACCb[:, c0:c0 + n]

    wba_v = dr["w_branch_a"].rearrange("(c p) n -> p c n", p=128)
    wbb_v = dr["w_branch_b"].rearrange("(c p) n -> p c n", p=128)
    for c in range(8):
        S.dma("pool", lambda e, c=c: e.dma_start(out=AR(c * 2048, 2048), in_=wba_v[:, c, :]),
              writes=[bA[0]], sem_buf=bA[0], same_gen=True)
    for c in range(8):
        S.dma("pool", lambda e, c=c: e.dma_start(out=AR(16384 + c * 2048, 2048), in_=wbb_v[:, c, :]),
              writes=[bA[1]], sem_buf=bA[1], same_gen=True)
    xTs = dr["xTs"].rearrange("(c p) t -> p c t", p=128)
    yall = dr["yall"].rearrange("(r p) t -> p r t", p=128)
    tok0 = dr["tok0"]
    bX = Buf("xhalf")
    bY = Buf("yhalf")
    for half in range(2):
        for c in range(16):
            S.dma("pool", lambda e, c=c, half=half: e.dma_start(
                out=XH(c * 512, 512), in_=xTs[:, c, half * 512:(half + 1) * 512]),
                writes=[bX], sem_buf=bX, same_gen=(c > 0))
        for r in range(16):
            S.dma("sp", lambda e, r=r, half=half: e.dma_start(
                out=XH(8192 + r * 512, 512), in_=yall[:, r, tok0 + half * 512:tok0 + (half + 1) * 512]),
                reads=yall_bufs, writes=[bY], sem_buf=bY, same_gen=(r > 0))
        for m in range(16):
            wg, bwg = WGT[m % 2]
            S.dma("pool", lambda e, wg=wg, m=m: e.dma_start(out=wg[:], in_=dr["wgates"][m]),
                  writes=[bwg], sem_buf=bwg)
            for ab in range(2):
                for c in range(16):
                    op("pe", lambda e, ab=ab, c=c, wg=wg: e.matmul(
                        pb[ab][:], lhsT=wg[:, c * 256 + ab * 128:c * 256 + ab * 128 + 128], rhs=XH(c * 512, 512),
                        start=(c == 0), stop=(c == 15)), reads=[bwg, bX], writes=[pq[ab]])
            for ab in range(2):
                for hh in range(8):
                    op("pe", lambda e, ab=ab, hh=hh, m=m: e.matmul(
                        pb[2 + ab][:], lhsT=AR(ab * 16384 + hh * 2048 + m * 128, 128),
                        rhs=XH(8192 + (2 * hh + ab) * 512, 512), start=(hh == 0), stop=(hh == 7)),
                        reads=[bA[ab], bY], writes=[pq[2 + ab]])
            t0_, bt0 = tmpf[0]; t1_, bt1 = tmpf[1]
            op("act", lambda e: e.activation(out=t0_[:], in_=pb[0][:], func=AF.Sigmoid), reads=[pq[0]], writes=[bt0])
            op("act", lambda e: e.activation(out=t1_[:], in_=pb[1][:], func=AF.Sigmoid), reads=[pq[1]], writes=[bt1])
            op("dve", lambda e: e.tensor_tensor(out=t0_[:], in0=t0_[:], in1=pb[2][:], op=ALU.mult),
               reads=[bt0, pq[2]], writes=[bt0])
            op("dve", lambda e: e.tensor_tensor(out=t1_[:], in0=t1_[:], in1=pb[3][:], op=ALU.mult),
               reads=[bt1, pq[3]], writes=[bt1])
            op("pool", lambda e, m=m, half=half: e.tensor_tensor(out=MT[:, m, half * 512:(half + 1) * 512], in0=t0_[:],
                                                                 in1=t1_[:], op=ALU.add), reads=[bt0, bt1], writes=[bMT[m]])

    wout_v = dr["w_out"].rearrange("(c p) n -> p c n", p=128)
    bWO = Buf("wout")
    for c in range(16):
        S.dma("pool", lambda e, c=c: e.dma_start(out=AR(c * 2048, 2048), in_=wout_v[:, c, :]),
              reads=[], writes=[bA[0], bA[1], bWO], sem_buf=bWO, same_gen=(c > 0))
    lnp = ARENA[:, 32768:40960].bitcast(F32).rearrange("p (a d) -> p a d", a=2)
    blnp = Buf("lnp")
    S.dma("sp", lambda e: e.dma_start(out=lnp, in_=dr["ln1"]), reads=[], writes=[blnp], sem_buf=blnp)
    xrows = dr["xrows"].rearrange("(t p) d -> p t d", p=128)
    for tt in range(8):
        S.dma("sp", lambda e, tt=tt: e.dma_start(out=ACC[:, tt, :], in_=xrows[:, tt, :]),
              writes=[bACC[tt], bX, bY], sem_buf=bACC[tt], same_gen=(tt > 0))
    hb = 0
    for tt in range(8):
        for dg in range(4):
            bank = 4 + (hb % 2)
            hb += 1
            for m in range(16):
                op("pe", lambda e, bank=bank, m=m, tt=tt, dg=dg: e.matmul(
                    pb[bank][:], lhsT=MT[:, m, tt * 128:(tt + 1) * 128], rhs=AR(m * 2048 + dg * 512, 512),
                    start=(m == 0), stop=(m == 15)), reads=[bMT[m], bWO], writes=[pq[bank]])
            op("dve", lambda e, bank=bank, tt=tt, dg=dg: e.scalar_tensor_tensor(
                out=ACC[:, tt, dg * 512:(dg + 1) * 512], in0=ACC[:, tt, dg * 512:(dg + 1) * 512], scalar=ALPHA,
                in1=pb[bank][:], op0=ALU.mult, op1=ALU.add), reads=[bACC[tt], pq[bank]], writes=[bACC[tt]])

    def layer_norm(tt, prm, bprm):
        mv, bmv = small["mv"]; rstd, brstd = small["rstd"]
        for q in range(4):
            op("dve", lambda e, q=q, tt=tt: e.bn_stats(out=st6[:, q, :], in_=ACC[:, tt, q * 512:(q + 1) * 512]),
               reads=[bACC[tt]], writes=[bst6])
        op("dve", lambda e: e.bn_aggr(out=mv[:, 0:2], in_=st6[:].rearrange("p a b -> p (a b)")), reads=[bst6], writes=[bmv])
        op("act", lambda e: e.activation(out=rstd[:, 0:1], in_=mv[:, 1:2], func=AF.Ln, bias=epsc[:, 0:1]),
           reads=[bmv, bepsc], writes=[brstd])
        op("act", lambda e: e.activation(out=rstd[:, 0:1], in_=rstd[:, 0:1], func=AF.Exp, scale=-0.5),
           reads=[brstd], writes=[brstd])
        op("dve", lambda e, tt=tt: e.tensor_scalar(out=ACC[:, tt, :], in0=ACC[:, tt, :], scalar1=mv[:, 0:1],
                                                   scalar2=rstd[:, 0:1], op0=ALU.subtract, op1=ALU.mult),
           reads=[bACC[tt], bmv, brstd], writes=[bACC[tt]])
        op("pool", lambda e, tt=tt: e.tensor_tensor(out=ACC[:, tt, :], in0=ACC[:, tt, :], in1=prm[:, 0, :], op=ALU.mult),
           reads=[bACC[tt], bprm], writes=[bACC[tt]])
        op("pool", lambda e, tt=tt: e.tensor_tensor(out=ACC[:, tt, :], in0=ACC[:, tt, :], in1=prm[:, 1, :], op=ALU.add),
           reads=[bACC[tt], bprm], writes=[bACC[tt]])

    for tt in range(8):
        layer_norm(tt, lnp, blnp)
        xf, bX1F = WGT[tt % 2]
        X1F = xf[:].bitcast(F32)
        for c4 in range(4):
            bank = 6 + (c4 % 2)
            for k in range(4):
                c = c4 * 4 + k
                op("pe", lambda e, bank=bank, k=k, c=c, tt=tt: e.transpose(
                    pb[bank][:, k * 128:(k + 1) * 128], ACC[:, tt, c * 128:(c + 1) * 128], identf[:]),
                    reads=[bACC[tt], bident], writes=[pq[bank]])
            op("act", lambda e, bank=bank, c4=c4, tt=tt: e.activation(
                out=MT[:, c4 * 4:(c4 + 1) * 4, tt * 128:(tt + 1) * 128],
                in_=pb[bank][:].rearrange("p (k t) -> p k t", k=4), func=AF.Copy),
                reads=[pq[bank]], writes=[bMT[c4 * 4 + k] for k in range(4)])
            op("dve", lambda e, bank=bank, c4=c4, X1F=X1F: e.tensor_copy(out=X1F[:, c4 * 512:(c4 + 1) * 512], in_=pb[bank][:]),
               reads=[pq[bank]], writes=[bX1F])
        for c in range(16):
            op("pe", lambda e, c=c, X1F=X1F: e.matmul(pb[0][:, 0:64], lhsT=X1F[:, c * 128:(c + 1) * 128], rhs=wr[:, c, :],
                                             start=(c == 0), stop=(c == 15)), reads=[bX1F, bwr], writes=[pq[0]])
        m8, bm8 = small["m8"]; gs, bgs = small["gs"]; g8, bg8 = small["g8"]; gm, bgm = small["gm"]
        t8, bt8 = small["t8"]; den, bden = small["den"]
        op("act", lambda e: e.activation(out=scs[:], in_=pb[0][:, 0:64], func=AF.Sigmoid), reads=[pq[0]], writes=[bscs])
        op("dve", lambda e: e.tensor_tensor(out=ch[:], in0=scs[:], in1=rb[:], op=ALU.add), reads=[bscs, brb], writes=[bch])
        for g in range(8):
            op("dve", lambda e, g=g: e.max(out=m8[:], in_=ch[:, g * 8:(g + 1) * 8]), reads=[bch], writes=[bm8])
            op("dve", lambda e, g=g: e.tensor_tensor(out=gs[:, g:g + 1], in0=m8[:, 0:1], in1=m8[:, 1:2], op=ALU.add),
               reads=[bm8], writes=[bgs])
        op("dve", lambda e: e.max(out=g8[:], in_=gs[:]), reads=[bgs], writes=[bg8])
        op("dve", lambda e: e.tensor_scalar(out=gm[:], in0=gs[:], scalar1=g8[:, 3:4], scalar2=None, op0=ALU.is_ge),
           reads=[bgs, bg8], writes=[bgm])
        op("dve", lambda e: e.tensor_scalar(out=gm[:], in0=gm[:], scalar1=-1.0, scalar2=1e30, op0=ALU.add, op1=ALU.mult),
           reads=[bgm], writes=[bgm])
        for g in range(8):
            op("dve", lambda e, g=g: e.tensor_scalar(out=mc[:, g * 8:(g + 1) * 8], in0=ch[:, g * 8:(g + 1) * 8],
                                                     scalar1=gm[:, g:g + 1], scalar2=None, op0=ALU.add),
               reads=[bch, bgm], writes=[bmc])
        op("dve", lambda e: e.max(out=t8[:], in_=mc[:]), reads=[bmc], writes=[bt8])
        op("dve", lambda e: e.tensor_scalar(out=mc[:], in0=mc[:], scalar1=t8[:, 7:8], scalar2=None, op0=ALU.is_ge),
           reads=[bmc, bt8], writes=[bmc])
        op("dve", lambda e: e.tensor_tensor(out=mc[:], in0=mc[:], in1=scs[:], op=ALU.mult), reads=[bmc, bscs], writes=[bmc])
        op("dve", lambda e: e.tensor_reduce(out=den[:, 0:1], in_=mc[:], axis=AX.X, op=ALU.add), reads=[bmc], writes=[bden])
        op("dve", lambda e: e.reciprocal(out=den[:, 0:1], in_=den[:, 0:1]), reads=[bden], writes=[bden])
        op("dve", lambda e, tt=tt: e.tensor_scalar(out=Gt[:, tt, :], in0=mc[:], scalar1=den[:, 0:1], scalar2=2.5,
                                                   op0=ALU.mult, op1=ALU.mult), reads=[bmc, bden], writes=[bGt])
        op("act", lambda e, tt=tt: e.activation(out=ACC[:, tt, :], in_=ACC[:, tt, :], func=AF.Copy, scale=ALPHA),
           reads=[bACC[tt]], writes=[bACC[tt]])

    bEW = [Buf("ew0"), Buf("ew1")]
    bWD = Buf("ewd")
    yb_ctr = 0
    for e_i in range(n_exp + 1):
        base = (e_i % 2) * 16384
        bew = bEW[e_i % 2]
        if e_i < n_exp:
            srcs = [dr["w_gate"][e_i].rearrange("(c p) f -> p c f", p=128), dr["w_up"][e_i].rearrange("(c p) f -> p c f", p=128),
                    dr["w_down"][e_i].rearrange("(c p) n -> p c n", p=128)]
        else:
            srcs = [dr["ws_gate"].rearrange("(c p) f -> p c f", p=128), dr["ws_up"].rearrange("(c p) f -> p c f", p=128),
                    dr["ws_down"].rearrange("(c p) n -> p c n", p=128)]
        extra_w = [bA[0], bA[1], bWO, blnp] if e_i < 2 else []
        for wi in range(2):
            dstv = ARENA[:, base + wi * 8192:base + (wi + 1) * 8192].rearrange("p (c f) -> p c f", c=16)
            S.dma("pool", lambda e, dstv=dstv, src=srcs[wi]: e.dma_start(out=dstv, in_=src),
                  writes=[bew] + extra_w, sem_buf=bew, same_gen=(wi > 0))
        dstv = ARENA[:, 32768:40960].rearrange("p (c n) -> p c n", c=4)
        S.dma("pool", lambda e, dstv=dstv, src=srcs[2]: e.dma_start(out=dstv, in_=src),
              writes=[bWD] + extra_w, sem_buf=bWD)
        hT, bhT = WGT[e_i % 2]
        sg_, bsg = tmpf[0]
        for half in range(2):
            for fc in range(4):
                for gu in range(2):
                    bank = gu * 2 + (fc % 2)
                    for c in range(16):
                        op("pe", lambda e, bank=bank, gu=gu, fc=fc, c=c, base=base, half=half: e.matmul(
                            pb[bank][:], lhsT=AR(base + gu * 8192 + c * 512 + fc * 128, 128),
                            rhs=MT[:, c, half * 512:(half + 1) * 512], start=(c == 0), stop=(c == 15)),
                            reads=[bew, bMT[c]], writes=[pq[bank]])
                bg_, bu_ = fc % 2, 2 + (fc % 2)
                op("act", lambda e, bg_=bg_: e.activation(out=sg_[:], in_=pb[bg_][:], func=AF.Silu), reads=[pq[bg_]], writes=[bsg])
                op("dve", lambda e, bu_=bu_, hT=hT, fc=fc, half=half: e.tensor_tensor(
                    out=hT[:, fc * 1024 + half * 512:fc * 1024 + (half + 1) * 512], in0=sg_[:], in1=pb[bu_][:], op=ALU.mult),
                    reads=[bsg, pq[bu_]], writes=[bhT])
        for tt in range(8):
            for dg in range(4):
                bank = 4 + (yb_ctr % 4)
                yb_ctr += 1
                for fc in range(4):
                    op("pe", lambda e, bank=bank, fc=fc, tt=tt, dg=dg, hT=hT, base=base: e.matmul(
                        pb[bank][:], lhsT=hT[:, fc * 1024 + tt * 128:fc * 1024 + (tt + 1) * 128],
                        rhs=AR(32768 + fc * 2048 + dg * 512, 512), start=(fc == 0), stop=(fc == 3)),
                        reads=[bhT, bWD], writes=[pq[bank]])
                if e_i < n_exp:
                    op("dve", lambda e, bank=bank, tt=tt, dg=dg, e_i=e_i: e.scalar_tensor_tensor(
                        out=ACC[:, tt, dg * 512:(dg + 1) * 512], in0=pb[bank][:], scalar=Gt[:, tt, e_i:e_i + 1],
                        in1=ACC[:, tt, dg * 512:(dg + 1) * 512], op0=ALU.mult, op1=ALU.add),
                        reads=[pq[bank], bGt, bACC[tt]], writes=[bACC[tt]])
                else:
                    op("dve", lambda e, bank=bank, tt=tt, dg=dg: e.tensor_tensor(
                        out=ACC[:, tt, dg * 512:(dg + 1) * 512], in0=pb[bank][:], in1=ACC[:, tt, dg * 512:(dg + 1) * 512],
                        op=ALU.add), reads=[pq[bank], bACC[tt]], writes=[bACC[tt]])

    ln2base = ((n_exp + 1) % 2) * 16384
    lnp2 = ARENA[:, ln2base:ln2base + 8192].bitcast(F32).rearrange("p (a d) -> p a d", a=2)
    blnp2 = Buf("lnp2")
    S.dma("sp", lambda e: e.dma_start(out=lnp2, in_=dr["ln2"]), writes=[bEW[(n_exp + 1) % 2], blnp2], sem_buf=blnp2)
    b_out = Buf("outB")
    outv = dr["out"].rearrange("(t p) d -> p t d", p=128)
    for tt in range(8):
        layer_norm(tt, lnp2, blnp2)
        S.dma("sp", lambda e, tt=tt: e.dma_start(out=outv[:, tt, :], in_=ACC[:, tt, :]), reads=[bACC[tt]], writes=[b_out],
              sem_buf=bACC[tt], same_gen=True)
    return [b_out]


def host_prep_b(inp, yall):
    x = inp["x"][0]
    w_in = inp["w_in"][0]
    ga = w_in[:, 7184:7184 + 2048].reshape(16, 128, 16, 128)
    gb = w_in[:, 9232:9232 + 2048].reshape(16, 128, 16, 128)
    wg = np.stack([ga, gb], axis=3)
    wgates = np.ascontiguousarray(wg.transpose(2, 1, 0, 3, 4).reshape(16, 128, 16 * 256))
    common = {
        "wgates": wgates, "yall": yall,
        "w_branch_a": inp["w_branch_a"][0], "w_branch_b": inp["w_branch_b"][0], "w_out": inp["w_out"][0],
        "ln1": np.ascontiguousarray(np.broadcast_to(np.stack([inp["ln1_g"][0], inp["ln1_b"][0]])[None], (128, 2, D))),
        "ln2": np.ascontiguousarray(np.broadcast_to(np.stack([inp["ln2_g"][0], inp["ln2_b"][0]])[None], (128, 2, D))),
        "rbias": np.ascontiguousarray(np.broadcast_to(inp["router_bias"][0][None], (128, 64))),
        "w_router": inp["w_router"][0],
        "w_gate": inp["w_gate"][0], "w_up": inp["w_up"][0], "w_down": inp["w_down"][0],
        "ws_gate": inp["ws_gate"][0], "ws_up": inp["ws_up"][0], "ws_down": inp["ws_down"][0],
    }
    maps = []
    xT = None
    for c in range(NCORE):
        xr = np.ascontiguousarray(x[c * TPC:(c + 1) * TPC])
        m = dict(common)
        m["xrows"] = xr
        m["xTs"] = np.ascontiguousarray(xr.T)
        maps.append(m)
    return maps


def declare_b(nc, dr, n_exp=N_EXP, fused=False):
    def din(name, shape, dt=F32):
        dr[name] = nc.dram_tensor(name, list(shape), dt, kind="ExternalInput").ap()
    din("wgates", [16, 128, 4096])
    din("w_branch_a", [1024, D]); din("w_branch_b", [1024, D]); din("w_out", [D, D])
    din("ln1", [128, 2, D]); din("ln2", [128, 2, D]); din("rbias", [128, 64]); din("w_router", [D, 64])
    din("w_gate", [64, D, 512]); din("w_up", [64, D, 512]); din("w_down", [64, 512, D])
    din("ws_gate", [D, 512]); din("ws_up", [D, 512]); din("ws_down", [512, D])
    din("xrows", [TPC, D]); din("xTs", [D, TPC])
    dr["out"] = nc.dram_tensor("out", [TPC, D], F32, kind="ExternalOutput").ap()


def build_nc_b(n_exp=N_EXP):
    nc = bass.Bass("TRN2", target_bir_lowering=False)
    dr = {}
    declare_b(nc, dr, n_exp)
    dr["yall"] = nc.dram_tensor("yall", [2048, TPC], BF16, kind="ExternalInput").ap()
    dr["tok0"] = 0
    S = Sched(nc)
    outs = build_phase_b(nc, S, dr, n_exp=n_exp)
    S.wait_final("sp", outs)
    S.emit()
    return nc, S


def _run_two_launch(inputs):
    maps_a = host_prep_a(inputs)
    nc_a, _ = build_nc_a()
    res_a = run_bass_kernel_spmd(nc_a, maps_a, core_ids=list(range(NCORE)))
    rows = []
    for h in range(NCORE):
        rows.append(np.asarray(res_a.results[h]["yaT"]))
        rows.append(np.asarray(res_a.results[h]["ybT"]))
    yall = np.concatenate(rows, axis=0)
    maps_b = host_prep_b(inputs, None)
    for c in range(NCORE):
        maps_b[c]["yall"] = np.ascontiguousarray(yall[:, c * TPC:(c + 1) * TPC])
    nc_b, _ = build_nc_b()
    res_b = run_bass_kernel_spmd(nc_b, maps_b, core_ids=list(range(NCORE)))
    out = np.concatenate([np.asarray(res_b.results[c]["out"]) for c in range(NCORE)], axis=0)
    return out.reshape(1, T, D).astype(np.float32)


def kernel(**inputs):
    inputs = {k: np.asarray(v) for k, v in inputs.items()}
    return _run_two_launch(inputs)
```

```python
import numpy as np
import ml_dtypes
import concourse.bass as bass
import concourse.mybir as mybir
from concourse.bass_utils import run_bass_kernel_spmd

F32 = mybir.dt.float32
BF16 = mybir.dt.bfloat16
I32 = mybir.dt.int32
AF = mybir.ActivationFunctionType
ALU = mybir.AluOpType
AX = mybir.AxisListType

T = 8192
D = 2048
NCORE = 8
ENGS = ["pe", "act", "dve", "pool", "sp"]


class Buf:
    __slots__ = ("name", "writes", "reads", "sem", "cnt", "excl")

    def __init__(self, name, excl=False):
        self.name = name
        self.excl = excl
        self.writes = []
        self.reads = []
        self.sem = None
        self.cnt = 0


class Sched:
    def __init__(self, nc):
        self.nc = nc
        self.prog = {e: [] for e in ENGS}
        self.sem = {e: nc.alloc_semaphore(name=f"s_{e}") for e in ENGS if e != "sp"}
        self.cnt = {e: 0 for e in ENGS}
        self.seen = {e: {} for e in ENGS}
        self.n_sems = 4

    def _waits(self, eng, reads, writes, same_gen=False):
        toks = []
        for b in reads:
            toks.extend(b.writes)
        for b in writes:
            toks.extend(b.reads)
            if not same_gen:
                toks.extend(b.writes)
        need = {}
        for (sem, val, src) in toks:
            if src == "pe" and eng == "pe":
                continue
            k = id(sem)
            if self.seen[eng].get(k, 0) >= val:
                continue
            if k not in need or need[k][1] < val:
                need[k] = (sem, val)
        out = []
        for k, (sem, val) in need.items():
            self.seen[eng][k] = val
            out.append((sem, val))
        return out

    def _commit(self, tok, reads, writes, same_gen=False):
        for b in reads:
            b.reads.append(tok)
            if len(b.reads) > 64:
                b.reads = b.reads[-64:] if False else b.reads
        for b in writes:
            if same_gen:
                b.writes.append(tok)
            else:
                b.writes = [tok]
                b.reads = []

    def op(self, eng, fn, reads=(), writes=()):
        self.nops = getattr(self, "nops", 0) + 1
        if self.nops > getattr(self, "max_ops", 10 ** 9):
            return None
        ex = [b for b in reads if b.excl]
        if ex:
            reads = [b for b in reads if not b.excl]
            writes = list(writes) + ex
        waits = self._waits(eng, reads, writes)
        self.cnt[eng] += 1
        tok = (self.sem[eng], self.cnt[eng], eng)
        self.prog[eng].append((waits, fn, (self.sem[eng], 1)))
        self._commit(tok, reads, writes)
        return tok

    def dma(self, q, fn, reads=(), writes=(), sem_buf=None, same_gen=False, inc=16):
        self.nops = getattr(self, "nops", 0) + 1
        if self.nops > getattr(self, "max_ops", 10 ** 9):
            return None
        waits = self._waits(q, reads, writes, same_gen=same_gen)
        if sem_buf.sem is None:
            sem_buf.sem = self.nc.alloc_semaphore(name=f"d_{sem_buf.name}")
            self.n_sems += 1
        sem_buf.cnt += inc
        tok = (sem_buf.sem, sem_buf.cnt, "dma")
        self.prog[q].append((waits, fn, (sem_buf.sem, inc)))
        self._commit(tok, reads, writes, same_gen=same_gen)
        return tok

    def wait_final(self, eng, bufs):
        need = {}
        for b in bufs:
            for (sem, val, src) in list(b.writes) + list(b.reads):
                k = id(sem)
                if k not in need or need[k][1] < val:
                    need[k] = (sem, val)
        self.prog[eng].append((list(need.values()), None, None))

    def emit(self):
        nc = self.nc
        handles = {"pe": "tensor", "act": "scalar", "dve": "vector", "pool": "gpsimd", "sp": "sync"}
        with nc.Block() as block:
            for e in ENGS:
                prog = self.prog[e]
                if not prog:
                    continue

                def body(eng, prog=prog):
                    for waits, fn, inc in prog:
                        for (sem, val) in waits:
                            eng.wait_ge(sem, val)
                        if fn is not None:
                            fn(eng).then_inc(inc[0], inc[1])

                getattr(block, handles[e])(body)


class Ctx:
    def __init__(self, nc, S):
        self.nc = nc
        self.S = S
        self.n = 0

    def sb(self, shape, dt, name=None):
        self.n += 1
        name = name or f"t{self.n}"
        t = self.nc.alloc_sbuf_tensor("sb_" + name, list(shape), dt)
        return t, Buf(name)


NT_A = 16
GDN_SQ = 6


def build_phase_a(nc, S, dr, n_tiles=NT_A, do_gdn=True, do_attn=True):
    C = Ctx(nc, S)
    cur = [None]

    def op(eng, fn, reads=(), writes=()):
        cur[0].append((0, eng, fn, tuple(reads), tuple(writes), None))

    def dma(q, fn, **kw):
        cur[0].append((1, q, fn, None, None, kw))

    def flush(lst):
        for (k, e, fn, r, w, kw) in lst:
            if k == 0:
                S.op(e, fn, reads=r, writes=w)
            else:
                Sched.dma(S, e, fn, **kw)

    def merge(a, b):
        out = []
        ia = ib = 0
        na, nb = len(a), len(b)
        while ia < na or ib < nb:
            if ib >= nb or (ia < na and ia * max(nb, 1) <= ib * max(na, 1)):
                out.append(a[ia]); ia += 1
            else:
                out.append(b[ib]); ib += 1
        return out
    setup_ops = []
    cur[0] = setup_ops

    xT = dr["xT"].rearrange("(c p) t -> p c t", p=128)
    W, bW = C.sb([128, 16, 898], BF16, "W")
    wa_v = dr["wA"].rearrange("(c p) n -> p c n", p=128)
    for c4 in range(4):
        b = Buf(f"Wl{c4}")
        dma("pool", lambda e, c4=c4: e.dma_start(out=W[:, c4 * 4:(c4 + 1) * 4, :], in_=wa_v[:, c4 * 4:(c4 + 1) * 4, :]),
              writes=[bW], sem_buf=bW, same_gen=True)
    misc, bmisc = C.sb([128, 8], F32, "misc")
    dma("sp", lambda e: e.dma_start(out=misc[:], in_=dr["miscA"]), writes=[bmisc], sem_buf=bmisc)
    cw, bcw = C.sb([128, 3, 4], F32, "cw")
    dma("sp", lambda e: e.dma_start(out=cw[:], in_=dr["convw"]), writes=[bcw], sem_buf=bcw)
    nrm, bnrm = C.sb([128, 2, 128], F32, "nrm")
    dma("sp", lambda e: e.dma_start(out=nrm[:], in_=dr["nrmw"]), writes=[bnrm], sem_buf=bnrm)
    lamt, blamt = C.sb([128, 2, 2, 64], F32, "lamt")
    dma("sp", lambda e: e.dma_start(out=lamt[:], in_=dr["lam"]), writes=[blamt], sem_buf=blamt)
    BT, bBT = C.sb([128, 2, 128], F32, "BT")
    dma("sp", lambda e: e.dma_start(out=BT[:], in_=dr["biasT"]), writes=[bBT], sem_buf=bBT)

    onesf, bones = C.sb([128, 128], F32, "onesf")
    op("pool", lambda e: e.memset(onesf[:], 1.0), writes=[bones])
    identf, bident = C.sb([128, 128], F32, "identf")
    op("pool", lambda e: e.affine_select(out=identf[:], in_=onesf[:], pattern=[[-1, 128]], compare_op=ALU.is_equal,
                                         fill=0.0, base=0, channel_multiplier=1), reads=[bones], writes=[bident])
    triu, btriu = C.sb([128, 128], F32, "triu")
    op("pool", lambda e: e.affine_select(out=triu[:], in_=onesf[:], pattern=[[1, 128]], compare_op=ALU.is_ge,
                                         fill=0.0, base=0, channel_multiplier=-1), reads=[bones], writes=[btriu])
    identb, bidentb = C.sb([128, 128], BF16, "identb")
    op("dve", lambda e: e.tensor_copy(out=identb[:], in_=identf[:]), reads=[bident], writes=[bidentb])

    epsc, bepsc = C.sb([128, 1], F32, "epsc")
    op("pool", lambda e: e.memset(epsc[:], 1e-6), writes=[bepsc])
    negA, bnegA = C.sb([128, 1], F32, "negA")
    op("act", lambda e: e.activation(out=negA[:], in_=misc[:, 0:1], func=AF.Exp), reads=[bmisc], writes=[bnegA])
    op("dve", lambda e: e.tensor_scalar(out=negA[:], in0=negA[:], scalar1=-1.0, scalar2=None, op0=ALU.mult),
       reads=[bnegA], writes=[bnegA])
    lprod, blprod = C.sb([128, 2, 64], F32, "lprod")
    op("dve", lambda e: e.tensor_tensor(out=lprod[:], in0=lamt[:, :, 0, :], in1=lamt[:, :, 1, :], op=ALU.mult),
       reads=[blamt], writes=[blprod])
    lsum, blsum = C.sb([128, 2], F32, "lsum")
    op("dve", lambda e: e.tensor_reduce(out=lsum[:], in_=lprod[:], axis=AX.X, op=ALU.add), reads=[blprod], writes=[blsum])
    op("act", lambda e: e.activation(out=lsum[:], in_=lsum[:], func=AF.Exp), reads=[blsum], writes=[blsum])
    lam, blam = C.sb([128, 1], F32, "lam")
    op("dve", lambda e: e.scalar_tensor_tensor(out=lam[:], in0=lsum[:, 0:1], scalar=0.2, in1=lsum[:, 1:2],
                                               op0=ALU.add, op1=ALU.subtract), reads=[blsum], writes=[blam])
    op("dve", lambda e: e.tensor_scalar(out=BT[:], in0=BT[:], scalar1=misc[:, 2:3], scalar2=None, op0=ALU.subtract),
       reads=[bBT, bmisc], writes=[bBT])
    op("dve", lambda e: e.memset(BT[64:128, 0, 0:64], -30000.0), writes=[bBT])
    op("dve", lambda e: e.tensor_scalar(out=nrm[:, 1, :], in0=nrm[:, 1, :], scalar1=0.8, scalar2=None, op0=ALU.mult),
       reads=[bnrm], writes=[bnrm])

    KT, _ = C.sb([128, T], BF16, "KT")
    bKT = [Buf(f"KT{j}") for j in range(NT_A)]
    VA, _ = C.sb([128, 64, 130], BF16, "VA")
    bVA = [Buf(f"VA{j}") for j in range(64)]
    op("pool", lambda e: e.memset(VA[:, :, 128:130], 1.0), writes=bVA)
    QT = [C.sb([128, 512], BF16, f"QT{i}") for i in range(2)]
    xt = [C.sb([128, 16, 512], BF16, f"xt{i}") for i in range(2)]
    cins = [C.sb([128, 3, 515], F32, f"cin{i}") for i in range(2)]
    cout, bcout = C.sb([128, 3, 512], F32, "cout")
    sqt, bsqt = C.sb([128, 512], F32, "sqt")
    qkn = [C.sb([128, 512], F32, f"qkn{i}") for i in range(2)]
    tms = [C.sb([128, 4, 130], F32, f"tm{i}") for i in range(2)]
    sc = {nm: C.sb([128, 4], F32, "sc_" + nm) for nm in
          ["x", "ax", "e", "l", "g", "beta", "gc", "gtot", "eg", "nbeg", "ekd", "glast", "nbeta", "tmp", "ss4", "rr4"]}
    Sst = [C.sb([128, 128], F32, f"Sst{i}") for i in range(2)]
    op("pool", lambda e: e.memset(Sst[0][0][:], 0.0), writes=[Sst[0][1]])
    ystage = [C.sb([128, 512], BF16, f"ystg{i}") for i in range(2)]

    pb = [nc.alloc_psum_tensor(f"pb{i}", [128, 512], F32) for i in range(8)]
    pq = [[Buf(f"pb{i}q{k}", excl=True) for k in range(4)] for i in range(8)]

    stream = ["proj"]
    BMAP = {"proj": {0: 0, 1: 0, 2: 0}, "attn": {7: 0, 3: 0, 4: 2, 5: 3, 6: 4},
            "gdn": {0: 7, 1: 7, 3: 5, 4: 6, 5: 7, 6: 5, 7: 6}}

    def preg(bank, c0, n):
        bank = BMAP[stream[0]][bank]
        return pb[bank][:, c0:c0 + n], [pq[bank][0]]
    g3, g4b, g5, g6, g7 = 5, 6, 7, 5, 6
    gxt = 1

    def mk(name, shape=(128, 128), dt=F32):
        return C.sb(list(shape), dt, name)
    G4 = {nm: mk("g4_" + nm, (128, 4, 128)) for nm in ["dg", "t1", "E1", "E2", "Erow", "M", "MT", "X", "XT", "AqkT", "kdec",
                                                       "ATm", "qd", "QeffT", "zs", "yb", "junk"]}
    G4["R"] = mk("g4_R", (128, 4, 256))
    nonesf, bnones = C.sb([128, 128], F32, "nonesf")
    op("pool", lambda e: e.memset(nonesf[:], -1.0), writes=[bnones])
    nrm4, bnrm4 = C.sb([128, 4, 128], F32, "nrm4")
    for s4 in range(4):
        op("pool", lambda e, s4=s4: e.tensor_copy(out=nrm4[:, s4, :], in_=nrm[:, 0, :]), reads=[bnrm], writes=[bnrm4])
    rr_t = {nm: C.sb([128, 1], F32, "rr_" + nm) for nm in ["ss", "rr", "r1", "r2", "ss2", "rr2"]}
    PT = [C.sb([128, 512], BF16, f"PT{i}") for i in range(4)]
    at_t2, bat_t2 = C.sb([128, 128], F32, "at_t2")
    at_a, bat_a = C.sb([128, 128], F32, "at_a")
    at_y, bat_y = C.sb([128, 128], F32, "at_y")
    at_junk, bat_junk = C.sb([128, 128], F32, "at_junk")

    b_out = Buf("outA")
    pt_ctr = [0]

    halo, bhalo = C.sb([128, 3, 3], F32, "halo")
    op("pool", lambda e: e.memset(halo[:], 0.0), writes=[bhalo])
    flush(setup_ops)
    Pl, Gl, Al = [], [], []
    for j in range(n_tiles):
        t0 = j * 512
        xtt, bxt = xt[j % 2]
        cin, bcin = cins[j % 2]
        tm, btm = tms[j % 2]
        Pl.append([]); Gl.append([]); Al.append([])
        cur[0] = Pl[j]
        stream[0] = "proj"
        for half in range(2):
            dma("pool", lambda e, xtt=xtt, half=half, t0=t0: e.dma_start(
                out=xtt[:, half * 8:(half + 1) * 8, :], in_=xT[:, half * 8:(half + 1) * 8, t0:t0 + 512]),
                writes=[bxt], sem_buf=bxt, same_gen=(half == 1))
        QTc, bQTc = QT[j % 2]
        for g in range(5):
            bank = g % 2
            ps, pbufs = preg(bank, 0, 512)
            for c in range(16):
                op("pe", lambda e, ps=ps, g=g, c=c, xtt=xtt: e.matmul(ps, lhsT=W[:, c, g * 128:(g + 1) * 128],
                                                                      rhs=xtt[:, c, :], start=(c == 0), stop=(c == 15)),
                   reads=[bW, bxt], writes=pbufs)
            if g == 0:
                op("act", lambda e, ps=ps, QTc=QTc: e.activation(out=QTc[:], in_=ps, func=AF.Copy, scale=0.125),
                   reads=pbufs, writes=[bQTc])
            elif g == 1:
                op("act", lambda e, ps=ps, t0=t0: e.activation(out=KT[:, t0:t0 + 512], in_=ps, func=AF.Copy),
                   reads=pbufs, writes=[bKT[j]])
            else:
                op("act", lambda e, ps=ps, g=g, cin=cin: e.activation(out=cin[:, g - 2, 3:515], in_=ps, func=AF.Copy),
                   reads=pbufs, writes=[bcin])
        for s in range(4):
            ps, pbufs = preg(2, 0, 258)
            for c in range(16):
                op("pe", lambda e, ps=ps, c=c, s=s, xtt=xtt: e.matmul(ps, lhsT=xtt[:, c, s * 128:(s + 1) * 128],
                                                                      rhs=W[:, c, 640:898], start=(c == 0), stop=(c == 15)),
                   reads=[bW, bxt], writes=pbufs)
            op("act", lambda e, s=s, j=j: e.activation(out=VA[:, 4 * j + s, 0:128], in_=pb[0][:, 0:128], func=AF.Copy),
               reads=pbufs, writes=[bVA[4 * j + s]])
            op("dve", lambda e, s=s, tm=tm: e.tensor_copy(out=tm[:, s, :], in_=pb[0][:, 128:258]), reads=pbufs, writes=[btm])

        cur[0] = Gl[j]
        stream[0] = "gdn"
        if do_gdn:
            op("dve", lambda e, cin=cin: e.tensor_copy(out=cin[:, :, 0:3], in_=halo[:]), reads=[bhalo], writes=[bcin])
            for i in range(3):
                op("dve", lambda e, i=i, cin=cin: e.tensor_scalar(out=cout[:, i, :], in0=cin[:, i, 0:512], scalar1=cw[:, i, 0:1],
                                                         scalar2=None, op0=ALU.mult), reads=[bcin, bcw], writes=[bcout])
                for jj in range(1, 4):
                    op("dve", lambda e, i=i, jj=jj, cin=cin: e.scalar_tensor_tensor(
                        out=cout[:, i, :], in0=cin[:, i, jj:jj + 512], scalar=cw[:, i, jj:jj + 1], in1=cout[:, i, :],
                        op0=ALU.mult, op1=ALU.add), reads=[bcin, bcw, bcout], writes=[bcout])
            op("dve", lambda e, cin=cin: e.tensor_copy(out=halo[:], in_=cin[:, :, 512:515]), reads=[bcin], writes=[bhalo])
            op("act", lambda e: e.activation(out=cout[:], in_=cout[:], func=AF.Silu), reads=[bcout], writes=[bcout])
            for i in range(2):
                qk, bqk = qkn[i]
                op("act", lambda e, i=i: e.activation(out=sqt[:], in_=cout[:, i, :], func=AF.Square),
                   reads=[bcout], writes=[bsqt])
                ps, pbufs = preg(i, 0, 512)
                op("pe", lambda e, ps=ps: e.matmul(ps, lhsT=onesf[:], rhs=sqt[:], start=True, stop=True),
                   reads=[bones, bsqt], writes=pbufs)
                op("act", lambda e, ps=ps: e.activation(out=sqt[:], in_=ps, func=AF.Ln, bias=epsc[:, 0:1]), reads=pbufs + [bepsc], writes=[bsqt])
                op("act", lambda e: e.activation(out=sqt[:], in_=sqt[:], func=AF.Exp, scale=-0.5), reads=[bsqt], writes=[bsqt])
                scl = (128.0 ** -0.5) if i == 0 else 1.0
                op("dve", lambda e, i=i, qk=qk, scl=scl: e.scalar_tensor_tensor(
                    out=qk[:], in0=cout[:, i, :], scalar=scl, in1=sqt[:], op0=ALU.mult, op1=ALU.mult),
                    reads=[bcout, bsqt], writes=[bqk])
            def sct(nm):
                return sc[nm][0], sc[nm][1]
            x_, bx_ = sct("x"); ax_, bax_ = sct("ax"); e_, be_ = sct("e"); l_, bl_ = sct("l"); g_, bg_ = sct("g")
            beta_, bbeta_ = sct("beta"); gc_, bgc_ = sct("gc"); gtot_, bgtot_ = sct("gtot"); eg_, beg_ = sct("eg")
            nbeg_, bnbeg_ = sct("nbeg"); ekd_, bekd_ = sct("ekd"); glast_, bglast_ = sct("glast")
            nbeta_, bnbeta_ = sct("nbeta"); tmp_, btmp_ = sct("tmp")
            op("dve", lambda e, tm=tm: e.tensor_scalar(out=x_[:], in0=tm[:, :, 128], scalar1=misc[:, 1:2], scalar2=None,
                                                op0=ALU.add), reads=[btm, bmisc], writes=[bx_])
            op("dve", lambda e: e.scalar_tensor_tensor(out=ax_[:], in0=x_[:], scalar=-1.0, in1=x_[:], op0=ALU.mult,
                                                       op1=ALU.max), reads=[bx_], writes=[bax_])
            op("act", lambda e: e.activation(out=e_[:], in_=ax_[:], func=AF.Exp, scale=-1.0), reads=[bax_], writes=[be_])
            op("act", lambda e: e.activation(out=l_[:], in_=e_[:], func=AF.Ln, bias=onesf[:, 0:1]), reads=[be_, bones], writes=[bl_])
            op("dve", lambda e: e.scalar_tensor_tensor(out=g_[:], in0=x_[:], scalar=0.0, in1=l_[:], op0=ALU.max,
                                                       op1=ALU.add), reads=[bx_, bl_], writes=[bg_])
            op("dve", lambda e: e.tensor_scalar(out=g_[:], in0=g_[:], scalar1=negA[:, 0:1], scalar2=None, op0=ALU.mult),
               reads=[bg_, bnegA], writes=[bg_])
            op("act", lambda e, tm=tm: e.activation(out=beta_[:], in_=tm[:, :, 129], func=AF.Sigmoid), reads=[btm], writes=[bbeta_])
            psg, pgb = preg(6, 384, 8)
            op("pe", lambda e: e.matmul(psg[:, 0:4], lhsT=triu[:], rhs=g_[:], start=True, stop=True),
               reads=[btriu, bg_], writes=pgb)
            op("pe", lambda e: e.matmul(psg[:, 4:8], lhsT=onesf[:], rhs=g_[:], start=True, stop=True),
               reads=[bones, bg_], writes=pgb)
            op("dve", lambda e: e.tensor_copy(out=gc_[:], in_=psg[:, 0:4]), reads=pgb, writes=[bgc_])
            op("dve", lambda e: e.tensor_copy(out=gtot_[:], in_=psg[:, 4:8]), reads=pgb, writes=[bgtot_])
            op("act", lambda e: e.activation(out=eg_[:], in_=gc_[:], func=AF.Exp), reads=[bgc_], writes=[beg_])
            op("act", lambda e: e.activation(out=glast_[:], in_=gtot_[:], func=AF.Exp), reads=[bgtot_], writes=[bglast_])
            op("dve", lambda e: e.tensor_tensor(out=tmp_[:], in0=gtot_[:], in1=gc_[:], op=ALU.subtract),
               reads=[bgtot_, bgc_], writes=[btmp_])
            op("act", lambda e: e.activation(out=ekd_[:], in_=tmp_[:], func=AF.Exp), reads=[btmp_], writes=[bekd_])
            op("dve", lambda e: e.tensor_scalar(out=nbeta_[:], in0=beta_[:], scalar1=-1.0, scalar2=None, op0=ALU.mult),
               reads=[bbeta_], writes=[bnbeta_])
            op("dve", lambda e: e.tensor_tensor(out=nbeg_[:], in0=nbeta_[:], in1=eg_[:], op=ALU.mult),
               reads=[bnbeta_, beg_], writes=[bnbeg_])

            qTn, bqTn = qkn[0]
            kTn, bkTn = qkn[1]

            def g4(nm):
                return G4[nm][0], G4[nm][1]
            dg, bdg = g4("dg"); t1, bt1 = g4("t1"); E1, bE1 = g4("E1"); E2, bE2 = g4("E2"); Erow, bErow = g4("Erow")
            M, bM = g4("M"); MT, bMT = g4("MT"); X, bX = g4("X"); XT, bXT = g4("XT"); AqkT, bAqkT = g4("AqkT")
            kdec, bkdec = g4("kdec"); ATm, bATm = g4("ATm"); qd, bqd = g4("qd"); QeffT, bQeffT = g4("QeffT")
            zs, bzs = g4("zs"); yb, byb = g4("yb"); junk, bjunk = g4("junk")
            R_, bR = G4["R"]

            def v4(bank):
                return pb[bank][:].rearrange("p (s c) -> p s c", s=4)
            for s in range(4):
                op("dve", lambda e, s=s: e.tensor_scalar(out=dg[:, s, :], in0=identf[:], scalar1=gc_[:, s:s + 1], scalar2=None,
                                                         op0=ALU.mult), reads=[bident, bgc_], writes=[bdg])
            for s in range(4):
                op("pe", lambda e, s=s: e.matmul(pb[g3][:, s * 128:(s + 1) * 128], lhsT=dg[:, s, :], rhs=onesf[:],
                                                 start=(s == 0), stop=False, skip_group_check=True),
                   reads=[bdg, bones], writes=[pq[g3][0]])
            for s in range(4):
                op("pe", lambda e, s=s: e.matmul(pb[g3][:, s * 128:(s + 1) * 128], lhsT=nonesf[:], rhs=dg[:, s, :],
                                                 start=False, stop=(s == 3), skip_group_check=True),
                   reads=[bdg, bnones], writes=[pq[g3][0]])
            for s in range(4):
                op("pe", lambda e, s=s: e.matmul(pb[g4b][:, s * 128:(s + 1) * 128], lhsT=onesf[:], rhs=dg[:, s, :],
                                                 start=(s == 0), stop=(s == 3), skip_group_check=True),
                   reads=[bdg, bones], writes=[pq[g4b][0]])
            op("dve", lambda e: e.tensor_scalar(out=t1[:], in0=v4(g3), scalar1=0.0, scalar2=None, op0=ALU.min),
               reads=[pq[g3][0]], writes=[bt1])
            op("act", lambda e: e.activation(out=E1[:], in_=t1[:], func=AF.Exp), reads=[bt1], writes=[bE1])
            op("pool", lambda e: e.affine_select(out=E1[:], in_=E1[:], pattern=[[0, 4], [-1, 128]], compare_op=ALU.is_gt,
                                                 fill=0.0, base=0, channel_multiplier=1), reads=[bE1], writes=[bE1])
            op("dve", lambda e: e.tensor_scalar(out=t1[:], in0=v4(g3), scalar1=0.0, scalar2=None, op0=ALU.max),
               reads=[pq[g3][0]], writes=[bt1])
            op("act", lambda e: e.activation(out=E2[:], in_=t1[:], func=AF.Exp, scale=-1.0), reads=[bt1], writes=[bE2])
            op("pool", lambda e: e.affine_select(out=E2[:], in_=E2[:], pattern=[[0, 4], [1, 128]], compare_op=ALU.is_ge,
                                                 fill=0.0, base=0, channel_multiplier=-1), reads=[bE2], writes=[bE2])
            op("act", lambda e: e.activation(out=Erow[:], in_=v4(g4b), func=AF.Exp), reads=[pq[g4b][0]], writes=[bErow])
            for s in range(4):
                cs = slice(s * 128, (s + 1) * 128)
                op("pe", lambda e, s=s, cs=cs: e.matmul(pb[g5][:, cs], lhsT=kTn[:, cs], rhs=kTn[:, cs], start=(s == 0),
                                                        stop=(s == 3), skip_group_check=True), reads=[bkTn], writes=[pq[g5][0]])
            for s in range(4):
                cs = slice(s * 128, (s + 1) * 128)
                op("pe", lambda e, s=s, cs=cs: e.matmul(pb[g6][:, cs], lhsT=kTn[:, cs], rhs=qTn[:, cs], start=(s == 0),
                                                        stop=(s == 3), skip_group_check=True), reads=[bkTn, bqTn], writes=[pq[g6][0]])
            for s in range(4):
                op("dve", lambda e, s=s: e.scalar_tensor_tensor(out=M[:, s, :], in0=pb[g5][:, s * 128:(s + 1) * 128],
                                                                scalar=nbeta_[:, s:s + 1], in1=E1[:, s, :], op0=ALU.mult,
                                                                op1=ALU.mult), reads=[pq[g5][0], bnbeta_, bE1], writes=[bM])
            op("dve", lambda e: e.tensor_tensor(out=AqkT[:], in0=v4(g6), in1=E2[:], op=ALU.mult), reads=[pq[g6][0], bE2], writes=[bAqkT])
            for s in range(4):
                op("pe", lambda e, s=s: e.transpose(pb[g7][:, s * 128:(s + 1) * 128], M[:, s, :], identf[:]),
                   reads=[bM, bident], writes=[pq[g7][0]])
            op("act", lambda e: e.activation(out=MT[:], in_=v4(g7), func=AF.Copy), reads=[pq[g7][0]], writes=[bMT])
            for s in range(4):
                cs = slice(s * 128, (s + 1) * 128)
                op("pe", lambda e, cs=cs: e.transpose(pb[g3][:, cs], kTn[:, cs], identf[:]), reads=[bkTn, bident], writes=[pq[g3][0]])
            for s in range(4):
                cs = slice(s * 128, (s + 1) * 128)
                op("pe", lambda e, cs=cs: e.transpose(pb[g4b][:, cs], cout[:, 2, cs], identf[:]), reads=[bcout, bident], writes=[pq[g4b][0]])
            for s in range(4):
                cs = slice(s * 128, (s + 1) * 128)
                op("dve", lambda e, s=s, cs=cs: e.tensor_scalar(out=kdec[:, s, :], in0=pb[g3][:, cs], scalar1=ekd_[:, s:s + 1],
                                                                scalar2=None, op0=ALU.mult), reads=[pq[g3][0], bekd_], writes=[bkdec])
                op("dve", lambda e, s=s, cs=cs: e.tensor_scalar(out=R_[:, s, 128:256], in0=pb[g3][:, cs], scalar1=nbeg_[:, s:s + 1],
                                                                scalar2=None, op0=ALU.mult), reads=[pq[g3][0], bnbeg_], writes=[bR])
                op("dve", lambda e, s=s, cs=cs: e.tensor_scalar(out=R_[:, s, 0:128], in0=pb[g4b][:, cs], scalar1=beta_[:, s:s + 1],
                                                                scalar2=None, op0=ALU.mult), reads=[pq[g4b][0], bbeta_], writes=[bR])
            Xc, bXc, XTc, bXTc = M, bM, MT, bMT
            for it in range(GDN_SQ + 1):
                for s in range(4):
                    bank = (g5, g6)[s // 2]
                    op("pe", lambda e, s=s, bank=bank, XTc=XTc: e.matmul(
                        pb[bank][:, (s % 2) * 256:(s % 2 + 1) * 256], lhsT=XTc[:, s, :], rhs=R_[:, s, :],
                        start=(s % 2 == 0), stop=(s % 2 == 1), skip_group_check=True), reads=[bXTc, bR], writes=[pq[bank][0]])
                for hb_ in range(2):
                    op("dve", lambda e, hb_=hb_: e.tensor_tensor(
                        out=R_[:, 2 * hb_:2 * hb_ + 2, :], in0=R_[:, 2 * hb_:2 * hb_ + 2, :],
                        in1=pb[(g5, g6)[hb_]][:].rearrange("p (s c) -> p s c", s=2), op=ALU.add),
                        reads=[bR, pq[(g5, g6)[hb_]][0]], writes=[bR])
                if it < GDN_SQ:
                    if it % 2 == 0:
                        Xn, bXn, XTn, bXTn = X, bX, XT, bXT
                    else:
                        Xn, bXn, XTn, bXTn = M, bM, MT, bMT
                    for s in range(4):
                        op("pe", lambda e, s=s, Xc=Xc, XTc=XTc: e.matmul(
                            pb[g7][:, s * 128:(s + 1) * 128], lhsT=XTc[:, s, :], rhs=Xc[:, s, :], start=(s == 0), stop=(s == 3),
                            skip_group_check=True), reads=[bXc, bXTc], writes=[pq[g7][0]])
                    for s in range(4):
                        op("pe", lambda e, s=s, Xc=Xc, XTc=XTc: e.matmul(
                            pb[gxt][:, s * 128:(s + 1) * 128], lhsT=Xc[:, s, :], rhs=XTc[:, s, :], start=(s == 0), stop=(s == 3),
                            skip_group_check=True), reads=[bXc, bXTc], writes=[pq[gxt][0]])
                    op("act", lambda e, Xn=Xn: e.activation(out=Xn[:], in_=v4(g7), func=AF.Copy), reads=[pq[g7][0]], writes=[bXn])
                    op("dve", lambda e, XTn=XTn: e.tensor_copy(out=XTn[:], in_=v4(gxt)), reads=[pq[gxt][0]], writes=[bXTn])
                    Xc, bXc, XTc, bXTc = Xn, bXn, XTn, bXTn
            for s in range(4):
                op("pe", lambda e, s=s: e.matmul(pb[g4b][:, s * 128:(s + 1) * 128], lhsT=R_[:, s, 128:256], rhs=kdec[:, s, :],
                                                 start=(s == 0), stop=(s == 3), skip_group_check=True),
                   reads=[bR, bkdec], writes=[pq[g4b][0]])
            for s in range(4):
                op("dve", lambda e, s=s: e.scalar_tensor_tensor(out=ATm[:, s, :], in0=identf[:], scalar=glast_[:, s:s + 1],
                                                                in1=pb[g4b][:, s * 128:(s + 1) * 128], op0=ALU.mult, op1=ALU.add),
                   reads=[bident, bglast_, pq[g4b][0]], writes=[bATm])
            for s in range(4):
                op("pe", lambda e, s=s: e.matmul(pb[g7][:, s * 128:(s + 1) * 128], lhsT=R_[:, s, 128:256], rhs=AqkT[:, s, :],
                                                 start=(s == 0), stop=(s == 3), skip_group_check=True),
                   reads=[bR, bAqkT], writes=[pq[g7][0]])
            op("pool", lambda e: e.tensor_tensor(out=qd[:], in0=qTn[:].rearrange("p (s c) -> p s c", s=4), in1=Erow[:],
                                                 op=ALU.mult), reads=[bqTn, bErow], writes=[bqd])
            op("dve", lambda e: e.tensor_tensor(out=QeffT[:], in0=qd[:], in1=v4(g7), op=ALU.add), reads=[bqd, pq[g7][0]], writes=[bQeffT])
            op("act", lambda e, tm=tm: e.activation(out=zs[:], in_=tm[:, :, 0:128], func=AF.Silu), reads=[btm], writes=[bzs])
            op("pool", lambda e: e.tensor_tensor(out=zs[:], in0=zs[:], in1=nrm4[:], op=ALU.mult), reads=[bzs, bnrm4], writes=[bzs])
            ss, bss = sc["ss4"]
            for s in range(4):
                n = 4 * j + s
                Scur, bScur = Sst[n % 2]
                Snxt, bSnxt = Sst[(n + 1) % 2]
                op("pe", lambda e, s=s, Scur=Scur: e.matmul(pb[g5][:, s * 128:(s + 1) * 128], lhsT=QeffT[:, s, :], rhs=Scur[:],
                                                            start=(s == 0), stop=False, skip_group_check=True),
                   reads=[bQeffT, bScur], writes=[pq[g5][0]])
                op("pe", lambda e, s=s: e.matmul(pb[g5][:, s * 128:(s + 1) * 128], lhsT=AqkT[:, s, :], rhs=R_[:, s, 0:128],
                                                 start=False, stop=True, skip_group_check=True),
                   reads=[bAqkT, bR], writes=[pq[g5][0]])
                op("pe", lambda e, s=s: e.matmul(pb[g6][:, 0:128], lhsT=kdec[:, s, :], rhs=R_[:, s, 0:128], start=True, stop=False),
                   reads=[bkdec, bR], writes=[pq[g6][0]])
                op("pe", lambda e, s=s, Scur=Scur: e.matmul(pb[g6][:, 0:128], lhsT=ATm[:, s, :], rhs=Scur[:], start=False, stop=True),
                   reads=[bATm, bScur], writes=[pq[g6][0]])
                op("act", lambda e, Snxt=Snxt: e.activation(out=Snxt[:], in_=pb[g6][:, 0:128], func=AF.Copy),
                   reads=[pq[g6][0]], writes=[bSnxt])
            for s in range(4):
                op("act", lambda e, s=s: e.activation(out=junk[:, 0, :], in_=pb[g5][:, s * 128:(s + 1) * 128], func=AF.Square,
                                                      accum_out=ss[:, s:s + 1]), reads=[pq[g5][0]], writes=[bjunk, bss])
            rr, brr = sc["rr4"]
            op("dve", lambda e: e.tensor_scalar(out=rr[:], in0=ss[:], scalar1=1.0 / 128, scalar2=1e-6, op0=ALU.mult, op1=ALU.add),
               reads=[bss], writes=[brr])
            op("act", lambda e: e.activation(out=rr[:], in_=rr[:], func=AF.Ln), reads=[brr], writes=[brr])
            op("act", lambda e: e.activation(out=rr[:], in_=rr[:], func=AF.Exp, scale=-0.5), reads=[brr], writes=[brr])
            for s in range(4):
                op("dve", lambda e, s=s: e.scalar_tensor_tensor(out=yb[:, s, :], in0=pb[g5][:, s * 128:(s + 1) * 128],
                                                                scalar=rr[:, s:s + 1], in1=zs[:, s, :], op0=ALU.mult, op1=ALU.mult),
                   reads=[pq[g5][0], brr, bzs], writes=[byb])
            for s in range(4):
                op("pe", lambda e, s=s: e.transpose(pb[g3][:, s * 128:(s + 1) * 128], yb[:, s, :], identf[:]),
                   reads=[byb, bident], writes=[pq[g3][0]])
            op("act", lambda e: e.activation(out=ystage[1][0][:], in_=pb[g3][:], func=AF.Copy), reads=[pq[g3][0]], writes=[ystage[1][1]])
            dma("sp", lambda e, t0=t0: e.dma_start(out=dr["ybT"][:, t0:t0 + 512], in_=ystage[1][0][:]),
                  reads=[ystage[1][1]], writes=[b_out], sem_buf=ystage[1][1], same_gen=True)

        cur[0] = Al[j]
        stream[0] = "attn"
        if do_attn:
            oacc = {}
            slots = [(4, 0), (4, 129), (4, 258), (5, 0), (5, 129), (5, 258), (6, 0), (6, 129)]
            for m in range(2):
                for s in range(4):
                    oacc[(m, s)] = slots[m * 4 + s]
            n_k = 4 * j + 4
            it = 0
            for i in range(n_k):
                s0 = max(0, i - 4 * j)
                q0 = s0 * 128
                for m in range(2):
                    bank = 7 if (it % 2 == 0) else 3
                    it += 1
                    pst, bpst = preg(bank, q0, 512 - q0)
                    ms = slice(m * 64, (m + 1) * 64)
                    near = [s for s in range(s0, 4) if (4 * j + s) - i <= 1]
                    op("pe", lambda e, pst=pst, ms=ms, i=i, q0=q0, QTc=QTc, near=near: e.matmul(
                        pst, lhsT=KT[ms, i * 128:(i + 1) * 128], rhs=QTc[ms, q0:512], start=True, stop=(len(near) == 0),
                        skip_group_check=True),
                        reads=[bKT[i // 4], bQTc], writes=bpst)
                    for idx, s in enumerate(near):
                        typ = 0 if (4 * j + s) == i else 1
                        op("pe", lambda e, bank=BMAP["attn"][bank], s=s, typ=typ, last=(idx == len(near) - 1): e.matmul(
                            pb[bank][:, s * 128:(s + 1) * 128], lhsT=identf[:], rhs=BT[:, typ, :], start=False, stop=last,
                            skip_group_check=True), reads=[bident, bBT], writes=bpst)
                    ptt, bptt = PT[pt_ctr[0] % 4]
                    pt_ctr[0] += 1
                    op("act", lambda e, pst=pst, ptt=ptt, q0=q0: e.activation(out=ptt[:, q0:512], in_=pst, func=AF.Exp,
                                                                              bias=misc[:, 2:3]),
                       reads=bpst + [bmisc], writes=[bptt])
                    for s in range(s0, 4):
                        bk, c0 = oacc[(m, s)]
                        po, bpo = preg(bk, c0, 129)
                        last_k = 4 * j + s
                        op("pe", lambda e, po=po, ptt=ptt, s=s, i=i, last_k=last_k, c0=c0: e.matmul(
                            po, lhsT=ptt[:, s * 128:(s + 1) * 128], rhs=VA[:, i, 0:129], start=(i == 0 and c0 == 0), stop=(i == last_k),
                            skip_group_check=True), reads=[bptt, bVA[i]], writes=bpo)
            for s in range(4):
                b1, c1 = oacc[(0, s)]
                b2, c2 = oacc[(1, s)]
                po1, bpo1 = preg(b1, c1, 129)
                po2, bpo2 = preg(b2, c2, 129)
                r1, br1 = rr_t["r1"]; r2, br2 = rr_t["r2"]; ss2, bss2 = rr_t["ss2"]; rr2, brr2 = rr_t["rr2"]
                op("dve", lambda e, po1=po1: e.reciprocal(out=r1[:], in_=po1[:, 128:129]), reads=bpo1, writes=[br1])
                op("dve", lambda e, po2=po2: e.reciprocal(out=r2[:], in_=po2[:, 128:129]), reads=bpo2, writes=[br2])
                op("dve", lambda e: e.tensor_tensor(out=r2[:], in0=r2[:], in1=lam[:], op=ALU.mult), reads=[br2, blam], writes=[br2])
                op("dve", lambda e, po2=po2: e.tensor_scalar(out=at_t2[:], in0=po2[:, 0:128], scalar1=r2[:, 0:1], scalar2=None,
                                                             op0=ALU.mult), reads=bpo2 + [br2], writes=[bat_t2])
                op("dve", lambda e, po1=po1: e.scalar_tensor_tensor(out=at_a[:], in0=po1[:, 0:128], scalar=r1[:, 0:1],
                                                                    in1=at_t2[:], op0=ALU.mult, op1=ALU.subtract),
                   reads=bpo1 + [br1, bat_t2], writes=[bat_a])
                op("act", lambda e: e.activation(out=at_junk[:], in_=at_a[:], func=AF.Square, accum_out=ss2[:]),
                   reads=[bat_a], writes=[bat_junk, bss2])
                op("dve", lambda e: e.tensor_scalar(out=rr2[:], in0=ss2[:], scalar1=1.0 / 128, scalar2=1e-6, op0=ALU.mult,
                                                    op1=ALU.add), reads=[bss2], writes=[brr2])
                op("act", lambda e: e.activation(out=rr2[:], in_=rr2[:], func=AF.Ln), reads=[brr2], writes=[brr2])
                op("act", lambda e: e.activation(out=rr2[:], in_=rr2[:], func=AF.Exp, scale=-0.5), reads=[brr2], writes=[brr2])
                op("dve", lambda e: e.scalar_tensor_tensor(out=at_y[:], in0=at_a[:], scalar=rr2[:, 0:1], in1=nrm[:, 1, :],
                                                           op0=ALU.mult, op1=ALU.mult), reads=[bat_a, brr2, bnrm], writes=[bat_y])
                pY, bpY = preg(6, 258, 128)
                op("pe", lambda e, pY=pY: e.transpose(pY, at_y[:], identf[:]), reads=[bat_y, bident], writes=bpY)
                op("act", lambda e, pY=pY, s=s: e.activation(out=ystage[0][0][:, s * 128:(s + 1) * 128], in_=pY, func=AF.Copy),
                   reads=bpY, writes=[ystage[0][1]])
            dma("sp", lambda e, t0=t0: e.dma_start(out=dr["yaT"][:, t0:t0 + 512], in_=ystage[0][0][:]),
                  reads=[ystage[0][1]], writes=[b_out], sem_buf=ystage[0][1], same_gen=True)
    flush(Pl[0])
    for j in range(n_tiles):
        nxt = Pl[j + 1] if j + 1 < n_tiles else []
        flush(merge(Gl[j], Al[j] + nxt))
    return [b_out]


def host_prep_a(inp):
    x = inp["x"][0]
    w_in = inp["w_in"][0]
    xT = np.ascontiguousarray(x.T)
    conv_w = inp["conv_w"][0]
    table = inp["rel_bias_table"]
    nb = 16
    ki = np.arange(128)[:, None]
    qi = np.arange(128)[None, :]

    def bucket(rel):
        base = np.where(rel > 0, nb, 0)
        n = np.abs(rel)
        max_exact = nb // 2
        nf = np.maximum(n, 1).astype(np.float32)
        large = max_exact + (np.log(nf / np.float32(max_exact)) / np.float32(np.log(128 / max_exact))
                             * np.float32(nb - max_exact)).astype(np.int32)
        large = np.minimum(large, nb - 1)
        return base + np.where(n < max_exact, n, large)
    bk = np.stack([bucket(ki - qi), bucket(ki - 128 - qi)], axis=1)
    maps = []
    for h in range(NCORE):
        cols = np.concatenate([
            np.arange(h * 128, h * 128 + 128),
            1024 + np.arange(h * 128, h * 128 + 128),
            3072 + np.arange(h * 128, h * 128 + 128),
            4096 + np.arange(h * 128, h * 128 + 128),
            5120 + np.arange(h * 128, h * 128 + 128),
            2048 + np.arange(h * 128, h * 128 + 128),
            6144 + np.arange(h * 128, h * 128 + 128),
            np.array([7168 + h, 7176 + h]),
        ])
        wA = np.ascontiguousarray(w_in[:, cols])
        misc = np.zeros((128, 8), np.float32)
        misc[:, 0] = inp["gdn_a_log"][0, h]
        misc[:, 1] = inp["gdn_dt_bias"][0, h]
        misc[:, 2] = table[15, h]
        cw = np.stack([conv_w[:, i * 1024 + h * 128:i * 1024 + (h + 1) * 128].T for i in range(3)], axis=1)
        nrmw = np.stack([np.broadcast_to(inp["gdn_norm_w"][0], (128, 128)),
                         np.broadcast_to(inp["diff_subln_w"][0], (128, 128))], axis=1)
        lamr = np.broadcast_to(inp["diff_lambda"][0].reshape(1, 2, 2, 64), (128, 2, 2, 64))
        biasT = table[:, h][bk]
        maps.append({"xT": xT, "wA": wA, "miscA": misc, "convw": np.ascontiguousarray(cw, dtype=np.float32),
                     "nrmw": np.ascontiguousarray(nrmw, dtype=np.float32),
                     "lam": np.ascontiguousarray(lamr, dtype=np.float32),
                     "biasT": np.ascontiguousarray(biasT, dtype=np.float32)})
    return maps


def build_nc_a(n_tiles=NT_A, do_gdn=True, do_attn=True, max_ops=10 ** 9):
    nc = bass.Bass("TRN2", target_bir_lowering=False)
    dr = {}
    dr["xT"] = nc.dram_tensor("xT", [D, T], F32, kind="ExternalInput").ap()
    dr["wA"] = nc.dram_tensor("wA", [D, 898], F32, kind="ExternalInput").ap()
    dr["miscA"] = nc.dram_tensor("miscA", [128, 8], F32, kind="ExternalInput").ap()
    dr["convw"] = nc.dram_tensor("convw", [128, 3, 4], F32, kind="ExternalInput").ap()
    dr["nrmw"] = nc.dram_tensor("nrmw", [128, 2, 128], F32, kind="ExternalInput").ap()
    dr["lam"] = nc.dram_tensor("lam", [128, 2, 2, 64], F32, kind="ExternalInput").ap()
    dr["biasT"] = nc.dram_tensor("biasT", [128, 2, 128], F32, kind="ExternalInput").ap()
    dr["yaT"] = nc.dram_tensor("yaT", [128, T], BF16, kind="ExternalOutput").ap()
    dr["ybT"] = nc.dram_tensor("ybT", [128, T], BF16, kind="ExternalOutput").ap()
    S = Sched(nc)
    S.max_ops = max_ops
    outs = build_phase_a(nc, S, dr, n_tiles=n_tiles, do_gdn=do_gdn, do_attn=do_attn)
    S.wait_final("sp", outs)
    S.emit()
    return nc, S


TPC = T // NCORE
ALPHA = 2.0 ** 0.25
N_EXP = 64


def build_phase_b(nc, S, dr, n_exp=N_EXP, yall_bufs=()):
    C = Ctx(nc, S)
    op = S.op
    yall_bufs = list(yall_bufs)
    ARENA, _ = C.sb([128, 40960], BF16, "arena")
    bA = [Buf("arena0"), Buf("arena1"), Buf("arena2")]
    MT, _ = C.sb([128, 16, 1024], BF16, "MT")
    bMT = [Buf(f"MT{i}") for i in range(16)]
    ACC, _ = C.sb([128, 8, 2048], F32, "ACC")
    bACC = [Buf(f"ACC{i}") for i in range(8)]
    WGT = [C.sb([128, 4096], BF16, f"wgt{i}") for i in range(2)]
    tmpf = [C.sb([128, 512], F32, f"tmpf{i}") for i in range(2)]
    Gt, bGt = C.sb([128, 8, 64], F32, "Gt")
    identf, bident = C.sb([128, 128], F32, "identfB")
    onesB, bonesB = C.sb([128, 128], F32, "onesB")
    op("pool", lambda e: e.memset(onesB[:], 1.0), writes=[bonesB])
    op("pool", lambda e: e.affine_select(out=identf[:], in_=onesB[:], pattern=[[-1, 128]], compare_op=ALU.is_equal,
                                         fill=0.0, base=0, channel_multiplier=1), reads=[bonesB], writes=[bident])
    rb, brb = C.sb([128, 64], F32, "rbias")
    S.dma("sp", lambda e: e.dma_start(out=rb[:], in_=dr["rbias"]), writes=[brb], sem_buf=brb)
    wr, bwr = C.sb([128, 16, 64], F32, "wr")
    S.dma("sp", lambda e: e.dma_start(out=wr[:], in_=dr["w_router"].rearrange("(c p) n -> p c n", p=128)),
          writes=[bwr], sem_buf=bwr)
    epsc, bepsc = C.sb([128, 2], F32, "epscB")
    op("pool", lambda e: e.memset(epsc[:, 0:1], 1e-5), writes=[bepsc])
    small = {nm: C.sb([128, 8], F32, "smB_" + nm) for nm in ["m8", "gs", "g8", "gm", "t8", "den", "mv", "rstd", "nmr"]}
    st6, bst6 = C.sb([128, 4, 6], F32, "st6")
    ch, bch = C.sb([128, 64], F32, "choice")
    scs, bscs = C.sb([128, 64], F32, "scores")
    mc, bmc = C.sb([128, 64], F32, "mchoice")

    pb = [nc.alloc_psum_tensor(f"pbB{i}", [128, 512], F32) for i in range(8)]
    pq = [Buf(f"pbB{i}", excl=True) for i in range(8)]

    def AR(c0, n):
        return ARENA[:, c0:c0 + n]

    ACCb = ACC[:].rearrange("p t d -> p (t d)").bitcast(BF16)

    def XH(c0, n):
        return ACCb[:, c0:c0 + n]

    wba_v = dr["w_branch_a"].rearrange("(c p) n -> p c n", p=128)
    wbb_v = dr["w_branch_b"].rearrange("(c p) n -> p c n", p=128)
    for c in range(8):
        S.dma("pool", lambda e, c=c: e.dma_start(out=AR(c * 2048, 2048), in_=wba_v[:, c, :]),
              writes=[bA[0]], sem_buf=bA[0], same_gen=True)
    for c in range(8):
        S.dma("pool", lambda e, c=c: e.dma_start(out=AR(16384 + c * 2048, 2048), in_=wbb_v[:, c, :]),
              writes=[bA[1]], sem_buf=bA[1], same_gen=True)
    xTs = dr["xTs"].rearrange("(c p) t -> p c t", p=128)
    yall = dr["yall"].rearrange("(r p) t -> p r t", p=128)
    tok0 = dr["tok0"]
    bX = Buf("xhalf")
    bY = Buf("yhalf")
    for half in range(2):
        for c in range(16):
            S.dma("pool", lambda e, c=c, half=half: e.dma_start(
                out=XH(c * 512, 512), in_=xTs[:, c, half * 512:(half + 1) * 512]),
                writes=[bX], sem_buf=bX, same_gen=(c > 0))
        for r in range(16):
            S.dma("sp", lambda e, r=r, half=half: e.dma_start(
                out=XH(8192 + r * 512, 512), in_=yall[:, r, tok0 + half * 512:tok0 + (half + 1) * 512]),
                reads=yall_bufs, writes=[bY], sem_buf=bY, same_gen=(r > 0))
        for m in range(16):
            wg, bwg = WGT[m % 2]
            S.dma("pool", lambda e, wg=wg, m=m: e.dma_start(out=wg[:], in_=dr["wgates"][m]),
                  writes=[bwg], sem_buf=bwg)
            for ab in range(2):
                for c in range(16):
                    op("pe", lambda e, ab=ab, c=c, wg=wg: e.matmul(
                        pb[ab][:], lhsT=wg[:, c * 256 + ab * 128:c * 256 + ab * 128 + 128], rhs=XH(c * 512, 512),
                        start=(c == 0), stop=(c == 15)), reads=[bwg, bX], writes=[pq[ab]])
            for ab in range(2):
                for hh in range(8):
                    op("pe", lambda e, ab=ab, hh=hh, m=m: e.matmul(
                        pb[2 + ab][:], lhsT=AR(ab * 16384 + hh * 2048 + m * 128, 128),
                        rhs=XH(8192 + (2 * hh + ab) * 512, 512), start=(hh == 0), stop=(hh == 7)),
                        reads=[bA[ab], bY], writes=[pq[2 + ab]])
            t0_, bt0 = tmpf[0]; t1_, bt1 = tmpf[1]
            op("act", lambda e: e.activation(out=t0_[:], in_=pb[0][:], func=AF.Sigmoid), reads=[pq[0]], writes=[bt0])
            op("act", lambda e: e.activation(out=t1_[:], in_=pb[1][:], func=AF.Sigmoid), reads=[pq[1]], writes=[bt1])
            op("dve", lambda e: e.tensor_tensor(out=t0_[:], in0=t0_[:], in1=pb[2][:], op=ALU.mult),
               reads=[bt0, pq[2]], writes=[bt0])
            op("dve", lambda e: e.tensor_tensor(out=t1_[:], in0=t1_[:], in1=pb[3][:], op=ALU.mult),
               reads=[bt1, pq[3]], writes=[bt1])
            op("pool", lambda e, m=m, half=half: e.tensor_tensor(out=MT[:, m, half * 512:(half + 1) * 512], in0=t0_[:],
                                                                 in1=t1_[:], op=ALU.add), reads=[bt0, bt1], writes=[bMT[m]])

    wout_v = dr["w_out"].rearrange("(c p) n -> p c n", p=128)
    bWO = Buf("wout")
    for c in range(16):
        S.dma("pool", lambda e, c=c: e.dma_start(out=AR(c * 2048, 2048), in_=wout_v[:, c, :]),
              reads=[], writes=[bA[0], bA[1], bWO], sem_buf=bWO, same_gen=(c > 0))
    lnp = ARENA[:, 32768:40960].bitcast(F32).rearrange("p (a d) -> p a d", a=2)
    blnp = Buf("lnp")
    S.dma("sp", lambda e: e.dma_start(out=lnp, in_=dr["ln1"]), reads=[], writes=[blnp], sem_buf=blnp)
    xrows = dr["xrows"].rearrange("(t p) d -> p t d", p=128)
    for tt in range(8):
        S.dma("sp", lambda e, tt=tt: e.dma_start(out=ACC[:, tt, :], in_=xrows[:, tt, :]),
              writes=[bACC[tt], bX, bY], sem_buf=bACC[tt], same_gen=(tt > 0))
    hb = 0
    for tt in range(8):
        for dg in range(4):
            bank = 4 + (hb % 2)
            hb += 1
            for m in range(16):
                op("pe", lambda e, bank=bank, m=m, tt=tt, dg=dg: e.matmul(
                    pb[bank][:], lhsT=MT[:, m, tt * 128:(tt + 1) * 128], rhs=AR(m * 2048 + dg * 512, 512),
                    start=(m == 0), stop=(m == 15)), reads=[bMT[m], bWO], writes=[pq[bank]])
            op("dve", lambda e, bank=bank, tt=tt, dg=dg: e.scalar_tensor_tensor(
                out=ACC[:, tt, dg * 512:(dg + 1) * 512], in0=ACC[:, tt, dg * 512:(dg + 1) * 512], scalar=ALPHA,
                in1=pb[bank][:], op0=ALU.mult, op1=ALU.add), reads=[bACC[tt], pq[bank]], writes=[bACC[tt]])

    def layer_norm(tt, prm, bprm):
        mv, bmv = small["mv"]; rstd, brstd = small["rstd"]
        for q in range(4):
            op("dve", lambda e, q=q, tt=tt: e.bn_stats(out=st6[:, q, :], in_=ACC[:, tt, q * 512:(q + 1) * 512]),
               reads=[bACC[tt]], writes=[bst6])
        op("dve", lambda e: e.bn_aggr(out=mv[:, 0:2], in_=st6[:].rearrange("p a b -> p (a b)")), reads=[bst6], writes=[bmv])
        op("act", lambda e: e.activation(out=rstd[:, 0:1], in_=mv[:, 1:2], func=AF.Ln, bias=epsc[:, 0:1]),
           reads=[bmv, bepsc], writes=[brstd])
        op("act", lambda e: e.activation(out=rstd[:, 0:1], in_=rstd[:, 0:1], func=AF.Exp, scale=-0.5),
           reads=[brstd], writes=[brstd])
        op("dve", lambda e, tt=tt: e.tensor_scalar(out=ACC[:, tt, :], in0=ACC[:, tt, :], scalar1=mv[:, 0:1],
                                                   scalar2=rstd[:, 0:1], op0=ALU.subtract, op1=ALU.mult),
           reads=[bACC[tt], bmv, brstd], writes=[bACC[tt]])
        op("pool", lambda e, tt=tt: e.tensor_tensor(out=ACC[:, tt, :], in0=ACC[:, tt, :], in1=prm[:, 0, :], op=ALU.mult),
           reads=[bACC[tt], bprm], writes=[bACC[tt]])
        op("pool", lambda e, tt=tt: e.tensor_tensor(out=ACC[:, tt, :], in0=ACC[:, tt, :], in1=prm[:, 1, :], op=ALU.add),
           reads=[bACC[tt], bprm], writes=[bACC[tt]])

    for tt in range(8):
        layer_norm(tt, lnp, blnp)
        xf, bX1F = WGT[tt % 2]
        X1F = xf[:].bitcast(F32)
        for c4 in range(4):
            bank = 6 + (c4 % 2)
            for k in range(4):
                c = c4 * 4 + k
                op("pe", lambda e, bank=bank, k=k, c=c, tt=tt: e.transpose(
                    pb[bank][:, k * 128:(k + 1) * 128], ACC[:, tt, c * 128:(c + 1) * 128], identf[:]),
                    reads=[bACC[tt], bident], writes=[pq[bank]])
            op("act", lambda e, bank=bank, c4=c4, tt=tt: e.activation(
                out=MT[:, c4 * 4:(c4 + 1) * 4, tt * 128:(tt + 1) * 128],
                in_=pb[bank][:].rearrange("p (k t) -> p k t", k=4), func=AF.Copy),
                reads=[pq[bank]], writes=[bMT[c4 * 4 + k] for k in range(4)])
            op("dve", lambda e, bank=bank, c4=c4, X1F=X1F: e.tensor_copy(out=X1F[:, c4 * 512:(c4 + 1) * 512], in_=pb[bank][:]),
               reads=[pq[bank]], writes=[bX1F])
        for c in range(16):
            op("pe", lambda e, c=c, X1F=X1F: e.matmul(pb[0][:, 0:64], lhsT=X1F[:, c * 128:(c + 1) * 128], rhs=wr[:, c, :],
                                             start=(c == 0), stop=(c == 15)), reads=[bX1F, bwr], writes=[pq[0]])
        m8, bm8 = small["m8"]; gs, bgs = small["gs"]; g8, bg8 = small["g8"]; gm, bgm = small["gm"]
        t8, bt8 = small["t8"]; den, bden = small["den"]
        op("act", lambda e: e.activation(out=scs[:], in_=pb[0][:, 0:64], func=AF.Sigmoid), reads=[pq[0]], writes=[bscs])
        op("dve", lambda e: e.tensor_tensor(out=ch[:], in0=scs[:], in1=rb[:], op=ALU.add), reads=[bscs, brb], writes=[bch])
        for g in range(8):
            op("dve", lambda e, g=g: e.max(out=m8[:], in_=ch[:, g * 8:(g + 1) * 8]), reads=[bch], writes=[bm8])
            op("dve", lambda e, g=g: e.tensor_tensor(out=gs[:, g:g + 1], in0=m8[:, 0:1], in1=m8[:, 1:2], op=ALU.add),
               reads=[bm8], writes=[bgs])
        op("dve", lambda e: e.max(out=g8[:], in_=gs[:]), reads=[bgs], writes=[bg8])
        op("dve", lambda e: e.tensor_scalar(out=gm[:], in0=gs[:], scalar1=g8[:, 3:4], scalar2=None, op0=ALU.is_ge),
           reads=[bgs, bg8], writes=[bgm])
        op("dve", lambda e: e.tensor_scalar(out=gm[:], in0=gm[:], scalar1=-1.0, scalar2=1e30, op0=ALU.add, op1=ALU.mult),
           reads=[bgm], writes=[bgm])
        for g in range(8):
            op("dve", lambda e, g=g: e.tensor_scalar(out=mc[:, g * 8:(g + 1) * 8], in0=ch[:, g * 8:(g + 1) * 8],
                                                     scalar1=gm[:, g:g + 1], scalar2=None, op0=ALU.add),
               reads=[bch, bgm], writes=[bmc])
        op("dve", lambda e: e.max(out=t8[:], in_=mc[:]), reads=[bmc], writes=[bt8])
        op("dve", lambda e: e.tensor_scalar(out=mc[:], in0=mc[:], scalar1=t8[:, 7:8], scalar2=None, op0=ALU.is_ge),
           reads=[bmc, bt8], writes=[bmc])
        op("dve", lambda e: e.tensor_tensor(out=mc[:], in0=mc[:], in1=scs[:], op=ALU.mult), reads=[bmc, bscs], writes=[bmc])
        op("dve", lambda e: e.tensor_reduce(out=den[:, 0:1], in_=mc[:], axis=AX.X, op=ALU.add), reads=[bmc], writes=[bden])
        op("dve", lambda e: e.reciprocal(out=den[:, 0:1], in_=den[:, 0:1]), reads=[bden], writes=[bden])
        op("dve", lambda e, tt=tt: e.tensor_scalar(out=Gt[:, tt, :], in0=mc[:], scalar1=den[:, 0:1], scalar2=2.5,
                                                   op0=ALU.mult, op1=ALU.mult), reads=[bmc, bden], writes=[bGt])
        op("act", lambda e, tt=tt: e.activation(out=ACC[:, tt, :], in_=ACC[:, tt, :], func=AF.Copy, scale=ALPHA),
           reads=[bACC[tt]], writes=[bACC[tt]])

    bEW = [Buf("ew0"), Buf("ew1")]
    bWD = Buf("ewd")
    yb_ctr = 0
    for e_i in range(n_exp + 1):
        base = (e_i % 2) * 16384
        bew = bEW[e_i % 2]
        if e_i < n_exp:
            srcs = [dr["w_gate"][e_i].rearrange("(c p) f -> p c f", p=128), dr["w_up"][e_i].rearrange("(c p) f -> p c f", p=128),
                    dr["w_down"][e_i].rearrange("(c p) n -> p c n", p=128)]
        else:
            srcs = [dr["ws_gate"].rearrange("(c p) f -> p c f", p=128), dr["ws_up"].rearrange("(c p) f -> p c f", p=128),
                    dr["ws_down"].rearrange("(c p) n -> p c n", p=128)]
        extra_w = [bA[0], bA[1], bWO, blnp] if e_i < 2 else []
        for wi in range(2):
            dstv = ARENA[:, base + wi * 8192:base + (wi + 1) * 8192].rearrange("p (c f) -> p c f", c=16)
            S.dma("pool", lambda e, dstv=dstv, src=srcs[wi]: e.dma_start(out=dstv, in_=src),
                  writes=[bew] + extra_w, sem_buf=bew, same_gen=(wi > 0))
        dstv = ARENA[:, 32768:40960].rearrange("p (c n) -> p c n", c=4)
        S.dma("pool", lambda e, dstv=dstv, src=srcs[2]: e.dma_start(out=dstv, in_=src),
              writes=[bWD] + extra_w, sem_buf=bWD)
        hT, bhT = WGT[e_i % 2]
        sg_, bsg = tmpf[0]
        for half in range(2):
            for fc in range(4):
                for gu in range(2):
                    bank = gu * 2 + (fc % 2)
                    for c in range(16):
                        op("pe", lambda e, bank=bank, gu=gu, fc=fc, c=c, base=base, half=half: e.matmul(
                            pb[bank][:], lhsT=AR(base + gu * 8192 + c * 512 + fc * 128, 128),
                            rhs=MT[:, c, half * 512:(half + 1) * 512], start=(c == 0), stop=(c == 15)),
                            reads=[bew, bMT[c]], writes=[pq[bank]])
                bg_, bu_ = fc % 2, 2 + (fc % 2)
                op("act", lambda e, bg_=bg_: e.activation(out=sg_[:], in_=pb[bg_][:], func=AF.Silu), reads=[pq[bg_]], writes=[bsg])
                op("dve", lambda e, bu_=bu_, hT=hT, fc=fc, half=half: e.tensor_tensor(
                    out=hT[:, fc * 1024 + half * 512:fc * 1024 + (half + 1) * 512], in0=sg_[:], in1=pb[bu_][:], op=ALU.mult),
                    reads=[bsg, pq[bu_]], writes=[bhT])
        for tt in range(8):
            for dg in range(4):
                bank = 4 + (yb_ctr % 4)
                yb_ctr += 1
                for fc in range(4):
                    op("pe", lambda e, bank=bank, fc=fc, tt=tt, dg=dg, hT=hT, base=base: e.matmul(
                        pb[bank][:], lhsT=hT[:, fc * 1024 + tt * 128:fc * 1024 + (tt + 1) * 128],
                        rhs=AR(32768 + fc * 2048 + dg * 512, 512), start=(fc == 0), stop=(fc == 3)),
                        reads=[bhT, bWD], writes=[pq[bank]])
                if e_i < n_exp:
                    op("dve", lambda e, bank=bank, tt=tt, dg=dg, e_i=e_i: e.scalar_tensor_tensor(
                        out=ACC[:, tt, dg * 512:(dg + 1) * 512], in0=pb[bank][:], scalar=Gt[:, tt, e_i:e_i + 1],
                        in1=ACC[:, tt, dg * 512:(dg + 1) * 512], op0=ALU.mult, op1=ALU.add),
                        reads=[pq[bank], bGt, bACC[tt]], writes=[bACC[tt]])
                else:
                    op("dve", lambda e, bank=bank, tt=tt, dg=dg: e.tensor_tensor(
                        out=ACC[:, tt, dg * 512:(dg + 1) * 512], in0=pb[bank][:], in1=ACC[:, tt, dg * 512:(dg + 1) * 512],
                        op=ALU.add), reads=[pq[bank], bACC[tt]], writes=[bACC[tt]])

    ln2base = ((n_exp + 1) % 2) * 16384
    lnp2 = ARENA[:, ln2base:ln2base + 8192].bitcast(F32).rearrange("p (a d) -> p a d", a=2)
    blnp2 = Buf("lnp2")
    S.dma("sp", lambda e: e.dma_start(out=lnp2, in_=dr["ln2"]), writes=[bEW[(n_exp + 1) % 2], blnp2], sem_buf=blnp2)
    b_out = Buf("outB")
    outv = dr["out"].rearrange("(t p) d -> p t d", p=128)
    for tt in range(8):
        layer_norm(tt, lnp2, blnp2)
        S.dma("sp", lambda e, tt=tt: e.dma_start(out=outv[:, tt, :], in_=ACC[:, tt, :]), reads=[bACC[tt]], writes=[b_out],
              sem_buf=bACC[tt], same_gen=True)
    return [b_out]


def host_prep_b(inp, yall):
    x = inp["x"][0]
    w_in = inp["w_in"][0]
    ga = w_in[:, 7184:7184 + 2048].reshape(16, 128, 16, 128)
    gb = w_in[:, 9232:9232 + 2048].reshape(16, 128, 16, 128)
    wg = np.stack([ga, gb], axis=3)
    wgates = np.ascontiguousarray(wg.transpose(2, 1, 0, 3, 4).reshape(16, 128, 16 * 256))
    common = {
        "wgates": wgates, "yall": yall,
        "w_branch_a": inp["w_branch_a"][0], "w_branch_b": inp["w_branch_b"][0], "w_out": inp["w_out"][0],
        "ln1": np.ascontiguousarray(np.broadcast_to(np.stack([inp["ln1_g"][0], inp["ln1_b"][0]])[None], (128, 2, D))),
        "ln2": np.ascontiguousarray(np.broadcast_to(np.stack([inp["ln2_g"][0], inp["ln2_b"][0]])[None], (128, 2, D))),
        "rbias": np.ascontiguousarray(np.broadcast_to(inp["router_bias"][0][None], (128, 64))),
        "w_router": inp["w_router"][0],
        "w_gate": inp["w_gate"][0], "w_up": inp["w_up"][0], "w_down": inp["w_down"][0],
        "ws_gate": inp["ws_gate"][0], "ws_up": inp["ws_up"][0], "ws_down": inp["ws_down"][0],
    }
    maps = []
    xT = None
    for c in range(NCORE):
        xr = np.ascontiguousarray(x[c * TPC:(c + 1) * TPC])
        m = dict(common)
        m["xrows"] = xr
        m["xTs"] = np.ascontiguousarray(xr.T)
        maps.append(m)
    return maps


def declare_b(nc, dr, n_exp=N_EXP, fused=False):
    def din(name, shape, dt=F32):
        dr[name] = nc.dram_tensor(name, list(shape), dt, kind="ExternalInput").ap()
    din("wgates", [16, 128, 4096])
    din("w_branch_a", [1024, D]); din("w_branch_b", [1024, D]); din("w_out", [D, D])
    din("ln1", [128, 2, D]); din("ln2", [128, 2, D]); din("rbias", [128, 64]); din("w_router", [D, 64])
    din("w_gate", [64, D, 512]); din("w_up", [64, D, 512]); din("w_down", [64, 512, D])
    din("ws_gate", [D, 512]); din("ws_up", [D, 512]); din("ws_down", [512, D])
    din("xrows", [TPC, D]); din("xTs", [D, TPC])
    dr["out"] = nc.dram_tensor("out", [TPC, D], F32, kind="ExternalOutput").ap()


def build_nc_b(n_exp=N_EXP):
    nc = bass.Bass("TRN2", target_bir_lowering=False)
    dr = {}
    declare_b(nc, dr, n_exp)
    dr["yall"] = nc.dram_tensor("yall", [2048, TPC], BF16, kind="ExternalInput").ap()
    dr["tok0"] = 0
    S = Sched(nc)
    outs = build_phase_b(nc, S, dr, n_exp=n_exp)
    S.wait_final("sp", outs)
    S.emit()
    return nc, S


def _run_two_launch(inputs):
    maps_a = host_prep_a(inputs)
    nc_a, _ = build_nc_a()
    res_a = run_bass_kernel_spmd(nc_a, maps_a, core_ids=list(range(NCORE)))
    rows = []
    for h in range(NCORE):
        rows.append(np.asarray(res_a.results[h]["yaT"]))
        rows.append(np.asarray(res_a.results[h]["ybT"]))
    yall = np.concatenate(rows, axis=0)
    maps_b = host_prep_b(inputs, None)
    for c in range(NCORE):
        maps_b[c]["yall"] = np.ascontiguousarray(yall[:, c * TPC:(c + 1) * TPC])
    nc_b, _ = build_nc_b()
    res_b = run_bass_kernel_spmd(nc_b, maps_b, core_ids=list(range(NCORE)))
    out = np.concatenate([np.asarray(res_b.results[c]["out"]) for c in range(NCORE)], axis=0)
    return out.reshape(1, T, D).astype(np.float32)


def kernel(**inputs):
    inputs = {k: np.asarray(v) for k, v in inputs.items()}
    return _run_two_launch(inputs)
```
